# Optimizing a Trainium2 kernel written in Bass

```python
import math
import jax, jax.numpy as jnp
from jax import lax
import numpy as np

D_MODEL = 1024
BATCH = 16
SEQ = 2048
DEPTH = 2

MIX_WIDTH = D_MODEL
M_DK = 64
M_DV = 64
M_WIDTH = MIX_WIDTH // 4
M_HEADS = M_WIDTH // M_DV
M_QK_WIDTH = M_HEADS * M_DK
M_CHUNK = 64
CONV_W = 4
A_DH = 64
A_WIDTH = MIX_WIDTH // 4
A_HEADS = A_WIDTH // A_DH
MOBA_BLOCK = 256
MOBA_TOPK = 3
MOBA_QCHUNK = 16
DF_DQK = 64
DF_DV = 2 * DF_DQK
DF_WIDTH = MIX_WIDTH // 2
DF_HEADS = DF_WIDTH // DF_DV
DF_QK_WIDTH = DF_HEADS * 2 * DF_DQK
Q_BLOCK = 128
N_SOFTMAX_HEADS = A_HEADS + DF_HEADS
D_FF = 2816
RMS_EPS = 1e-6
SPLIT_SIZES = (M_QK_WIDTH, M_QK_WIDTH, M_WIDTH, M_WIDTH, M_HEADS, M_HEADS,
               A_WIDTH, A_WIDTH, A_WIDTH,
               DF_QK_WIDTH, DF_QK_WIDTH, DF_WIDTH)
PROJ_WIDTH = sum(SPLIT_SIZES)

kernel_name = "hymba_style_mlstm_moba_diffattn_macaron"

F32 = jnp.float32


def rmsnorm(x, g):
    xf = x.astype(F32)
    y = xf * lax.rsqrt(jnp.mean(xf * xf, axis=-1, keepdims=True) + RMS_EPS) * g.astype(F32)
    return y.astype(x.dtype)


def swiglu(h, w_gate, w_up, w_down):
    return (jax.nn.silu(h @ w_gate) * (h @ w_up)) @ w_down


def split_cols(p):
    outs, o = [], 0
    for s in SPLIT_SIZES:
        outs.append(p[..., o:o + s])
        o += s
    return outs


def to_heads(t, n_heads):
    b, s, _ = t.shape
    return t.reshape(b, s, n_heads, -1).transpose(0, 2, 1, 3)


def from_heads(t):
    b, h, s, d = t.shape
    return t.transpose(0, 2, 1, 3).reshape(b, s, h * d)


def causal_conv(u, w):
    s = u.shape[1]
    up = jnp.pad(u, ((0, 0), (CONV_W - 1, 0), (0, 0)))
    return sum(up[:, j:j + s] * w[j] for j in range(CONV_W))


def alibi_slopes():
    n = N_SOFTMAX_HEADS
    return jnp.exp2(-8.0 * jnp.arange(1, n + 1, dtype=F32) / n)


def mlstm_chunkwise(q, k, v, i_pre, f_pre):
    b_, h_, s_, dk = q.shape
    dv = v.shape[-1]
    L = M_CHUNK
    nc = s_ // L
    qc = q.astype(F32).reshape(b_, h_, nc, L, dk)
    kc = k.astype(F32).reshape(b_, h_, nc, L, dk) * (dk ** -0.5)
    vc = v.astype(F32).reshape(b_, h_, nc, L, dv)
    log_f = jax.nn.log_sigmoid(f_pre.astype(F32)).reshape(b_, h_, nc, L)
    ig = i_pre.astype(F32).reshape(b_, h_, nc, L)
    bcum = jnp.cumsum(log_f, axis=-1)
    g = bcum[..., -1]
    a = g[..., None] - bcum + ig
    m_loc = jnp.max(a, axis=-1)
    wgt = jnp.exp(a - m_loc[..., None])
    c_loc = jnp.einsum('bhcl,bhcld,bhcle->bhcde', wgt, kc, vc)
    n_loc = jnp.einsum('bhcl,bhcld->bhcd', wgt, kc)

    def step(carry, xs):
        c_st, n_st, m_st = carry
        g_c, m_l, c_l, n_l = xs
        m_new = jnp.maximum(g_c + m_st, m_l)
        sp = jnp.exp(g_c + m_st - m_new)
        sl = jnp.exp(m_l - m_new)
        c_new = sp[..., None, None] * c_st + sl[..., None, None] * c_l
        n_new = sp[..., None] * n_st + sl[..., None] * n_l
        return (c_new, n_new, m_new), (c_st, n_st, m_st)

    init = (jnp.zeros((b_, h_, dk, dv), F32), jnp.zeros((b_, h_, dk), F32), jnp.zeros((b_, h_), F32))
    xs = (jnp.moveaxis(g, 2, 0), jnp.moveaxis(m_loc, 2, 0),
          jnp.moveaxis(c_loc, 2, 0), jnp.moveaxis(n_loc, 2, 0))
    _, (c_prev, n_prev, m_prev) = lax.scan(step, init, xs)
    c_prev = jnp.moveaxis(c_prev, 0, 2)
    n_prev = jnp.moveaxis(n_prev, 0, 2)
    m_prev = jnp.moveaxis(m_prev, 0, 2)

    causal = jnp.tril(jnp.ones((L, L), dtype=bool))
    dmat = bcum[..., :, None] - bcum[..., None, :] + ig[..., None, :]
    dmat = jnp.where(causal, dmat, -jnp.inf)
    m_inter = bcum + m_prev[..., None]
    m_t = jnp.maximum(m_inter, jnp.max(dmat, axis=-1))
    scores = jnp.einsum('bhctd,bhcsd->bhcts', qc, kc) * jnp.exp(dmat - m_t[..., None])
    inter_w = jnp.exp(m_inter - m_t)
    num = (jnp.einsum('bhcts,bhcse->bhcte', scores, vc)
           + inter_w[..., None] * jnp.einsum('bhctd,bhcde->bhcte', qc, c_prev))
    den = jnp.sum(scores, axis=-1) + inter_w * jnp.einsum('bhctd,bhcd->bhct', qc, n_prev)
    h = num / jnp.maximum(jnp.abs(den), jnp.exp(-m_t))[..., None]
    return h.reshape(b_, h_, s_, dv)


def moba_attention(q, k, v, slopes):
    b_, h_, s_, dh = q.shape
    nb = -(-s_ // MOBA_BLOCK)
    topk = min(MOBA_TOPK, nb)
    pad = nb * MOBA_BLOCK - s_
    kp = jnp.pad(k, ((0, 0), (0, 0), (0, pad), (0, 0)))
    vp = jnp.pad(v, ((0, 0), (0, 0), (0, pad), (0, 0)))
    kb = kp.reshape(b_, h_, nb, MOBA_BLOCK, dh)
    vb = vp.reshape(b_, h_, nb, MOBA_BLOCK, dh)
    k_mean = jnp.mean(kb.astype(F32), axis=3)
    scale = dh ** -0.5
    bi = jnp.arange(b_)[:, None, None, None]
    hi = jnp.arange(h_)[None, :, None, None]
    blk_pos = jnp.arange(MOBA_BLOCK)

    def one_chunk(c):
        start = c * MOBA_QCHUNK
        qs = lax.dynamic_slice_in_dim(q, start, MOBA_QCHUNK, axis=2).astype(F32)
        t = start + jnp.arange(MOBA_QCHUNK)
        j = start // MOBA_BLOCK
        gate = jnp.einsum('bhqd,bhnd->bhqn', qs, k_mean)
        gate = jnp.where(jnp.arange(nb) < j, gate, -jnp.inf)
        _, idx = lax.top_k(gate, topk)
        valid = jnp.arange(topk) < j
        ksel = kb[bi, hi, idx]
        vsel = vb[bi, hi, idx]
        pos_sel = idx[..., None] * MOBA_BLOCK + blk_pos
        s_sel = (jnp.einsum('bhqd,bhqrkd->bhqrk', qs, ksel) * scale
                 - slopes[:, None, None, None] * (t[:, None, None] - pos_sel).astype(F32))
        s_sel = jnp.where(valid[:, None], s_sel, -jnp.inf).reshape(b_, h_, MOBA_QCHUNK, topk * MOBA_BLOCK)
        kown = lax.dynamic_slice_in_dim(kp, j * MOBA_BLOCK, MOBA_BLOCK, axis=2)
        vown = lax.dynamic_slice_in_dim(vp, j * MOBA_BLOCK, MOBA_BLOCK, axis=2)
        dist_own = (t[:, None] - (j * MOBA_BLOCK + blk_pos)[None, :])
        s_own = (jnp.einsum('bhqd,bhkd->bhqk', qs, kown) * scale
                 - slopes[:, None, None] * dist_own.astype(F32))
        s_own = jnp.where(dist_own >= 0, s_own, -jnp.inf)
        p = jax.nn.softmax(jnp.concatenate([s_sel, s_own], axis=-1), axis=-1)
        p_sel = p[..., :topk * MOBA_BLOCK].reshape(b_, h_, MOBA_QCHUNK, topk, MOBA_BLOCK)
        return (jnp.einsum('bhqrk,bhqrke->bhqe', p_sel, vsel)
                + jnp.einsum('bhqk,bhke->bhqe', p[..., topk * MOBA_BLOCK:], vown))

    out = lax.map(one_chunk, jnp.arange(s_ // MOBA_QCHUNK))
    return jnp.moveaxis(out, 0, 2).reshape(b_, h_, s_, dh)


def diff_attention(q, k, v, lam, lam_init, g_sub, slopes):
    b_, h_, _, s_, d = q.shape
    scale = d ** -0.5
    key_pos = jnp.arange(s_)

    def one_block(c):
        start = c * Q_BLOCK
        qs = lax.dynamic_slice_in_dim(q, start, Q_BLOCK, axis=3).astype(F32)
        t = start + jnp.arange(Q_BLOCK)
        dist = t[:, None] - key_pos[None, :]
        s = (jnp.einsum('bhmqd,bhmkd->bhmqk', qs, k) * scale
             - slopes[:, None, None, None] * dist.astype(F32))
        s = jnp.where(dist >= 0, s, -jnp.inf)
        p = jax.nn.softmax(s, axis=-1)
        a = p[:, :, 0] - lam * p[:, :, 1]
        return jnp.einsum('bhqk,bhke->bhqe', a, v)

    out = lax.map(one_block, jnp.arange(s_ // Q_BLOCK))
    out = jnp.moveaxis(out, 0, 2).reshape(b_, h_, s_, v.shape[-1])
    return rmsnorm(out, g_sub) * (1.0 - lam_init)


def setup_inputs(seed: int = 0) -> dict:
    key = jax.random.key(seed)
    ks = jax.random.split(key, 24)
    nrm = lambda k, shape, scale: jax.random.normal(k, shape, F32) * scale
    gain = lambda k, n: 1.0 + 0.05 * jax.random.normal(k, (DEPTH, n), F32)
    return {
        "x": jax.random.normal(ks[0], (BATCH, SEQ, D_MODEL), F32),
        "ffn1_pre_norm": gain(ks[1], D_MODEL),
        "ffn1_w_gate": nrm(ks[2], (DEPTH, D_MODEL, D_FF), D_MODEL ** -0.5),
        "ffn1_w_up": nrm(ks[3], (DEPTH, D_MODEL, D_FF), D_MODEL ** -0.5),
        "ffn1_w_down": nrm(ks[4], (DEPTH, D_FF, D_MODEL), D_FF ** -0.5),
        "ffn1_post_norm": gain(ks[5], D_MODEL),
        "mix_pre_norm": gain(ks[6], D_MODEL),
        "w_in": nrm(ks[7], (DEPTH, D_MODEL, PROJ_WIDTH), D_MODEL ** -0.5),
        "conv_qk": nrm(ks[8], (DEPTH, CONV_W, 2 * M_QK_WIDTH), CONV_W ** -0.5),
        "igate_bias": nrm(ks[9], (DEPTH, M_HEADS), 0.1),
        "fgate_bias": jnp.linspace(3.0, 6.0, M_HEADS, dtype=F32)[None, :] + nrm(ks[10], (DEPTH, M_HEADS), 0.1),
        "lambda_q1": nrm(ks[11], (DEPTH, DF_DQK), 0.1),
        "lambda_k1": nrm(ks[12], (DEPTH, DF_DQK), 0.1),
        "lambda_q2": nrm(ks[13], (DEPTH, DF_DQK), 0.1),
        "lambda_k2": nrm(ks[14], (DEPTH, DF_DQK), 0.1),
        "diff_subln": gain(ks[15], DF_DV),
        "w_out": nrm(ks[16], (DEPTH, MIX_WIDTH, D_MODEL), MIX_WIDTH ** -0.5),
        "mix_post_norm": gain(ks[17], D_MODEL),
        "ffn2_pre_norm": gain(ks[18], D_MODEL),
        "ffn2_w_gate": nrm(ks[19], (DEPTH, D_MODEL, D_FF), D_MODEL ** -0.5),
        "ffn2_w_up": nrm(ks[20], (DEPTH, D_MODEL, D_FF), D_MODEL ** -0.5),
        "ffn2_w_down": nrm(ks[21], (DEPTH, D_FF, D_MODEL), D_FF ** -0.5),
        "ffn2_post_norm": gain(ks[22], D_MODEL),
    }


def reference(x, ffn1_pre_norm, ffn1_w_gate, ffn1_w_up, ffn1_w_down, ffn1_post_norm,
              mix_pre_norm, w_in, conv_qk, igate_bias, fgate_bias,
              lambda_q1, lambda_k1, lambda_q2, lambda_k2, diff_subln, w_out, mix_post_norm,
              ffn2_pre_norm, ffn2_w_gate, ffn2_w_up, ffn2_w_down, ffn2_post_norm):
    slopes = alibi_slopes()
    moba_slopes = slopes[0::2]
    diff_slopes = slopes[1::2]
    for l in range(DEPTH):
        h = rmsnorm(x, ffn1_pre_norm[l])
        x = x + 0.5 * rmsnorm(swiglu(h, ffn1_w_gate[l], ffn1_w_up[l], ffn1_w_down[l]), ffn1_post_norm[l])

        h = rmsnorm(x, mix_pre_norm[l])
        (mq, mk, mv, mo, mi, mf, aq, ak, av, dq, dk, dv) = split_cols(h @ w_in[l])
        b_, s_, _ = h.shape

        qk = jax.nn.silu(causal_conv(jnp.concatenate([mq, mk], axis=-1), conv_qk[l]))
        i_pre = (mi.astype(F32) + igate_bias[l].astype(F32)).transpose(0, 2, 1)
        f_pre = (mf.astype(F32) + fgate_bias[l].astype(F32)).transpose(0, 2, 1)
        h_m = mlstm_chunkwise(to_heads(qk[..., :M_QK_WIDTH], M_HEADS), to_heads(qk[..., M_QK_WIDTH:], M_HEADS),
                              to_heads(mv, M_HEADS), i_pre, f_pre)
        y_m = from_heads(h_m) * jax.nn.sigmoid(mo.astype(F32))

        y_a = from_heads(moba_attention(to_heads(aq, A_HEADS), to_heads(ak, A_HEADS),
                                        to_heads(av, A_HEADS), moba_slopes))

        dq5 = dq.reshape(b_, s_, DF_HEADS, 2, DF_DQK).transpose(0, 2, 3, 1, 4)
        dk5 = dk.reshape(b_, s_, DF_HEADS, 2, DF_DQK).transpose(0, 2, 3, 1, 4)
        lam_init = 0.8 - 0.6 * math.exp(-0.3 * l)
        lam = (jnp.exp(jnp.sum(lambda_q1[l].astype(F32) * lambda_k1[l].astype(F32)))
               - jnp.exp(jnp.sum(lambda_q2[l].astype(F32) * lambda_k2[l].astype(F32))) + lam_init)
        y_d = from_heads(diff_attention(dq5, dk5, to_heads(dv, DF_HEADS), lam, lam_init,
                                        diff_subln[l], diff_slopes))

        mix = jnp.concatenate([y_m, y_a, y_d], axis=-1).astype(x.dtype)
        x = x + rmsnorm(mix @ w_out[l], mix_post_norm[l])

        h = rmsnorm(x, ffn2_pre_norm[l])
        x = x + 0.5 * rmsnorm(swiglu(h, ffn2_w_gate[l], ffn2_w_up[l], ffn2_w_down[l]), ffn2_post_norm[l])
    return x
```

```python
import contextlib
import math
import numpy as np
import concourse.bass as bass
import concourse.mybir as mybir
from concourse.bass_utils import run_bass_kernel_spmd

F32 = mybir.dt.float32
BF16 = mybir.dt.bfloat16
AF = mybir.ActivationFunctionType
ALU = mybir.AluOpType
AX = mybir.AxisListType

NCORES = 8
SEQ_PER_CORE = 2
S = 2048
D = 1024
DFF = 2816
L = 2
NBLK = S // 512
NT = S // 128
NEG = -30000.0
EPS = 1e-6
COMPUTE = ("pe", "act", "dve", "pool")


class _Op:
    __slots__ = ("eng", "fn", "waits", "signal", "pos", "dma", "dsem", "dval", "sigval", "uid")
    _n = 0

    def __init__(self, eng, fn, dma):
        _Op._n += 1
        self.uid = _Op._n
        self.eng = eng
        self.fn = fn
        self.waits = []
        self.signal = False
        self.pos = -1
        self.dma = dma
        self.dsem = None
        self.dval = 0
        self.sigval = None


class Prog:
    def __init__(self, nc, es, n_dma_sems=24):
        self.nc = nc
        self.ops = []
        self.state = {}
        self.default = None
        self.eng_obj = {"pe": nc.tensor, "act": nc.scalar, "dve": nc.vector,
                        "pool": nc.gpsimd, "sp": nc.sync}
        self.esem = {e: es.enter_context(nc.semaphore("c_" + e)) for e in COMPUTE}
        self.dsems = [es.enter_context(nc.semaphore("d%d" % i)) for i in range(n_dma_sems)]
        self.nd = n_dma_sems
        self.dsem_val = [0] * n_dma_sems
        self.di = 0
        self.di_sp = 0
        self.nd_sp = 6
        self.ecount = {e: 0 for e in COMPUTE}
        self.epos = {}
        self.waited = {}
        self.dma_waited = {}
        self.bar_tile = es.enter_context(nc.sbuf_tensor("bar_tile", [1, 8], F32))
        self.n_inst = 0

    def op(self, eng, fn, reads=(), writes=(), dma=0):
        o = _Op(eng, fn, dma)
        deps = []
        for k in reads:
            st = self.state.get(k)
            if st is None:
                if self.default is not None:
                    deps.append((self.default, "raw"))
            else:
                if st[0] is not None:
                    deps.append((st[0], "raw"))
                if isinstance(k, tuple) and k[0] == "ps":
                    for r in st[1]:
                        if r.eng != eng:
                            deps.append((r, "war"))
        for k in writes:
            st = self.state.get(k)
            if st is None:
                if self.default is not None:
                    deps.append((self.default, "waw"))
            else:
                if st[0] is not None:
                    deps.append((st[0], "waw"))
                for r in st[1]:
                    deps.append((r, "war"))
        for k in reads:
            st = self.state.get(k)
            if st is None:
                st = self.state[k] = [self.default, []]
            st[1].append(o)
        for k in writes:
            self.state[k] = [o, []]
        o.waits = deps
        self.ops.append(o)
        return o

    def flush(self, final=False):
        nc = self.nc
        allkeys = list(self.state.keys())
        bt = self.bar_tile
        bar = self.op("dve", lambda: nc.vector.memset(bt[0:1, 0:8], 0.0), reads=(), writes=allkeys)
        bar.signal = True
        for o in self.ops:
            self.epos[o.eng] = self.epos.get(o.eng, 0) + 1
            o.pos = self.epos[o.eng]
        plan = []
        for o in self.ops:
            need = {}
            dneed = []
            for (p, kind) in o.waits:
                if p is o:
                    continue
                if p.dma:
                    s = self.dma_waited.setdefault(o.eng, set())
                    if p.uid not in s:
                        s.add(p.uid)
                        dneed.append(p)
                    continue
                if p.eng == o.eng:
                    if o.eng == "pe" or o.eng == "sp":
                        continue
                if p.pos <= self.waited.get((o.eng, p.eng), 0):
                    continue
                if p.eng not in need or need[p.eng].pos < p.pos:
                    need[p.eng] = p
            for pe_, p in need.items():
                self.waited[(o.eng, pe_)] = p.pos
                p.signal = True
            plan.append((list(need.values()), dneed))
        for o, (cw, dw) in zip(self.ops, plan):
            eng = self.eng_obj[o.eng]
            for p in cw:
                eng.wait_ge(self.esem[p.eng], p.sigval)
            for p in dw:
                eng.wait_ge(self.dsems[p.dsem], p.dval)
            if o.dma:
                if o.eng == "sp":
                    k = self.di_sp % self.nd_sp
                    self.di_sp += 1
                else:
                    k = self.nd_sp + self.di % (self.nd - self.nd_sp)
                    self.di += 1
                if self.dsem_val[k]:
                    eng.wait_ge(self.dsems[k], self.dsem_val[k])
                o.dsem = k
                self.dsem_val[k] += 16 * o.dma
                o.dval = self.dsem_val[k]
                o.fn(self.dsems[k])
            else:
                ins = o.fn()
                if o.signal:
                    self.ecount[o.eng] += 1
                    o.sigval = self.ecount[o.eng]
                    ins.then_inc(self.esem[o.eng], 1)
            self.n_inst += 1
        self.ops = []
        self.state = {}
        self.default = bar
        if final:
            for k in range(self.nd):
                if self.dsem_val[k]:
                    nc.sync.wait_ge(self.dsems[k], self.dsem_val[k])
            for e in COMPUTE:
                if e != "dve":
                    self.eng_obj[e].wait_ge(self.esem["dve"], bar.sigval)


M_QK = 256
SPLIT = (256, 256, 256, 256, 4, 4, 256, 256, 256, 512, 512, 512)
OFF = np.cumsum((0,) + SPLIT)
(O_MQ, O_MK, O_MV, O_MO, O_MI, O_MF, O_AQ, O_AK, O_AV, O_DQ, O_DK, O_DV, _) = OFF
NFM = 20
U_MQ, U_MK, U_MO, U_GF, U_GI, U_AQ, U_AK, U_DQ, U_DK = 0, 2, 4, 6, 7, 8, 10, 12, 16

SM_G = 0
SM_CONV = SM_G + L * 6 * 8
SM_BF = SM_CONV + L * 4 * 4
SM_BI = SM_BF + L
SM_GSUB = SM_BI + L
SM_LAM = SM_GSUB + L
SM_SEL4 = SM_LAM + L * 4 * 64
SM_C123 = SM_SEL4 + 4
NSM = SM_C123 + 3

CB_ID = 0
CB_TRI = 128
CB_ONES = 256
CB_ROWSEL = 384
NCB = CB_ROWSEL + 4 * 128


def _host_tables(inp):
    f = np.float32
    small = np.zeros((128, NSM), f)
    names = ["ffn1_pre_norm", "ffn1_post_norm", "mix_pre_norm", "mix_post_norm", "ffn2_pre_norm", "ffn2_post_norm"]
    for l in range(L):
        for n, nm in enumerate(names):
            small[:, SM_G + (l * 6 + n) * 8: SM_G + (l * 6 + n) * 8 + 8] = np.asarray(inp[nm][l], f).reshape(8, 128).T
        cq = np.asarray(inp["conv_qk"][l], f)
        for cc in range(4):
            small[:, SM_CONV + (l * 4 + cc) * 4: SM_CONV + (l * 4 + cc) * 4 + 4] = cq[:, cc * 128:(cc + 1) * 128].T
        for h in range(4):
            small[32 * h:32 * h + 3, SM_BF + l] = np.asarray(inp["fgate_bias"], f)[l, h]
            small[32 * h, SM_BI + l] = np.asarray(inp["igate_bias"], f)[l, h]
        small[:, SM_GSUB + l] = np.asarray(inp["diff_subln"][l], f)
        for v, nm in enumerate(["lambda_q1", "lambda_k1", "lambda_q2", "lambda_k2"]):
            small[:, SM_LAM + (l * 4 + v) * 64: SM_LAM + (l * 4 + v) * 64 + 64] = np.asarray(inp[nm][l], f)[None, :]
    for h in range(4):
        small[32 * h, SM_SEL4 + h] = 1.0
        small[32 * h, SM_C123 + 0] = 1.0
        small[32 * h + 1, SM_C123 + 1] = 1.0
        small[32 * h + 2, SM_C123 + 2] = 1.0
    cb = np.zeros((128, NCB), f)
    cb[:, CB_ID:CB_ID + 128] = np.eye(128, dtype=f)
    pk = np.arange(128)[:, None]
    pq = np.arange(128)[None, :]
    cb[:, CB_TRI:CB_TRI + 128] = np.where(pk > pq, NEG, 0.0)
    cb[:, CB_ONES:CB_ONES + 128] = 1.0
    for h in range(4):
        cb[32 * h:32 * h + 3, CB_ROWSEL + h * 128: CB_ROWSEL + (h + 1) * 128] = 1.0
    t = np.arange(S)
    thi = (t // 128 * 128).astype(f)
    tlo = (t % 128).astype(f)
    qrows = np.stack([np.ones(S, f), np.ones(S, f), thi, tlo]).astype(f)
    slopes = np.exp2(-8.0 * np.arange(1, 9, dtype=np.float64) / 8).astype(f)
    head_slopes = list(slopes[0::2]) + list(slopes[1::2])
    krows = np.zeros((8, 4, S), f)
    for i, sg in enumerate(head_slopes):
        krows[i, 0] = sg * thi
        krows[i, 1] = sg * tlo
        krows[i, 2] = -sg
        krows[i, 3] = -sg
    blockind = (t[None, :] // 256 == np.arange(8)[:, None]).astype(f)
    return small, cb, qrows, krows, blockind, np.eye(128, dtype=f)


def _host_weights(inp):
    f = np.float32
    out = {}
    for k, (g, u, d) in enumerate([("ffn1_w_gate", "ffn1_w_up", "ffn1_w_down"), ("ffn2_w_gate", "ffn2_w_up", "ffn2_w_down")]):
        wg = np.asarray(inp[g], f).reshape(L, 8, 128, 11, 256).transpose(0, 3, 2, 1, 4)
        wu = np.asarray(inp[u], f).reshape(L, 8, 128, 11, 256).transpose(0, 3, 2, 1, 4)
        wd = np.asarray(inp[d], f).reshape(L, 11, 2, 128, 1024).transpose(0, 1, 3, 2, 4)
        out["wg%d" % k] = np.ascontiguousarray(wg).reshape(L, 11, 128, 2048)
        out["wu%d" % k] = np.ascontiguousarray(wu).reshape(L, 11, 128, 2048)
        out["wd%d" % k] = np.ascontiguousarray(wd).reshape(L, 11, 128, 2048)
    w_in = np.asarray(inp["w_in"], f)
    fm = np.zeros((L, NFM, 1024, 128), f)
    for l in range(L):
        w = w_in[l]
        for i in range(2):
            fm[l, U_MQ + i] = w[:, O_MQ + i * 128: O_MQ + (i + 1) * 128]
            fm[l, U_MK + i] = w[:, O_MK + i * 128: O_MK + (i + 1) * 128]
            fm[l, U_MO + i] = w[:, O_MO + i * 128: O_MO + (i + 1) * 128]
            fm[l, U_AQ + i] = w[:, O_AQ + i * 128: O_AQ + (i + 1) * 128]
            fm[l, U_AK + i] = w[:, O_AK + i * 128: O_AK + (i + 1) * 128]
        for h in range(4):
            for r in range(3):
                fm[l, U_GF, :, 32 * h + r] = w[:, O_MF + h]
            fm[l, U_GI, :, 32 * h] = w[:, O_MI + h]
            fm[l, U_DQ + h] = w[:, O_DQ + h * 128: O_DQ + (h + 1) * 128]
            fm[l, U_DK + h] = w[:, O_DK + h * 128: O_DK + (h + 1) * 128]
    out["win_fm"] = np.ascontiguousarray(fm.reshape(L, NFM, 8, 128, 128).transpose(0, 1, 3, 2, 4)).reshape(L, NFM, 128, 1024)
    wv = np.concatenate([w_in[:, :, O_MV:O_MV + 256], w_in[:, :, O_AV:O_AV + 256], w_in[:, :, O_DV:O_DV + 512]], axis=2)
    out["win_v"] = np.ascontiguousarray(wv.reshape(L, 8, 128, 1024).transpose(0, 2, 1, 3)).reshape(L, 128, 8192)
    wo = np.asarray(inp["w_out"], f).reshape(L, 8, 128, 8, 128).transpose(0, 3, 2, 1, 4)
    out["wo"] = np.ascontiguousarray(wo).reshape(L, 8, 128, 1024)
    return out


def build_program(debug=False, nseq=SEQ_PER_CORE, nlayers=L, stop=None):
    nc = bass.Bass("TRN2", target_bir_lowering=False)
    dt_in = lambda name, shape: nc.dram_tensor(name, list(shape), F32, kind="ExternalInput").ap()
    x_d = dt_in("x", (SEQ_PER_CORE, S, D))
    small_d = dt_in("small", (128, NSM))
    cb_d = dt_in("cb", (128, NCB))
    qrows_d = dt_in("qrows", (4, S))
    krows_d = dt_in("krows", (8, 4, S))
    bind_d = dt_in("blockind", (8, S))
    idf_d = dt_in("identf", (128, 128))
    wg_d = [dt_in("wg%d" % k, (L, 11, 128, 2048)) for k in range(2)]
    wu_d = [dt_in("wu%d" % k, (L, 11, 128, 2048)) for k in range(2)]
    wd_d = [dt_in("wd%d" % k, (L, 11, 128, 2048)) for k in range(2)]
    winfm_d = dt_in("win_fm", (L, NFM, 128, 1024))
    winv_d = dt_in("win_v", (L, 128, 8192))
    wo_d = dt_in("wo", (L, 8, 128, 1024))
    out_d = nc.dram_tensor("out", [SEQ_PER_CORE, S, D], F32, kind="ExternalOutput").ap()
    dbg_d = None
    if debug:
        dbg_d = nc.dram_tensor("dbg", [8, 128, 8 * S], F32, kind="ExternalOutput").ap()

    with contextlib.ExitStack() as top:
        P = Prog(nc, top)
        _cnt = [0]

        def sbt(es, name, shape, dt):
            _cnt[0] += 1
            return es.enter_context(nc.sbuf_tensor("%s_%d" % (name, _cnt[0]), shape, dt))
        ps = [top.enter_context(nc.psum_tensor("psb%d" % i, [128, 512], F32)) for i in range(8)]
        pools = {}

        def bank(pool, banks):
            i = pools.get(pool, 0)
            pools[pool] = i + 1
            return banks[i % len(banks)]

        def PK(b):
            return ("ps", b)

        xT = sbt(top, "xT", [128, 8, S], F32)
        small = sbt(top, "small", [128, NSM], F32)
        cb = sbt(top, "cbf", [128, NCB], BF16)
        identf = sbt(top, "identf", [128, 128], F32)
        ghalf = sbt(top, "ghalf", [128, L * 2 * 8], F32)
        lamt = sbt(top, "lamt", [128, 8 * L], F32)
        ident_bf = cb[:, CB_ID:CB_ID + 128]
        tri_bf = cb[:, CB_TRI:CB_TRI + 128]
        ones_bf = cb[:, CB_ONES:CB_ONES + 128]

        def X(c, blk):
            return ("x", c, blk)

        P.op("sp", lambda s: nc.sync.dma_start(out=small[:], in_=small_d).then_inc(s, 16), writes=["small"], dma=1)
        P.op("sp", lambda s: nc.sync.dma_start(out=identf[:], in_=idf_d).then_inc(s, 16), writes=["identf"], dma=1)
        P.op("pool", lambda s: nc.gpsimd.dma_start(out=cb[:], in_=cb_d).then_inc(s, 16), writes=["cb"], dma=1)
        for l in range(L):
            for k, n in enumerate((1, 5)):
                src = small[:, SM_G + (l * 6 + n) * 8: SM_G + (l * 6 + n) * 8 + 8]
                dst = ghalf[:, (l * 2 + k) * 8:(l * 2 + k) * 8 + 8]
                P.op("dve", lambda src=src, dst=dst: nc.vector.tensor_scalar(dst, src, 0.5, None, op0=ALU.mult),
                     reads=["small"], writes=["ghalf"])
        with contextlib.ExitStack() as es0:
            ltmp = sbt(es0, "ltmp", [128, 64], F32)
            lsum = sbt(es0, "lsum", [128, 4], F32)
            for l in range(L):
                lam_init = 0.8 - 0.6 * math.exp(-0.3 * l)
                for j in range(2):
                    a = small[:, SM_LAM + (l * 4 + 2 * j) * 64: SM_LAM + (l * 4 + 2 * j) * 64 + 64]
                    b = small[:, SM_LAM + (l * 4 + 2 * j + 1) * 64: SM_LAM + (l * 4 + 2 * j + 1) * 64 + 64]
                    P.op("dve", lambda a=a, b=b: nc.vector.tensor_tensor(ltmp[:], a, b, ALU.mult), reads=["small", "ltmp"], writes=["ltmp"])
                    P.op("dve", lambda j=j: nc.vector.reduce_sum(out=lsum[:, j:j + 1], in_=ltmp[:], axis=AX.X), reads=["ltmp"], writes=["lsum"])
                P.op("act", lambda: nc.scalar.activation(lsum[:, 2:4], lsum[:, 0:2], AF.Exp), reads=["lsum"], writes=["lsum"])
                P.op("dve", lambda l=l, li=lam_init: nc.vector.scalar_tensor_tensor(
                    out=lamt[:, 8 * l:8 * l + 1], in0=lsum[:, 3:4], scalar=-li, in1=lsum[:, 2:3], op0=ALU.add, op1=ALU.subtract),
                    reads=["lsum"], writes=["lamt"])
                P.op("dve", lambda l=l, li=lam_init: nc.vector.tensor_scalar(
                    lamt[:, 8 * l + 1:8 * l + 2], small[:, SM_GSUB + l:SM_GSUB + l + 1], 1.0 - li, None, op0=ALU.mult),
                    reads=["small", "lamt"], writes=["lamt"])
            P.flush()

        def rms_stats(es, tag, src_fn, src_keys, nchunk, ncols, inv_n, sq_tile, stat_tile, bankset):
            b = bank("gu", bankset)
            for c in range(nchunk):
                P.op("act", lambda c=c: nc.scalar.activation(sq_tile[:, c % 2, :ncols], src_fn(c), AF.Square),
                     reads=[src_keys(c)], writes=[(tag + "sq", c % 2)])
                P.op("pe", lambda c=c, b=b: nc.tensor.matmul(ps[b][:, :ncols], ones_bf, sq_tile[:, c % 2, :ncols],
                                                            start=(c == 0), stop=(c == nchunk - 1)),
                     reads=[(tag + "sq", c % 2), "cb"], writes=[PK(b)])
            P.op("act", lambda b=b: nc.scalar.activation(stat_tile[:, :ncols], ps[b][:, :ncols], AF.Ln, bias=EPS, scale=inv_n),
                 reads=[PK(b)], writes=[tag + "stat"])
            P.op("act", lambda: nc.scalar.activation(stat_tile[:, :ncols], stat_tile[:, :ncols], AF.Exp, scale=-0.5),
                 reads=[tag + "stat"], writes=[tag + "stat"])

        def dump2(idx, c, ap2, n):
            P.op("pool", lambda s: nc.gpsimd.dma_start(out=dbg_d[idx, 0:ap2.shape[0], c * S:c * S + n], in_=ap2).then_inc(s, 16), reads=["__dump"], dma=1)

        def dump(idx, src3, keys):
            if dbg_d is None:
                return
            for c in range(8):
                P.op("pool", lambda s, c=c: nc.gpsimd.dma_start(out=dbg_d[idx, :, c * S:(c + 1) * S], in_=src3[:, c, :]).then_inc(s, 16),
                     reads=list(keys) + ["__dump"], dma=1)

        for sq in range(nseq):
            with contextlib.ExitStack() as es:
                xin = sbt(es, "xin", [128, 2, D], F32)
                for tt in range(NT):
                    P.op("sp", lambda s, tt=tt: nc.sync.dma_start(out=xin[:, tt % 2, :], in_=x_d[sq, tt * 128:(tt + 1) * 128, :]).then_inc(s, 16),
                         writes=[("xin", tt % 2)], dma=1)
                    for c in range(8):
                        b = bank("tr", [0, 1, 2, 3])
                        P.op("pe", lambda tt=tt, c=c, b=b: nc.tensor.transpose(ps[b][:, 0:128], xin[:, tt % 2, c * 128:(c + 1) * 128], identf[:]),
                             reads=[("xin", tt % 2), "identf"], writes=[PK(b)])
                        eng = "act" if c % 2 else "dve"
                        if eng == "act":
                            P.op("act", lambda tt=tt, c=c, b=b: nc.scalar.copy(xT[:, c, tt * 128:(tt + 1) * 128], ps[b][:, 0:128]),
                                 reads=[PK(b)], writes=[("xtile", c, tt)])
                        else:
                            P.op("dve", lambda tt=tt, c=c, b=b: nc.vector.tensor_copy(xT[:, c, tt * 128:(tt + 1) * 128], ps[b][:, 0:128]),
                                 reads=[PK(b)], writes=[("xtile", c, tt)])
                P.flush()

            for l in range(nlayers if stop != "load" else 0):
                for ffn_i in range(2):
                    if stop in ("ffn1", "mlstm_prep", "mlstm", "moba", "diff") and ffn_i == 1:
                        continue
                    with contextlib.ExitStack() as es:
                        hT = sbt(es, "f_hT", [128, 1, 8, 1024], BF16)
                        yb = sbt(es, "f_y", [128, 2, 8, 1024], F32)
                        wgt = sbt(es, "f_wg", [128, 2, 2048], BF16)
                        wut = sbt(es, "f_wu", [128, 2, 2048], BF16)
                        wdt = sbt(es, "f_wd", [128, 2, 2048], BF16)
                        At = sbt(es, "f_A", [128, 2, 2, 1024], BF16)
                        sgt = sbt(es, "f_sg", [128, 2, 512], F32)
                        sqt = sbt(es, "f_sq", [128, 8, 512], BF16)
                        rst = sbt(es, "f_rs", [128, 2, 512], F32)
                        tmp = sbt(es, "f_tmp", [128, 2, 512], F32)
                        n_pre = 0 if ffn_i == 0 else 4
                        gpre = lambda c: small[:, SM_G + (l * 6 + n_pre) * 8 + c: SM_G + (l * 6 + n_pre) * 8 + c + 1]
                        gpost = lambda c: ghalf[:, (l * 2 + ffn_i) * 8 + c:(l * 2 + ffn_i) * 8 + c + 1]

                        def n_src(kind, half, blk):
                            cs = slice(half * 1024 + blk * 512, half * 1024 + (blk + 1) * 512)
                            if kind == "pre":
                                return (lambda c: xT[:, c, cs]), (lambda c: X(c, half * 2 + blk))
                            return (lambda c: yb[:, half, c, blk * 512:(blk + 1) * 512]), (lambda c: ("fy", half, c, blk))

                        def n_sq(kind, half, blk):
                            src, key = n_src(kind, half, blk)
                            for c in range(8):
                                P.op("act", lambda c=c, src=src: nc.scalar.activation(sqt[:, c, :], src(c), AF.Square),
                                     reads=[key(c)], writes=[("fsq", c)])

                        def n_stat(r):
                            b = bank("gu", [0, 1, 2, 3])
                            for c in range(8):
                                P.op("pe", lambda c=c, b=b: nc.tensor.matmul(ps[b][:, :], ones_bf, sqt[:, c, :], start=(c == 0), stop=(c == 7)),
                                     reads=[("fsq", c), "cb"], writes=[PK(b)])
                            P.op("act", lambda b=b: nc.scalar.activation(rst[:, r, :], ps[b][:, :], AF.Ln, bias=EPS, scale=1.0 / D),
                                 reads=[PK(b)], writes=[("frs", r)])
                            P.op("act", lambda: nc.scalar.activation(rst[:, r, :], rst[:, r, :], AF.Exp, scale=-0.5),
                                 reads=[("frs", r)], writes=[("frs", r)])

                        def n_apply(kind, half, blk, r):
                            cs = slice(half * 1024 + blk * 512, half * 1024 + (blk + 1) * 512)
                            for c in range(8):
                                if kind == "pre":
                                    P.op("dve", lambda c=c: nc.vector.scalar_tensor_tensor(
                                        out=hT[:, 0, c, blk * 512:(blk + 1) * 512], in0=xT[:, c, cs], scalar=gpre(c), in1=rst[:, r, :],
                                        op0=ALU.mult, op1=ALU.mult),
                                        reads=[X(c, half * 2 + blk), ("frs", r), "small"], writes=[("fh", 0, c, blk)])
                                else:
                                    ti = c % 2
                                    P.op("dve", lambda c=c, ti=ti: nc.vector.scalar_tensor_tensor(
                                        out=tmp[:, ti, :], in0=yb[:, half, c, blk * 512:(blk + 1) * 512], scalar=gpost(c), in1=rst[:, r, :],
                                        op0=ALU.mult, op1=ALU.mult), reads=[("fy", half, c, blk), ("frs", r), "ghalf"], writes=[("ftmp", ti)])
                                    if c % 2:
                                        P.op("pool", lambda c=c, ti=ti: nc.gpsimd.tensor_tensor(xT[:, c, cs], xT[:, c, cs], tmp[:, ti, :], ALU.add),
                                             reads=[("ftmp", ti), X(c, half * 2 + blk)], writes=[X(c, half * 2 + blk)])
                                    else:
                                        P.op("dve", lambda c=c, ti=ti: nc.vector.tensor_tensor(xT[:, c, cs], xT[:, c, cs], tmp[:, ti, :], ALU.add),
                                             reads=[("ftmp", ti), X(c, half * 2 + blk)], writes=[X(c, half * 2 + blk)])

                        def load_group(gidx):
                            fg, slot = gidx % 11, gidx % 2
                            P.op("pool", lambda s, fg=fg, slot=slot: nc.gpsimd.dma_start(out=wgt[:, slot, :], in_=wg_d[ffn_i][l, fg]).then_inc(s, 16),
                                 writes=[("fwg", slot)], dma=1)
                            P.op("pool", lambda s, fg=fg, slot=slot: nc.gpsimd.dma_start(out=wut[:, slot, :], in_=wu_d[ffn_i][l, fg]).then_inc(s, 16),
                                 writes=[("fwu", slot)], dma=1)
                            P.op("pool", lambda s, fg=fg, slot=slot: nc.gpsimd.dma_start(out=wdt[:, slot, :], in_=wd_d[ffn_i][l, fg]).then_inc(s, 16),
                                 writes=[("fwd", slot)], dma=1)

                        def compute_group(gidx):
                            half, fg = divmod(gidx, 11)
                            slot = gidx % 2
                            for blk in range(2):
                                for j in range(2):
                                    bg = bank("gu", [0, 1, 2, 3])
                                    bu = bank("gu", [0, 1, 2, 3])
                                    for c in range(8):
                                        P.op("pe", lambda c=c, j=j, blk=blk, bg=bg: nc.tensor.matmul(
                                            ps[bg][:, :], wgt[:, slot, c * 256 + j * 128: c * 256 + (j + 1) * 128], hT[:, 0, c, blk * 512:(blk + 1) * 512],
                                            start=(c == 0), stop=(c == 7)), reads=[("fwg", slot), ("fh", 0, c, blk)], writes=[PK(bg)])
                                    for c in range(8):
                                        P.op("pe", lambda c=c, j=j, blk=blk, bu=bu: nc.tensor.matmul(
                                            ps[bu][:, :], wut[:, slot, c * 256 + j * 128: c * 256 + (j + 1) * 128], hT[:, 0, c, blk * 512:(blk + 1) * 512],
                                            start=(c == 0), stop=(c == 7)), reads=[("fwu", slot), ("fh", 0, c, blk)], writes=[PK(bu)])
                                    si = bank("sg", [0, 1])
                                    P.op("act", lambda bg=bg, si=si: nc.scalar.activation(sgt[:, si, :], ps[bg][:, :], AF.Silu),
                                         reads=[PK(bg)], writes=[("fsg", si)])
                                    P.op("dve", lambda bu=bu, si=si, j=j, blk=blk: nc.vector.tensor_tensor(
                                        At[:, slot, j, blk * 512:(blk + 1) * 512], ps[bu][:, :], sgt[:, si, :], ALU.mult),
                                        reads=[PK(bu), ("fsg", si)], writes=[("fA", slot, j, blk)])
                            for blk in range(2):
                                for dm in range(8):
                                    by = bank("yp", [4, 5, 6, 7])
                                    for j in range(2):
                                        P.op("pe", lambda j=j, dm=dm, blk=blk, by=by: nc.tensor.matmul(
                                            ps[by][:, :], wdt[:, slot, j * 1024 + dm * 128: j * 1024 + (dm + 1) * 128],
                                            At[:, slot, j, blk * 512:(blk + 1) * 512], start=(j == 0), stop=(j == 1)),
                                            reads=[("fwd", slot), ("fA", slot, j, blk)], writes=[PK(by)])
                                    ysl = yb[:, half, dm, blk * 512:(blk + 1) * 512]
                                    if fg == 0:
                                        P.op("act", lambda by=by, ysl=ysl: nc.scalar.copy(ysl, ps[by][:, :]),
                                             reads=[PK(by)], writes=[("fy", half, dm, blk)])
                                    else:
                                        P.op("dve", lambda by=by, ysl=ysl: nc.vector.tensor_tensor(ysl, ps[by][:, :], ysl, ALU.add),
                                             reads=[PK(by), ("fy", half, dm, blk)], writes=[("fy", half, dm, blk)])

                        for blk in range(2):
                            n_sq("pre", 0, blk)
                            n_stat(blk)
                            n_apply("pre", 0, blk, blk)
                        hooks = {8: [lambda: n_sq("pre", 1, 0)],
                                 9: [lambda: n_stat(0), lambda: n_sq("pre", 1, 1)],
                                 10: [lambda: n_stat(1)],
                                 12: [lambda: n_sq("post", 0, 0)],
                                 13: [lambda: n_stat(0), lambda: n_apply("post", 0, 0, 0), lambda: n_sq("post", 0, 1)],
                                 14: [lambda: n_stat(1), lambda: n_apply("post", 0, 1, 1)]}
                        load_group(0)
                        for gidx in range(22):
                            if gidx + 1 < 22:
                                load_group(gidx + 1)
                            if gidx == 11:
                                for blk in range(2):
                                    n_apply("pre", 1, blk, blk)
                            compute_group(gidx)
                            for hk in hooks.get(gidx, ()):
                                hk()
                        for blk in range(2):
                            n_sq("post", 1, blk)
                            n_stat(blk)
                            n_apply("post", 1, blk, blk)
                        P.flush()
                    if debug and sq == 0:
                        dump(l * 3 + (0 if ffn_i == 0 else 2), xT[:, :, :], [X(c, b) for c in range(8) for b in range(4)])
                        P.flush()
                    if ffn_i == 1 or stop == "ffn1":
                        continue
                    mixer(nc, P, top, sbt, ps, bank, PK, X, xT, small, cb, identf, lamt, l, sq,
                          winfm_d, winv_d, wo_d, qrows_d, krows_d, bind_d, rms_stats, dump if (debug and sq == 0) else None, stop, dump2)
                    if debug and sq == 0:
                        dump(l * 3 + 1, xT[:, :, :], [X(c, b) for c in range(8) for b in range(4)])
                        P.flush()

            with contextlib.ExitStack() as es:
                xo = sbt(es, "xo", [128, 2, D], F32)
                for tt in range(NT):
                    for c in range(8):
                        b = bank("tr", [0, 1, 2, 3])
                        P.op("pe", lambda tt=tt, c=c, b=b: nc.tensor.transpose(ps[b][:, 0:128], xT[:, c, tt * 128:(tt + 1) * 128], identf[:]),
                             reads=[X(c, tt // 4), "identf"], writes=[PK(b)])
                        if c % 2:
                            P.op("act", lambda tt=tt, c=c, b=b: nc.scalar.copy(xo[:, tt % 2, c * 128:(c + 1) * 128], ps[b][:, 0:128]),
                                 reads=[PK(b)], writes=[("xo", tt % 2, c)])
                        else:
                            P.op("dve", lambda tt=tt, c=c, b=b: nc.vector.tensor_copy(xo[:, tt % 2, c * 128:(c + 1) * 128], ps[b][:, 0:128]),
                                 reads=[PK(b)], writes=[("xo", tt % 2, c)])
                    P.op("sp", lambda s, tt=tt: nc.sync.dma_start(out=out_d[sq, tt * 128:(tt + 1) * 128, :], in_=xo[:, tt % 2, :]).then_inc(s, 16),
                         reads=[("xo", tt % 2, c) for c in range(8)], dma=1)
                P.flush(final=(sq == nseq - 1))
    return nc


def mixer(nc, P, top, sbt, ps, bank, PK, X, xT, small, cb, identf, lamt, l, sq,
          winfm_d, winv_d, wo_d, qrows_d, krows_d, bind_d, rms_stats, dump, stop=None, dump2=None):
    ident_bf = cb[:, CB_ID:CB_ID + 128]
    tri_bf = cb[:, CB_TRI:CB_TRI + 128]
    ones_bf = cb[:, CB_ONES:CB_ONES + 128]
    with contextlib.ExitStack() as mes:
        hT = sbt(mes, "m_hT", [128, 8, S], BF16)
        mixT = sbt(mes, "m_mix", [128, 8, S], BF16)
        wsl = sbt(mes, "m_wsl", [128, 3, 1024], BF16)
        wv = sbt(mes, "m_wv", [128, 8, 512], BF16)
        pt = sbt(mes, "m_pt", [128, 3, 512], BF16)
        sqt = sbt(mes, "m_sq", [128, 2, 512], BF16)
        rst = sbt(mes, "m_rs", [128, 512], F32)
        H = lambda c, blk: ("mh", c, blk)
        MX = lambda c, blk: ("mix", c, blk)
        gpre = lambda c: small[:, SM_G + (l * 6 + 2) * 8 + c: SM_G + (l * 6 + 2) * 8 + c + 1]
        gpost = lambda c: small[:, SM_G + (l * 6 + 3) * 8 + c: SM_G + (l * 6 + 3) * 8 + c + 1]

        for blk in range(NBLK):
            cs = slice(blk * 512, (blk + 1) * 512)
            rms_stats(mes, "m", lambda c, cs=cs: xT[:, c, cs], lambda c, blk=blk: X(c, blk), 8, 512, 1.0 / D, sqt, rst, [0, 1, 2, 3])
            for c in range(8):
                P.op("dve", lambda c=c, cs=cs: nc.vector.scalar_tensor_tensor(
                    out=hT[:, c, cs], in0=xT[:, c, cs], scalar=gpre(c), in1=rst[:, :], op0=ALU.mult, op1=ALU.mult),
                    reads=[X(c, blk), "mstat", "small"], writes=[H(c, blk)])

        def load_slab(u):
            si = bank("wsl", [0, 1, 2])
            P.op("pool", lambda s, u=u, si=si: nc.gpsimd.dma_start(out=wsl[:, si, :], in_=winfm_d[l, u]).then_inc(s, 16),
                 writes=[("wsl", si)], dma=1)
            return si

        def proj_fm(si, blk, b, M=128):
            for c in range(8):
                P.op("pe", lambda c=c: nc.tensor.matmul(ps[b][0:M, :], wsl[:, si, c * 128: c * 128 + M], hT[:, c, blk * 512:(blk + 1) * 512],
                                                        start=(c == 0), stop=(c == 7)),
                     reads=[("wsl", si), H(c, blk)], writes=[PK(b)])

        def proj_v(voff, ncol, dst_fn, es):
            for c in range(8):
                P.op("pool", lambda s, c=c: nc.gpsimd.dma_start(out=wv[:, c, 0:ncol], in_=winv_d[l, :, c * 1024 + voff: c * 1024 + voff + ncol]).then_inc(s, 16),
                     writes=[("wv", c)], dma=1)
            for tt in range(NT):
                b = bank("pj", [0, 1, 2, 3])
                for c in range(8):
                    P.op("pe", lambda c=c, tt=tt, b=b: nc.tensor.matmul(ps[b][:, 0:ncol], hT[:, c, tt * 128:(tt + 1) * 128], wv[:, c, 0:ncol],
                                                                      start=(c == 0), stop=(c == 7)),
                         reads=[("wv", c), H(c, tt // 4)], writes=[PK(b)])
                dst_fn(tt, b)

        def attn_pair(streams, pv_fn, final_fn, score_fn, look=2):
            steps = []
            for J in range(NBLK):
                ni = 4 * J + 4
                for i in range(ni):
                    c0 = max(0, i - 4 * J) * 128
                    for st in streams:
                        steps.append((st, i, J, c0, i == 0, i == ni - 1))
            ptis = []
            for k in range(len(steps)):
                while len(ptis) < min(len(steps), k + look + 1):
                    st, i, J, c0, f_, l_ = steps[len(ptis)]
                    ptis.append(score_fn(st, i, J, c0))
                st, i, J, c0, f_, l_ = steps[k]
                pv_fn(st, i, J, c0, ptis[k], f_, l_)
                if l_ and st == streams[-1]:
                    final_fn(J)

        def diag_mask(b, c0, i, J):
            if i >= 4 * J:
                P.op("pe", lambda: nc.tensor.matmul(ps[b][:, c0:c0 + 128], ident_bf, tri_bf, start=False, stop=True),
                     reads=["cb"], writes=[PK(b)])

        with contextlib.ExitStack() as es:
            Fs = sbt(es, "fs", [128, S], BF16)
            Qm = sbt(es, "qm", [128, 2, S], BF16)
            Km = sbt(es, "km", [128, 2, S], BF16)
            bcol = sbt(es, "bcol", [128, 68], F32)
            eM = sbt(es, "eM", [128, 4], F32)
            Mx = sbt(es, "Mx", [128, 2], F32)
            Mrep = sbt(es, "Mrep", [128, 128], F32)
            ges = contextlib.ExitStack()
            G0 = sbt(ges, "g0", [128, S + 3], F32)
            G1 = sbt(ges, "g1", [128, S + 3], F32)
            G2 = sbt(ges, "g2", [128, S + 3], F32)
            B0 = sbt(ges, "b0", [128, S], BF16)
            B1 = sbt(ges, "b1", [128, S], BF16)
            nbf = small[:, SM_BF + l:SM_BF + l + 1]
            sgf = load_slab(U_GF)
            for blk in range(NBLK):
                b = bank("pj", [0, 1, 2, 3])
                proj_fm(sgf, blk, b)
                P.op("dve", lambda: nc.vector.tensor_scalar(Mx[:, 1:2], nbf, -1.0, None, op0=ALU.mult), reads=["small"], writes=["negbf"])
                P.op("act", lambda b=b, blk=blk: nc.scalar.activation(G0[:, blk * 512:(blk + 1) * 512], ps[b][:, :], AF.Exp, bias=Mx[:, 1:2], scale=-1.0),
                     reads=[PK(b), "negbf"], writes=[("G0", blk)])
            P.op("act", lambda: nc.scalar.activation(G0[:, 0:S], G0[:, 0:S], AF.Ln, bias=1.0),
                 reads=[("G0", k) for k in range(4)], writes=[("G0", k) for k in range(4)])
            P.op("dve", lambda: nc.vector.memset(G2[:, 0:S], 1.0), writes=["G2"])
            P.op("dve", lambda: nc.vector.tensor_tensor_scan(G1[:, 0:S], G2[:, 0:S], G0[:, 0:S], 0.0, ALU.mult, ALU.add),
                 reads=["G2"] + [("G0", k) for k in range(4)], writes=["G1"])
            sgi = load_slab(U_GI)
            bi = small[:, SM_BI + l:SM_BI + l + 1]
            for blk in range(NBLK):
                b = bank("pj", [0, 1, 2, 3])
                proj_fm(sgi, blk, b)
                P.op("act", lambda b=b, blk=blk: nc.scalar.activation(G0[:, blk * 512:(blk + 1) * 512], ps[b][:, :], AF.Identity, bias=bi, scale=1.0),
                     reads=[PK(b), "small", "G1"], writes=[("G0", blk)])
            P.op("dve", lambda: nc.vector.reduce_max(out=Mx[:, 0:1], in_=G0[:, 0:S], axis=AX.X),
                 reads=[("G0", k) for k in range(4)], writes=["Mx"])
            P.op("dve", lambda: nc.vector.scalar_tensor_tensor(out=G2[:, 0:S], in0=G0[:, 0:S], scalar=Mx[:, 0:1], in1=G1[:, 0:S],
                                                               op0=ALU.subtract, op1=ALU.add),
                 reads=[("G0", k) for k in range(4)] + ["Mx", "G1", "G2"], writes=["G2"])
            sel4 = small[:, SM_SEL4:SM_SEL4 + 4]
            bb = bank("pj", [0, 1, 2, 3])
            for tt in range(NT):
                P.op("pe", lambda tt=tt: nc.tensor.matmul(ps[bb][:, tt * 4:tt * 4 + 4], G2[:, tt * 128:(tt + 1) * 128], sel4, start=True, stop=True),
                     reads=["G2", "small"], writes=[PK(bb)])
            P.op("dve", lambda: nc.vector.memset(Mrep[:], 0.0), writes=["Mrep"])
            P.op("dve", lambda: nc.vector.tensor_scalar(Mrep[:], Mrep[:], Mx[:, 0:1], None, op0=ALU.add), reads=["Mrep", "Mx"], writes=["Mrep"])
            P.op("pe", lambda: nc.tensor.matmul(ps[bb][:, 64:68], Mrep[:], sel4, start=True, stop=True), reads=["Mrep", "small"], writes=[PK(bb)])
            P.op("dve", lambda: nc.vector.tensor_copy(bcol[:, 0:68], ps[bb][:, 0:68]), reads=[PK(bb)], writes=["bcol"])
            P.op("act", lambda: nc.scalar.activation(eM[:, 0:4], bcol[:, 64:68], AF.Exp, scale=-1.0), reads=["bcol"], writes=["eM"])
            c1 = small[:, SM_C123:SM_C123 + 1]
            c2 = small[:, SM_C123 + 1:SM_C123 + 2]
            c3 = small[:, SM_C123 + 2:SM_C123 + 3]
            P.op("dve", lambda: nc.vector.tensor_scalar(B0[:], G1[:, 0:S], -1.0, None, op0=ALU.mult), reads=["G1"], writes=["B0"])
            P.op("dve", lambda: nc.vector.scalar_tensor_tensor(out=G0[:, 0:S], in0=G1[:, 0:S], scalar=-1.0, in1=B0[:], op0=ALU.mult, op1=ALU.subtract),
                 reads=["G1", "B0", "G2"] + [("G0", k) for k in range(4)], writes=[("G0", k) for k in range(4)])
            P.op("dve", lambda: nc.vector.tensor_copy(B1[:], G0[:, 0:S]), reads=[("G0", k) for k in range(4)], writes=["B1"])
            P.op("dve", lambda: nc.vector.tensor_scalar(Fs[:], B0[:], c1, None, op0=ALU.mult), reads=["B0", "small"], writes=["Fs"])
            P.op("dve", lambda: nc.vector.scalar_tensor_tensor(out=Fs[:], in0=B1[:], scalar=c2, in1=Fs[:], op0=ALU.mult, op1=ALU.add),
                 reads=["B1", "Fs", "small"], writes=["Fs"])
            P.op("dve", lambda: nc.vector.tensor_tensor(G0[:, 0:S], G0[:, 0:S], B1[:], ALU.subtract),
                 reads=[("G0", k) for k in range(4)] + ["B1"], writes=[("G0", k) for k in range(4)])
            P.op("dve", lambda: nc.vector.tensor_copy(B0[:], G0[:, 0:S]), reads=[("G0", k) for k in range(4)] + ["Fs"], writes=["B0"])
            P.op("dve", lambda: nc.vector.scalar_tensor_tensor(out=Fs[:], in0=B0[:], scalar=c3, in1=Fs[:], op0=ALU.mult, op1=ALU.add),
                 reads=["B0", "Fs", "small"], writes=["Fs"])
            P.op("dve", lambda: nc.vector.memset(G0[:, 0:3], 0.0), reads=["B0"] + [("G0", k) for k in range(4)], writes=["G0pad"])
            for cc in range(4):
                su = load_slab((U_MQ if cc < 2 else U_MK) + cc % 2)
                for blk in range(NBLK):
                    b = bank("pj", [0, 1, 2, 3])
                    proj_fm(su, blk, b)
                    P.op("act", lambda b=b, blk=blk: nc.scalar.copy(G0[:, 3 + blk * 512: 3 + (blk + 1) * 512], ps[b][:, :]),
                         reads=[PK(b), "G0pad"], writes=[("G0", blk)])
                wc = lambda j, cc=cc: small[:, SM_CONV + (l * 4 + cc) * 4 + j: SM_CONV + (l * 4 + cc) * 4 + j + 1]
                allg0 = [("G0", k) for k in range(4)] + ["G0pad"]
                P.op("dve", lambda wc=wc: nc.vector.tensor_scalar(G2[:, 0:S], G0[:, 0:S], wc(0), None, op0=ALU.mult),
                     reads=allg0 + ["small", "G2"], writes=["G2"])
                for j in (1, 2, 3):
                    P.op("dve", lambda wc=wc, j=j: nc.vector.scalar_tensor_tensor(out=G2[:, 0:S], in0=G0[:, j:j + S], scalar=wc(j), in1=G2[:, 0:S],
                                                                                  op0=ALU.mult, op1=ALU.add),
                         reads=allg0 + ["small", "G2"], writes=["G2"])
                dstt = Qm if cc < 2 else Km
                P.op("act", lambda dstt=dstt, cc=cc: nc.scalar.activation(dstt[:, cc % 2, :], G2[:, 0:S], AF.Silu),
                     reads=["G2"], writes=[("qk", cc)])
            P.flush()
            if dump is not None:
                dump2(3, 0, Fs[:, :], S)
                dump2(3, 1, G1[:, 0:S], S)
                dump2(3, 2, bcol[:, 0:68], 68)
                dump2(3, 3, eM[:, 0:4], 4)
                dump2(4, 0, Qm[:, 0, :], S)
                dump2(4, 1, Qm[:, 1, :], S)
                dump2(4, 2, Km[:, 0, :], S)
                dump2(4, 3, Km[:, 1, :], S)
                P.flush()
            ges.close()
            if stop == "mlstm_prep":
                return
            Vb = sbt(es, "m_V", [128, NT, 512], BF16)
            wt = sbt(es, "wt", [128, 3, 512], F32)
            ft = sbt(es, "ft", [128, 4, 512], F32)
            P.op("dve", lambda: nc.vector.memset(Vb[:, :, :].rearrange("p t (q c) -> p t q c", q=2)[:, :, :, 64:192], 1.0),
                 writes=[("V", tt) for tt in range(NT)])
            def vdst(tt, b):
                src = ps[b][:, 0:256].rearrange("p (q s e) -> p q s e", q=2, s=2)
                dst = Vb[:, tt, :].rearrange("p (q c) -> p q c", q=2)
                P.op("act", lambda src=src, dst=dst: nc.scalar.copy(dst[:, :, 0:64], src[:, :, 0, :]), reads=[PK(b)], writes=[("V", tt)])
                P.op("act", lambda src=src, dst=dst: nc.scalar.copy(dst[:, :, 192:256], src[:, :, 1, :]), reads=[PK(b), ("V", tt)], writes=[("V", tt)])
            proj_v(0, 256, vdst, es)
            if dump is not None:
                P.flush()
                for tt in range(4):
                    dump2(5, 0, Vb[:, tt, :], 512) if tt == 0 else dump2(5, tt, Vb[:, tt, :], 512)
                P.flush()
            for pr in range(2):
                def score(st, i, J, c0, pr=pr):
                    h = 2 * pr + st
                    rows = slice(64 * st, 64 * st + 64)
                    be = bank("scm", [0, 1, 2, 3, 6, 7])
                    bs_ = bank("scm", [0, 1, 2, 3, 6, 7])
                    P.op("pe", lambda: nc.tensor.matmul(ps[be][:, c0:512], cb[:, CB_ROWSEL + h * 128: CB_ROWSEL + (h + 1) * 128],
                                                        Fs[:, J * 512 + c0:(J + 1) * 512], start=True, stop=(i < 4 * J)),
                         reads=["cb", "Fs"], writes=[PK(be)])
                    diag_mask(be, c0, i, J)
                    wi = bank("wt", [0, 1, 2])
                    P.op("act", lambda: nc.scalar.activation(wt[:, wi, c0:512], ps[be][:, c0:512], AF.Exp, bias=bcol[:, i * 4 + h:i * 4 + h + 1], scale=1.0),
                         reads=[PK(be), "bcol"], writes=[("wt", wi)])
                    P.op("pe", lambda: nc.tensor.matmul(ps[bs_][:, c0:512], Km[rows, pr, i * 128:(i + 1) * 128], Qm[rows, pr, J * 512 + c0:(J + 1) * 512],
                                                        start=True, stop=True),
                         reads=[("qk", 2 + pr), ("qk", pr)], writes=[PK(bs_)])
                    pti = bank("pt", [0, 1, 2])
                    P.op("dve", lambda: nc.vector.scalar_tensor_tensor(out=pt[:, pti, c0:512], in0=ps[bs_][:, c0:512], scalar=0.125, in1=wt[:, wi, c0:512],
                                                                       op0=ALU.mult, op1=ALU.mult),
                         reads=[PK(bs_), ("wt", wi)], writes=[("pt", pti)])
                    return pti

                def pv(st, i, J, c0, pti, first, last, pr=pr):
                    h = 2 * pr + st
                    P.op("pe", lambda: nc.tensor.matmul(ps[4 + st][:, c0:512], Vb[:, i, h * 128:(h + 1) * 128], pt[:, pti, c0:512], start=first, stop=last),
                         reads=[("V", i), ("pt", pti)], writes=[PK(4 + st)])

                smo = load_slab(U_MO + pr)

                def final(J, pr=pr, smo=smo):
                    bo = bank("scm", [0, 1, 2, 3, 6, 7])
                    proj_fm(smo, J, bo)
                    P.op("act", lambda: nc.scalar.activation(ft[:, 0, :], ps[bo][:, :], AF.Exp, scale=-1.0), reads=[PK(bo), "ft0"], writes=["ft0"])
                    P.op("act", lambda: nc.scalar.activation(ft[:, 0, :], ft[:, 0, :], AF.Ln, bias=1.0), reads=["ft0"], writes=["ft0"])
                    P.op("act", lambda: nc.scalar.activation(ft[:, 0, :], ft[:, 0, :], AF.Exp, scale=-1.0), reads=["ft0"], writes=["ft0"])
                    for st in range(2):
                        h = 2 * pr + st
                        o_ps = ps[4 + st]
                        wr = slice(64 * st, 64 * st + 64)
                        dr = slice(64 * (1 - st), 64 * (1 - st) + 64)
                        k1, k2 = ("ft1", st), ("ft2", st)
                        P.op("act", lambda o_ps=o_ps, wr=wr, dr=dr: nc.scalar.activation(ft[wr, 1, :], o_ps[dr, :], AF.Abs), reads=[PK(4 + st), k1], writes=[k1])
                        P.op("dve", lambda h=h, wr=wr: nc.vector.tensor_scalar(ft[wr, 1, :], ft[wr, 1, :], eM[wr, h:h + 1], None, op0=ALU.max),
                             reads=[k1, "eM"], writes=[k1])
                        P.op("act", lambda wr=wr: nc.scalar.activation(ft[wr, 1, :], ft[wr, 1, :], AF.Ln), reads=[k1], writes=[k1])
                        P.op("act", lambda wr=wr: nc.scalar.activation(ft[wr, 1, :], ft[wr, 1, :], AF.Exp, scale=-1.0), reads=[k1], writes=[k1])
                        P.op("dve", lambda o_ps=o_ps, wr=wr: nc.vector.tensor_tensor(ft[wr, 2, :], o_ps[wr, :], ft[wr, 1, :], ALU.mult),
                             reads=[PK(4 + st), k1, k2], writes=[k2])
                        P.op("dve", lambda wr=wr: nc.vector.tensor_tensor(mixT[wr, pr, J * 512:(J + 1) * 512], ft[wr, 2, :], ft[wr, 0, :], ALU.mult),
                             reads=[k2, "ft0"], writes=[("mixm%d" % st, pr, J)])

                attn_pair([0, 1], pv, final, score)
            P.flush()
            if dump is not None:
                for k in range(4):
                    dump2(5, 4 + k, ft[:, k, :], 512)
                dump2(3, 4, wt[:, 0, :], 512)
                dump2(3, 5, pt[:, 0, :], 512)
                P.flush()
        if dump is not None:
            dump(6, mixT[:, :, :], [])
            P.flush()
        if stop == "mlstm":
            return

        with contextlib.ExitStack() as es:
            Vb = sbt(es, "s_V", [128, NT, 512], BF16)
            P.op("dve", lambda: nc.vector.memset(Vb[:, :, :].rearrange("p t (h e) -> p t h e", h=4)[:, :, :, 64:128], 1.0),
                 writes=[("V", tt) for tt in range(NT)])
            Qa = [sbt(es, "qa%d" % i, [128, S], BF16) for i in range(2)]
            Ka = [sbt(es, "ka%d" % i, [128, S], BF16) for i in range(2)]
            ksq = sbt(es, "ksq", [128, 4, 512], BF16)
            kst = sbt(es, "kst", [128, 16], F32)
            kmf = sbt(es, "kmf", [128, 8], F32)
            kmb = [sbt(es, "kmb%d" % i, [64, 8], BF16) for i in range(2)]
            gw = sbt(es, "gw", [128, 64], F32)
            top8 = sbt(es, "top8", [128, 64], F32)
            selb = sbt(es, "selb", [128, 64], F32)
            ft = sbt(es, "sft", [128, 6, 512], F32)
            for i in range(2):
                P.op("dve", lambda i=i: nc.vector.memset(Qa[i][64:128, :], 0.0), writes=[("Qa", i, k) for k in range(4)] + [("Qrow", i)])
                P.op("dve", lambda i=i: nc.vector.memset(Ka[i][64:128, :], 0.0), writes=[("Ka", i)])
                P.op("pool", lambda s, i=i: nc.gpsimd.dma_start(out=Qa[i][72:76, :], in_=qrows_d).then_inc(s, 16),
                     reads=[("Qrow", i)], writes=[("Qrow", i)] + [("Qa", i, k) for k in range(4)], dma=1)
                P.op("dve", lambda i=i: nc.vector.memset(Ka[i][96:97, :], 1.0), reads=[("Ka", i)], writes=[("Ka", i)])

            def softmax_pair(uq, uk, slope_idx, moba, vcol_fn, pv_m, nacc, clear_sel=False):
                for st in range(2):
                    P.op("pool", lambda s, st=st: nc.gpsimd.dma_start(out=Ka[st][72:76, :], in_=krows_d[slope_idx[st]]).then_inc(s, 16),
                         reads=[("Ka", st)], writes=[("Ka", st)], dma=1)
                    if moba:
                        P.op("pool", lambda s, st=st: nc.gpsimd.dma_start(out=Ka[st][64:72, :], in_=bind_d).then_inc(s, 16),
                             reads=[("Ka", st)], writes=[("Ka", st)], dma=1)
                    elif clear_sel:
                        P.op("dve", lambda st=st: nc.vector.memset(Ka[st][64:72, :], 0.0), reads=[("Ka", st)], writes=[("Ka", st)])
                        P.op("dve", lambda st=st: nc.vector.memset(Qa[st][64:72, :], 0.0), reads=[("Qa", st, k) for k in range(4)],
                             writes=[("Qa", st, k) for k in range(4)])
                sk = load_slab(uk)
                sq_ = load_slab(uq)
                for blk in range(NBLK):
                    b = bank("pj", [0, 1, 2, 3])
                    proj_fm(sk, blk, b)
                    cs = slice(blk * 512, (blk + 1) * 512)
                    P.op("dve", lambda b=b, cs=cs: nc.vector.tensor_copy(Ka[0][0:64, cs], ps[b][0:64, :]), reads=[PK(b), ("Ka", 0)], writes=[("Ka", 0)])
                    P.op("dve", lambda b=b, cs=cs: nc.vector.tensor_copy(Ka[1][0:64, cs], ps[b][64:128, :]), reads=[PK(b), ("Ka", 1)], writes=[("Ka", 1)])
                    P.op("act", lambda b=b, blk=blk: nc.scalar.activation(ksq[:, blk, :], ps[b][:, :], AF.Square), reads=[PK(b)], writes=[("ksq", blk)])
                    if moba:
                        P.op("dve", lambda b=b, blk=blk: nc.vector.reduce_sum(out=kmf[:, 2 * blk:2 * blk + 2],
                                                                             in_=ps[b][:, :].rearrange("p (n k) -> p n k", n=2), axis=AX.X),
                             reads=[PK(b), "kmf"], writes=["kmf"])
                qbanks = []
                for blk in range(NBLK):
                    b = bank("pj", [0, 1, 2, 3])
                    qbanks.append(b)
                    proj_fm(sq_, blk, b)
                    cs = slice(blk * 512, (blk + 1) * 512)
                    P.op("act", lambda b=b, cs=cs: nc.scalar.activation(Qa[0][0:64, cs], ps[b][0:64, :], AF.Copy, scale=0.125),
                         reads=[PK(b), ("Qa", 0, blk)], writes=[("Qa", 0, blk)])
                    P.op("act", lambda b=b, cs=cs: nc.scalar.activation(Qa[1][0:64, cs], ps[b][64:128, :], AF.Copy, scale=0.125),
                         reads=[PK(b), ("Qa", 1, blk)], writes=[("Qa", 1, blk)])
                for blk in range(NBLK):
                    for st in range(2):
                        bn = bank("fin4", [4, 5, 6, 7])
                        rows = slice(64 * st, 64 * st + 64)
                        P.op("pe", lambda rows=rows, blk=blk, bn=bn: nc.tensor.matmul(ps[bn][0:1, :], cb[rows, CB_ONES:CB_ONES + 1], ksq[rows, blk, :],
                                                                                   start=True, stop=True),
                             reads=[("ksq", blk), "cb"], writes=[PK(bn)])
                        P.op("dve", lambda bn=bn, st=st, blk=blk: nc.vector.reduce_max(out=kst[0:1, st * 4 + blk: st * 4 + blk + 1], in_=ps[bn][0:1, :], axis=AX.X),
                             reads=[PK(bn), "kst"], writes=["kst"])
                for st in range(2):
                    P.op("dve", lambda st=st: nc.vector.reduce_max(out=kst[0:1, 8 + st:9 + st], in_=kst[0:1, st * 4:st * 4 + 4], axis=AX.X),
                         reads=["kst"], writes=["kst"])
                    P.op("dve", lambda st=st: nc.vector.tensor_scalar(kst[0:1, 10 + st:11 + st], kst[0:1, 8 + st:9 + st], -1.0 / 16, None, op0=ALU.mult),
                         reads=["kst"], writes=["kst"])
                if moba:
                    P.op("act", lambda: nc.scalar.activation(kmb[0][0:64, :], kmf[0:64, :], AF.Copy, scale=1.0 / 256), reads=["kmf"], writes=["kmb0"])
                    P.op("act", lambda: nc.scalar.activation(kmb[1][0:64, :], kmf[64:128, :], AF.Copy, scale=1.0 / 256), reads=["kmf"], writes=["kmb1"])
                for blk in range(NBLK):
                    b = qbanks[blk]
                    cs = slice(blk * 512, (blk + 1) * 512)
                    P.op("act", lambda b=b, blk=blk: nc.scalar.activation(ksq[:, blk, :], ps[b][:, :], AF.Square), reads=[PK(b)], writes=[("ksq", blk)])
                    for st in range(2):
                        bn = bank("fin4", [4, 5, 6, 7])
                        rows = slice(64 * st, 64 * st + 64)
                        P.op("pe", lambda rows=rows, blk=blk, bn=bn: nc.tensor.matmul(ps[bn][0:1, :], cb[rows, CB_ONES:CB_ONES + 1], ksq[rows, blk, :],
                                                                                   start=True, stop=True),
                             reads=[("ksq", blk), "cb"], writes=[PK(bn)])
                        P.op("act", lambda bn=bn, st=st, cs=cs: nc.scalar.activation(Qa[st][96:97, cs], ps[bn][0:1, :], AF.Identity,
                                                                                  bias=kst[0:1, 10 + st:11 + st], scale=-1.0 / 16),
                             reads=[PK(bn), "kst", ("Qa", st, blk)], writes=[("Qa", st, blk)])
                if moba:
                    for st in range(2):
                        bg = bank("fin", [6, 7])
                        for k in range(8):
                            qt = 8 + k
                            P.op("pe", lambda st=st, qt=qt, k=k, bg=bg: nc.tensor.matmul(ps[bg][:, k * 8:k * 8 + 8], Qa[st][0:64, qt * 128:(qt + 1) * 128],
                                                                                      kmb[st][0:64, :], start=True, stop=True),
                                 reads=[("Qa", st, qt // 4), "kmb%d" % st], writes=[PK(bg)])
                        P.op("dve", lambda: nc.vector.memset(gw[:], -1e30), reads=["gw"], writes=["gw"])
                        P.op("dve", lambda: nc.vector.memset(selb[:], 0.0), reads=["selb"], writes=["selb"])
                        for k in range(8):
                            j = (8 + k) // 2
                            P.op("dve", lambda bg=bg, j=j, k=k: nc.vector.tensor_copy(gw[:, k * 8:k * 8 + j], ps[bg][:, k * 8:k * 8 + j]),
                                 reads=[PK(bg), "gw"], writes=[("gw", k)])
                        for k in range(8):
                            P.op("dve", lambda k=k: nc.vector.max(out=top8[:, k * 8:k * 8 + 8], in_=gw[:, k * 8:k * 8 + 8]),
                                 reads=[("gw", k), "gw", "top8"], writes=[("top8", k)])
                        for k in range(8):
                            j = (8 + k) // 2
                            P.op("dve", lambda j=j, k=k: nc.vector.tensor_scalar(selb[:, k * 8:k * 8 + j], gw[:, k * 8:k * 8 + j], top8[:, k * 8 + 2:k * 8 + 3], 1.0,
                                                                                 op0=ALU.is_ge, op1=ALU.subtract),
                                 reads=[("gw", k), ("top8", k), "selb"], writes=[("selb", k)])
                        for hb in range(2):
                            bt = bank("pj", [0, 1, 2, 3])
                            for kk in range(4):
                                k = hb * 4 + kk
                                P.op("pe", lambda bt=bt, k=k, kk=kk: nc.tensor.transpose(ps[bt][0:8, kk * 128:(kk + 1) * 128], selb[:, k * 8:k * 8 + 8], identf[:]),
                                     reads=[("selb", k), "selb", "identf"], writes=[PK(bt)])
                            P.op("act", lambda st=st, hb=hb, bt=bt: nc.scalar.activation(Qa[st][64:72, 1024 + hb * 512:1024 + (hb + 1) * 512], ps[bt][0:8, :],
                                                                                     AF.Copy, scale=-NEG),
                                 reads=[PK(bt), ("Qa", st, 2 + hb)], writes=[("Qa", st, 2 + hb)])
                        P.op("dve", lambda: nc.vector.memset(top8[:, 0:1], 0.0), reads=[("gw", k) for k in range(8)] + [("top8", k) for k in range(8)] + [("selb", k) for k in range(8)],
                             writes=["gw", "top8", "selb"])

                def score(st, i, J, c0):
                    b = bank("sc", [0, 1, 2, 3])
                    P.op("pe", lambda: nc.tensor.matmul(ps[b][:, c0:512], Ka[st][0:97, i * 128:(i + 1) * 128], Qa[st][0:97, J * 512 + c0:(J + 1) * 512],
                                                        start=True, stop=(i < 4 * J)),
                         reads=[("Ka", st), ("Qa", st, J)], writes=[PK(b)])
                    diag_mask(b, c0, i, J)
                    pti = bank("pt", [0, 1, 2])
                    P.op("act", lambda: nc.scalar.activation(pt[:, pti, c0:512], ps[b][:, c0:512], AF.Exp), reads=[PK(b)], writes=[("pt", pti)])
                    return pti

                def pv(st, i, J, c0, pti, first, last):
                    for a in range(nacc):
                        ba = 4 + st * nacc + a
                        lhs = vcol_fn(st, i) if a == 0 else ones_bf
                        P.op("pe", lambda ba=ba, lhs=lhs: nc.tensor.matmul(ps[ba][:, c0:512], lhs, pt[:, pti, c0:512], start=first, stop=last),
                             reads=[("V", i), ("pt", pti), "cb"], writes=[PK(ba)])

                attn_pair([0, 1], pv, pv_m, score)

            def vdst_a(tt, b):
                P.op("act", lambda tt=tt, b=b: nc.scalar.copy(
                    Vb[:, tt, :].rearrange("p (h e) -> p h e", h=4)[:, :, 0:64], ps[b][:, 0:256].rearrange("p (h e) -> p h e", h=4)),
                    reads=[PK(b)], writes=[("V", tt)])
            proj_v(256, 256, vdst_a, es)
            for pr in range(2):
                def fin_moba(J, pr=pr):
                    for st in range(2):
                        o_ps = ps[4 + st]
                        P.op("act", lambda o_ps=o_ps: nc.scalar.activation(ft[0:64, 0, :], o_ps[64:128, :], AF.Ln), reads=[PK(4 + st), "sft0"], writes=["sft0"])
                        P.op("act", lambda: nc.scalar.activation(ft[0:64, 0, :], ft[0:64, 0, :], AF.Exp, scale=-1.0), reads=["sft0"], writes=["sft0"])
                        P.op("dve", lambda o_ps=o_ps, st=st: nc.vector.tensor_tensor(mixT[64 * st:64 * st + 64, 2 + pr, J * 512:(J + 1) * 512],
                                                                                     o_ps[0:64, :], ft[0:64, 0, :], ALU.mult),
                             reads=[PK(4 + st), "sft0"], writes=[("mixm", st, pr, J)])
                softmax_pair(U_AQ + pr, U_AK + pr, [2 * pr, 2 * pr + 1], True,
                             lambda st, i, pr=pr: Vb[:, i, (2 * pr + st) * 128:(2 * pr + st + 1) * 128], fin_moba, 1)
            if stop == "moba":
                P.flush()
                if dump is not None:
                    dump(7, mixT[:, :, :], [])
                    P.flush()
                return
            def vdst_d(tt, b):
                P.op("act", lambda tt=tt, b=b: nc.scalar.copy(Vb[:, tt, :], ps[b][:, :]), reads=[PK(b)], writes=[("V", tt)])
            proj_v(512, 512, vdst_d, es)
            neglam = lamt[:, 8 * l:8 * l + 1]
            gsub = lamt[:, 8 * l + 1:8 * l + 2]
            for h in range(4):
                def fin_diff(J, h=h):
                    P.op("act", lambda: nc.scalar.activation(ft[:, 0, :], ps[5][:, :], AF.Ln), reads=[PK(5), "sft0"], writes=["sft0"])
                    P.op("act", lambda: nc.scalar.activation(ft[:, 1, :], ps[7][:, :], AF.Ln), reads=[PK(7), "sft1"], writes=["sft1"])
                    P.op("act", lambda: nc.scalar.activation(ft[:, 0, :], ft[:, 0, :], AF.Exp, scale=-1.0), reads=["sft0"], writes=["sft0"])
                    P.op("act", lambda: nc.scalar.activation(ft[:, 1, :], ft[:, 1, :], AF.Exp, scale=-1.0), reads=["sft1"], writes=["sft1"])
                    P.op("dve", lambda: nc.vector.tensor_tensor(ft[:, 2, :], ps[4][:, :], ft[:, 0, :], ALU.mult), reads=[PK(4), "sft0", "sft2"], writes=["sft2"])
                    P.op("dve", lambda: nc.vector.tensor_tensor(ft[:, 3, :], ps[6][:, :], ft[:, 1, :], ALU.mult), reads=[PK(6), "sft1", "sft3"], writes=["sft3"])
                    P.op("dve", lambda: nc.vector.scalar_tensor_tensor(out=ft[:, 4, :], in0=ft[:, 3, :], scalar=neglam, in1=ft[:, 2, :], op0=ALU.mult, op1=ALU.add),
                         reads=["sft2", "sft3", "lamt", "sft4"], writes=["sft4"])
                    qi = bank("ksq", [0, 1])
                    P.op("act", lambda qi=qi: nc.scalar.activation(ksq[:, qi, :], ft[:, 4, :], AF.Square), reads=["sft4"], writes=[("ksq", qi)])
                    bn = bank("sc", [0, 1, 2, 3])
                    P.op("pe", lambda qi=qi, bn=bn: nc.tensor.matmul(ps[bn][:, :], ones_bf, ksq[:, qi, :], start=True, stop=True),
                         reads=[("ksq", qi), "cb"], writes=[PK(bn)])
                    P.op("act", lambda bn=bn: nc.scalar.activation(ft[:, 5, :], ps[bn][:, :], AF.Ln, bias=EPS, scale=1.0 / 128), reads=[PK(bn), "sft5"], writes=["sft5"])
                    P.op("act", lambda: nc.scalar.activation(ft[:, 5, :], ft[:, 5, :], AF.Exp, scale=-0.5), reads=["sft5"], writes=["sft5"])
                    P.op("dve", lambda: nc.vector.scalar_tensor_tensor(out=mixT[:, 4 + h, J * 512:(J + 1) * 512], in0=ft[:, 4, :], scalar=gsub, in1=ft[:, 5, :],
                                                                       op0=ALU.mult, op1=ALU.mult),
                         reads=["sft4", "sft5", "lamt"], writes=[("mixd", h, J)])
                softmax_pair(U_DQ + h, U_DK + h, [4 + h, 4 + h], False,
                             lambda st, i, h=h: Vb[:, i, h * 128:(h + 1) * 128], fin_diff, 2, clear_sel=(h == 0))
            P.flush()
        if dump is not None:
            dump(7, mixT[:, :, :], [])
            P.flush()
        if stop == "diff":
            return

        with contextlib.ExitStack() as es:
            wo = sbt(es, "wo", [128, 8, 1024], BF16)
            yb = sbt(es, "o_y", [128, 2, 8, 512], F32)
            tmp = sbt(es, "o_tmp", [128, 2, 512], F32)
            for dm in range(8):
                P.op("pool", lambda s, dm=dm: nc.gpsimd.dma_start(out=wo[:, dm, :], in_=wo_d[l, dm]).then_inc(s, 16), writes=[("wo", dm)], dma=1)

            def o_proj(blk):
                cs = slice(blk * 512, (blk + 1) * 512)
                ys = blk % 2
                for dm in range(8):
                    b = bank("yp", [4, 5, 6, 7])
                    for c in range(8):
                        P.op("pe", lambda c=c, dm=dm, b=b: nc.tensor.matmul(ps[b][:, :], wo[:, dm, c * 128:(c + 1) * 128], mixT[:, c, cs],
                                                                         start=(c == 0), stop=(c == 7)),
                             reads=[("wo", dm)], writes=[PK(b)])
                    if dm % 2:
                        P.op("act", lambda dm=dm, b=b: nc.scalar.copy(yb[:, ys, dm, :], ps[b][:, :]), reads=[PK(b)], writes=[("oy", ys, dm)])
                    else:
                        P.op("dve", lambda dm=dm, b=b: nc.vector.tensor_copy(yb[:, ys, dm, :], ps[b][:, :]), reads=[PK(b)], writes=[("oy", ys, dm)])

            def o_norm(blk):
                cs = slice(blk * 512, (blk + 1) * 512)
                ys = blk % 2
                rms_stats(es, "m", lambda c: yb[:, ys, c, :], lambda c: ("oy", ys, c), 8, 512, 1.0 / D, sqt, rst, [0, 1, 2, 3])
                for c in range(8):
                    ti = c % 2
                    P.op("dve", lambda c=c, ti=ti: nc.vector.scalar_tensor_tensor(out=tmp[:, ti, :], in0=yb[:, ys, c, :], scalar=gpost(c), in1=rst[:, :],
                                                                                  op0=ALU.mult, op1=ALU.mult),
                         reads=[("oy", ys, c), "mstat", "small"], writes=[("otmp", ti)])
                    P.op("pool", lambda c=c, ti=ti: nc.gpsimd.tensor_tensor(xT[:, c, cs], xT[:, c, cs], tmp[:, ti, :], ALU.add),
                         reads=[("otmp", ti), X(c, blk)], writes=[X(c, blk)])

            o_proj(0)
            for blk in range(NBLK):
                if blk + 1 < NBLK:
                    o_proj(blk + 1)
                o_norm(blk)
            P.flush()


_CACHE = {}


def _prep_inputs(inputs):
    small, cbt, qrows, krows, blockind, identf = _host_tables(inputs)
    w = _host_weights(inputs)
    shared = {"small": small, "cb": cbt, "qrows": qrows, "krows": krows, "blockind": blockind, "identf": identf}
    shared.update(w)
    return shared


def kernel(**inputs):
    x = np.ascontiguousarray(np.asarray(inputs["x"], np.float32))
    shared = _prep_inputs(inputs)
    if "nc" not in _CACHE:
        _CACHE["nc"] = build_program()
    nc = _CACHE["nc"]
    in_maps = []
    for c in range(NCORES):
        m = {"x": x[c * SEQ_PER_CORE:(c + 1) * SEQ_PER_CORE]}
        m.update(shared)
        in_maps.append(m)
    res = run_bass_kernel_spmd(nc, in_maps, core_ids=list(range(NCORES)))
    out = np.concatenate([np.asarray(r["out"], np.float32) for r in res.results], axis=0)
    return out
```

```python
import contextlib
import math
import numpy as np
import concourse.bass as bass
import concourse.mybir as mybir
from concourse.bass_utils import run_bass_kernel_spmd

F32 = mybir.dt.float32
BF16 = mybir.dt.bfloat16
AF = mybir.ActivationFunctionType
ALU = mybir.AluOpType
AX = mybir.AxisListType

NCORES = 8
SEQ_PER_CORE = 2
S = 2048
D = 1024
DFF = 2816
L = 2
NBLK = S // 512
NT = S // 128
NEG = -30000.0
EPS = 1e-6
COMPUTE = ("pe", "act", "dve", "pool")


class _Op:
    __slots__ = ("eng", "fn", "waits", "signal", "pos", "dma", "dsem", "dval", "sigval", "uid")
    _n = 0

    def __init__(self, eng, fn, dma):
        _Op._n += 1
        self.uid = _Op._n
        self.eng = eng
        self.fn = fn
        self.waits = []
        self.signal = False
        self.pos = -1
        self.dma = dma
        self.dsem = None
        self.dval = 0
        self.sigval = None


class Prog:
    def __init__(self, nc, es, n_dma_sems=24):
        self.nc = nc
        self.ops = []
        self.state = {}
        self.default = None
        self.eng_obj = {"pe": nc.tensor, "act": nc.scalar, "dve": nc.vector,
                        "pool": nc.gpsimd, "sp": nc.sync}
        self.esem = {e: es.enter_context(nc.semaphore("c_" + e)) for e in COMPUTE}
        self.dsems = [es.enter_context(nc.semaphore("d%d" % i)) for i in range(n_dma_sems)]
        self.nd = n_dma_sems
        self.dsem_val = [0] * n_dma_sems
        self.di = 0
        self.di_sp = 0
        self.nd_sp = 6
        self.ecount = {e: 0 for e in COMPUTE}
        self.epos = {}
        self.waited = {}
        self.dma_waited = {}
        self.bar_tile = es.enter_context(nc.sbuf_tensor("bar_tile", [1, 8], F32))
        self.n_inst = 0

    def op(self, eng, fn, reads=(), writes=(), dma=0):
        o = _Op(eng, fn, dma)
        deps = []
        for k in reads:
            st = self.state.get(k)
            if st is None:
                if self.default is not None:
                    deps.append((self.default, "raw"))
            else:
                if st[0] is not None:
                    deps.append((st[0], "raw"))
                if isinstance(k, tuple) and k[0] == "ps":
                    for r in st[1]:
                        if r.eng != eng:
                            deps.append((r, "war"))
        for k in writes:
            st = self.state.get(k)
            if st is None:
                if self.default is not None:
                    deps.append((self.default, "waw"))
            else:
                if st[0] is not None:
                    deps.append((st[0], "waw"))
                for r in st[1]:
                    deps.append((r, "war"))
        for k in reads:
            st = self.state.get(k)
            if st is None:
                st = self.state[k] = [self.default, []]
            st[1].append(o)
        for k in writes:
            self.state[k] = [o, []]
        o.waits = deps
        self.ops.append(o)
        return o

    def flush(self, final=False):
        nc = self.nc
        allkeys = list(self.state.keys())
        bt = self.bar_tile
        bar = self.op("dve", lambda: nc.vector.memset(bt[0:1, 0:8], 0.0), reads=(), writes=allkeys)
        bar.signal = True
        for o in self.ops:
            self.epos[o.eng] = self.epos.get(o.eng, 0) + 1
            o.pos = self.epos[o.eng]
        plan = []
        for o in self.ops:
            need = {}
            dneed = []
            for (p, kind) in o.waits:
                if p is o:
                    continue
                if p.dma:
                    s = self.dma_waited.setdefault(o.eng, set())
                    if p.uid not in s:
                        s.add(p.uid)
                        dneed.append(p)
                    continue
                if p.eng == o.eng:
                    if o.eng == "pe" or o.eng == "sp":
                        continue
                if p.pos <= self.waited.get((o.eng, p.eng), 0):
                    continue
                if p.eng not in need or need[p.eng].pos < p.pos:
                    need[p.eng] = p
            for pe_, p in need.items():
                self.waited[(o.eng, pe_)] = p.pos
                p.signal = True
            plan.append((list(need.values()), dneed))
        for o, (cw, dw) in zip(self.ops, plan):
            eng = self.eng_obj[o.eng]
            for p in cw:
                eng.wait_ge(self.esem[p.eng], p.sigval)
            for p in dw:
                eng.wait_ge(self.dsems[p.dsem], p.dval)
            if o.dma:
                if o.eng == "sp":
                    k = self.di_sp % self.nd_sp
                    self.di_sp += 1
                else:
                    k = self.nd_sp + self.di % (self.nd - self.nd_sp)
                    self.di += 1
                if self.dsem_val[k]:
                    eng.wait_ge(self.dsems[k], self.dsem_val[k])
                o.dsem = k
                self.dsem_val[k] += 16 * o.dma
                o.dval = self.dsem_val[k]
                o.fn(self.dsems[k])
            else:
                ins = o.fn()
                if o.signal:
                    self.ecount[o.eng] += 1
                    o.sigval = self.ecount[o.eng]
                    ins.then_inc(self.esem[o.eng], 1)
            self.n_inst += 1
        self.ops = []
        self.state = {}
        self.default = bar
        if final:
            for k in range(self.nd):
                if self.dsem_val[k]:
                    nc.sync.wait_ge(self.dsems[k], self.dsem_val[k])
            for e in COMPUTE:
                if e != "dve":
                    self.eng_obj[e].wait_ge(self.esem["dve"], bar.sigval)


M_QK = 256
SPLIT = (256, 256, 256, 256, 4, 4, 256, 256, 256, 512, 512, 512)
OFF = np.cumsum((0,) + SPLIT)
(O_MQ, O_MK, O_MV, O_MO, O_MI, O_MF, O_AQ, O_AK, O_AV, O_DQ, O_DK, O_DV, _) = OFF
NFM = 20
U_MQ, U_MK, U_MO, U_GF, U_GI, U_AQ, U_AK, U_DQ, U_DK = 0, 2, 4, 6, 7, 8, 10, 12, 16

SM_G = 0
SM_CONV = SM_G + L * 6 * 8
SM_BF = SM_CONV + L * 4 * 4
SM_BI = SM_BF + L
SM_GSUB = SM_BI + L
SM_LAM = SM_GSUB + L
SM_SEL4 = SM_LAM + L * 4 * 64
SM_C123 = SM_SEL4 + 4
NSM = SM_C123 + 3

CB_ID = 0
CB_TRI = 128
CB_ONES = 256
CB_ROWSEL = 384
NCB = CB_ROWSEL + 4 * 128


def _host_tables(inp):
    f = np.float32
    small = np.zeros((128, NSM), f)
    names = ["ffn1_pre_norm", "ffn1_post_norm", "mix_pre_norm", "mix_post_norm", "ffn2_pre_norm", "ffn2_post_norm"]
    for l in range(L):
        for n, nm in enumerate(names):
            small[:, SM_G + (l * 6 + n) * 8: SM_G + (l * 6 + n) * 8 + 8] = np.asarray(inp[nm][l], f).reshape(8, 128).T
        cq = np.asarray(inp["conv_qk"][l], f)
        for cc in range(4):
            small[:, SM_CONV + (l * 4 + cc) * 4: SM_CONV + (l * 4 + cc) * 4 + 4] = cq[:, cc * 128:(cc + 1) * 128].T
        for h in range(4):
            small[32 * h:32 * h + 3, SM_BF + l] = np.asarray(inp["fgate_bias"], f)[l, h]
            small[32 * h, SM_BI + l] = np.asarray(inp["igate_bias"], f)[l, h]
        small[:, SM_GSUB + l] = np.asarray(inp["diff_subln"][l], f)
        for v, nm in enumerate(["lambda_q1", "lambda_k1", "lambda_q2", "lambda_k2"]):
            small[:, SM_LAM + (l * 4 + v) * 64: SM_LAM + (l * 4 + v) * 64 + 64] = np.asarray(inp[nm][l], f)[None, :]
    for h in range(4):
        small[32 * h, SM_SEL4 + h] = 1.0
        small[32 * h, SM_C123 + 0] = 1.0
        small[32 * h + 1, SM_C123 + 1] = 1.0
        small[32 * h + 2, SM_C123 + 2] = 1.0
    cb = np.zeros((128, NCB), f)
    cb[:, CB_ID:CB_ID + 128] = np.eye(128, dtype=f)
    pk = np.arange(128)[:, None]
    pq = np.arange(128)[None, :]
    cb[:, CB_TRI:CB_TRI + 128] = np.where(pk > pq, NEG, 0.0)
    cb[:, CB_ONES:CB_ONES + 128] = 1.0
    for h in range(4):
        cb[32 * h:32 * h + 3, CB_ROWSEL + h * 128: CB_ROWSEL + (h + 1) * 128] = 1.0
    t = np.arange(S)
    thi = (t // 128 * 128).astype(f)
    tlo = (t % 128).astype(f)
    qrows = np.stack([np.ones(S, f), np.ones(S, f), thi, tlo]).astype(f)
    slopes = np.exp2(-8.0 * np.arange(1, 9, dtype=np.float64) / 8).astype(f)
    head_slopes = list(slopes[0::2]) + list(slopes[1::2])
    krows = np.zeros((8, 4, S), f)
    for i, sg in enumerate(head_slopes):
        krows[i, 0] = sg * thi
        krows[i, 1] = sg * tlo
        krows[i, 2] = -sg
        krows[i, 3] = -sg
    blockind = (t[None, :] // 256 == np.arange(8)[:, None]).astype(f)
    return small, cb, qrows, krows, blockind, np.eye(128, dtype=f)


def _host_weights(inp):
    f = np.float32
    out = {}
    for k, (g, u, d) in enumerate([("ffn1_w_gate", "ffn1_w_up", "ffn1_w_down"), ("ffn2_w_gate", "ffn2_w_up", "ffn2_w_down")]):
        wg = np.asarray(inp[g], f).reshape(L, 8, 128, 11, 256).transpose(0, 3, 2, 1, 4)
        wu = np.asarray(inp[u], f).reshape(L, 8, 128, 11, 256).transpose(0, 3, 2, 1, 4)
        wd = np.asarray(inp[d], f).reshape(L, 11, 2, 128, 1024).transpose(0, 1, 3, 2, 4)
        out["wg%d" % k] = np.ascontiguousarray(wg).reshape(L, 11, 128, 2048)
        out["wu%d" % k] = np.ascontiguousarray(wu).reshape(L, 11, 128, 2048)
        out["wd%d" % k] = np.ascontiguousarray(wd).reshape(L, 11, 128, 2048)
    w_in = np.asarray(inp["w_in"], f)
    fm = np.zeros((L, NFM, 1024, 128), f)
    for l in range(L):
        w = w_in[l]
        for i in range(2):
            fm[l, U_MQ + i] = w[:, O_MQ + i * 128: O_MQ + (i + 1) * 128]
            fm[l, U_MK + i] = w[:, O_MK + i * 128: O_MK + (i + 1) * 128]
            fm[l, U_MO + i] = w[:, O_MO + i * 128: O_MO + (i + 1) * 128]
            fm[l, U_AQ + i] = w[:, O_AQ + i * 128: O_AQ + (i + 1) * 128]
            fm[l, U_AK + i] = w[:, O_AK + i * 128: O_AK + (i + 1) * 128]
        for h in range(4):
            for r in range(3):
                fm[l, U_GF, :, 32 * h + r] = w[:, O_MF + h]
            fm[l, U_GI, :, 32 * h] = w[:, O_MI + h]
            fm[l, U_DQ + h] = w[:, O_DQ + h * 128: O_DQ + (h + 1) * 128]
            fm[l, U_DK + h] = w[:, O_DK + h * 128: O_DK + (h + 1) * 128]
    out["win_fm"] = np.ascontiguousarray(fm.reshape(L, NFM, 8, 128, 128).transpose(0, 1, 3, 2, 4)).reshape(L, NFM, 128, 1024)
    wv = np.concatenate([w_in[:, :, O_MV:O_MV + 256], w_in[:, :, O_AV:O_AV + 256], w_in[:, :, O_DV:O_DV + 512]], axis=2)
    out["win_v"] = np.ascontiguousarray(wv.reshape(L, 8, 128, 1024).transpose(0, 2, 1, 3)).reshape(L, 128, 8192)
    wo = np.asarray(inp["w_out"], f).reshape(L, 8, 128, 8, 128).transpose(0, 3, 2, 1, 4)
    out["wo"] = np.ascontiguousarray(wo).reshape(L, 8, 128, 1024)
    return out


def build_program(debug=False, nseq=SEQ_PER_CORE, nlayers=L, stop=None):
    nc = bass.Bass("TRN2", target_bir_lowering=False)
    dt_in = lambda name, shape: nc.dram_tensor(name, list(shape), F32, kind="ExternalInput").ap()
    x_d = dt_in("x", (SEQ_PER_CORE, S, D))
    small_d = dt_in("small", (128, NSM))
    cb_d = dt_in("cb", (128, NCB))
    qrows_d = dt_in("qrows", (4, S))
    krows_d = dt_in("krows", (8, 4, S))
    bind_d = dt_in("blockind", (8, S))
    idf_d = dt_in("identf", (128, 128))
    wg_d = [dt_in("wg%d" % k, (L, 11, 128, 2048)) for k in range(2)]
    wu_d = [dt_in("wu%d" % k, (L, 11, 128, 2048)) for k in range(2)]
    wd_d = [dt_in("wd%d" % k, (L, 11, 128, 2048)) for k in range(2)]
    winfm_d = dt_in("win_fm", (L, NFM, 128, 1024))
    winv_d = dt_in("win_v", (L, 128, 8192))
    wo_d = dt_in("wo", (L, 8, 128, 1024))
    out_d = nc.dram_tensor("out", [SEQ_PER_CORE, S, D], F32, kind="ExternalOutput").ap()
    dbg_d = None
    if debug:
        dbg_d = nc.dram_tensor("dbg", [8, 128, 8 * S], F32, kind="ExternalOutput").ap()

    with contextlib.ExitStack() as top:
        P = Prog(nc, top)
        _cnt = [0]

        def sbt(es, name, shape, dt):
            _cnt[0] += 1
            return es.enter_context(nc.sbuf_tensor("%s_%d" % (name, _cnt[0]), shape, dt))
        ps = [top.enter_context(nc.psum_tensor("psb%d" % i, [128, 512], F32)) for i in range(8)]
        pools = {}

        def bank(pool, banks):
            i = pools.get(pool, 0)
            pools[pool] = i + 1
            return banks[i % len(banks)]

        def PK(b):
            return ("ps", b)

        xT = sbt(top, "xT", [128, 8, S], F32)
        small = sbt(top, "small", [128, NSM], F32)
        cb = sbt(top, "cbf", [128, NCB], BF16)
        identf = sbt(top, "identf", [128, 128], F32)
        ghalf = sbt(top, "ghalf", [128, L * 2 * 8], F32)
        lamt = sbt(top, "lamt", [128, 8 * L], F32)
        ident_bf = cb[:, CB_ID:CB_ID + 128]
        tri_bf = cb[:, CB_TRI:CB_TRI + 128]
        ones_bf = cb[:, CB_ONES:CB_ONES + 128]

        def X(c, blk):
            return ("x", c, blk)

        P.op("sp", lambda s: nc.sync.dma_start(out=small[:], in_=small_d).then_inc(s, 16), writes=["small"], dma=1)
        P.op("sp", lambda s: nc.sync.dma_start(out=identf[:], in_=idf_d).then_inc(s, 16), writes=["identf"], dma=1)
        P.op("pool", lambda s: nc.gpsimd.dma_start(out=cb[:], in_=cb_d).then_inc(s, 16), writes=["cb"], dma=1)
        for l in range(L):
            for k, n in enumerate((1, 5)):
                src = small[:, SM_G + (l * 6 + n) * 8: SM_G + (l * 6 + n) * 8 + 8]
                dst = ghalf[:, (l * 2 + k) * 8:(l * 2 + k) * 8 + 8]
                P.op("dve", lambda src=src, dst=dst: nc.vector.tensor_scalar(dst, src, 0.5, None, op0=ALU.mult),
                     reads=["small"], writes=["ghalf"])
        with contextlib.ExitStack() as es0:
            ltmp = sbt(es0, "ltmp", [128, 64], F32)
            lsum = sbt(es0, "lsum", [128, 4], F32)
            for l in range(L):
                lam_init = 0.8 - 0.6 * math.exp(-0.3 * l)
                for j in range(2):
                    a = small[:, SM_LAM + (l * 4 + 2 * j) * 64: SM_LAM + (l * 4 + 2 * j) * 64 + 64]
                    b = small[:, SM_LAM + (l * 4 + 2 * j + 1) * 64: SM_LAM + (l * 4 + 2 * j + 1) * 64 + 64]
                    P.op("dve", lambda a=a, b=b: nc.vector.tensor_tensor(ltmp[:], a, b, ALU.mult), reads=["small", "ltmp"], writes=["ltmp"])
                    P.op("dve", lambda j=j: nc.vector.reduce_sum(out=lsum[:, j:j + 1], in_=ltmp[:], axis=AX.X), reads=["ltmp"], writes=["lsum"])
                P.op("act", lambda: nc.scalar.activation(lsum[:, 2:4], lsum[:, 0:2], AF.Exp), reads=["lsum"], writes=["lsum"])
                P.op("dve", lambda l=l, li=lam_init: nc.vector.scalar_tensor_tensor(
                    out=lamt[:, 8 * l:8 * l + 1], in0=lsum[:, 3:4], scalar=-li, in1=lsum[:, 2:3], op0=ALU.add, op1=ALU.subtract),
                    reads=["lsum"], writes=["lamt"])
                P.op("dve", lambda l=l, li=lam_init: nc.vector.tensor_scalar(
                    lamt[:, 8 * l + 1:8 * l + 2], small[:, SM_GSUB + l:SM_GSUB + l + 1], 1.0 - li, None, op0=ALU.mult),
                    reads=["small", "lamt"], writes=["lamt"])
            P.flush()

        def rms_stats(es, tag, src_fn, src_keys, nchunk, ncols, inv_n, sq_tile, stat_tile, bankset):
            b = bank("gu", bankset)
            for c in range(nchunk):
                P.op("act", lambda c=c: nc.scalar.activation(sq_tile[:, c % 2, :ncols], src_fn(c), AF.Square),
                     reads=[src_keys(c)], writes=[(tag + "sq", c % 2)])
                P.op("pe", lambda c=c, b=b: nc.tensor.matmul(ps[b][:, :ncols], ones_bf, sq_tile[:, c % 2, :ncols],
                                                            start=(c == 0), stop=(c == nchunk - 1)),
                     reads=[(tag + "sq", c % 2), "cb"], writes=[PK(b)])
            P.op("act", lambda b=b: nc.scalar.activation(stat_tile[:, :ncols], ps[b][:, :ncols], AF.Ln, bias=EPS, scale=inv_n),
                 reads=[PK(b)], writes=[tag + "stat"])
            P.op("act", lambda: nc.scalar.activation(stat_tile[:, :ncols], stat_tile[:, :ncols], AF.Exp, scale=-0.5),
                 reads=[tag + "stat"], writes=[tag + "stat"])

        def dump2(idx, c, ap2, n):
            P.op("pool", lambda s: nc.gpsimd.dma_start(out=dbg_d[idx, 0:ap2.shape[0], c * S:c * S + n], in_=ap2).then_inc(s, 16), reads=["__dump"], dma=1)

        def dump(idx, src3, keys):
            if dbg_d is None:
                return
            for c in range(8):
                P.op("pool", lambda s, c=c: nc.gpsimd.dma_start(out=dbg_d[idx, :, c * S:(c + 1) * S], in_=src3[:, c, :]).then_inc(s, 16),
                     reads=list(keys) + ["__dump"], dma=1)

        for sq in range(nseq):
            with contextlib.ExitStack() as es:
                xin = sbt(es, "xin", [128, 2, D], F32)
                for tt in range(NT):
                    P.op("sp", lambda s, tt=tt: nc.sync.dma_start(out=xin[:, tt % 2, :], in_=x_d[sq, tt * 128:(tt + 1) * 128, :]).then_inc(s, 16),
                         writes=[("xin", tt % 2)], dma=1)
                    for c in range(8):
                        b = bank("tr", [0, 1, 2, 3])
                        P.op("pe", lambda tt=tt, c=c, b=b: nc.tensor.transpose(ps[b][:, 0:128], xin[:, tt % 2, c * 128:(c + 1) * 128], identf[:]),
                             reads=[("xin", tt % 2), "identf"], writes=[PK(b)])
                        eng = "act" if c % 2 else "dve"
                        if eng == "act":
                            P.op("act", lambda tt=tt, c=c, b=b: nc.scalar.copy(xT[:, c, tt * 128:(tt + 1) * 128], ps[b][:, 0:128]),
                                 reads=[PK(b)], writes=[("xtile", c, tt)])
                        else:
                            P.op("dve", lambda tt=tt, c=c, b=b: nc.vector.tensor_copy(xT[:, c, tt * 128:(tt + 1) * 128], ps[b][:, 0:128]),
                                 reads=[PK(b)], writes=[("xtile", c, tt)])
                P.flush()

            for l in range(nlayers if stop != "load" else 0):
                for ffn_i in range(2):
                    if stop in ("ffn1", "mlstm_prep", "mlstm", "moba", "diff") and ffn_i == 1:
                        continue
                    with contextlib.ExitStack() as es:
                        hT = sbt(es, "f_hT", [128, 1, 8, 1024], BF16)
                        yb = sbt(es, "f_y", [128, 2, 8, 1024], F32)
                        wgt = sbt(es, "f_wg", [128, 2, 2048], BF16)
                        wut = sbt(es, "f_wu", [128, 2, 2048], BF16)
                        wdt = sbt(es, "f_wd", [128, 2, 2048], BF16)
                        At = sbt(es, "f_A", [128, 2, 2, 1024], BF16)
                        sgt = sbt(es, "f_sg", [128, 2, 512], F32)
                        sqt = sbt(es, "f_sq", [128, 8, 512], BF16)
                        rst = sbt(es, "f_rs", [128, 2, 512], F32)
                        tmp = sbt(es, "f_tmp", [128, 2, 512], F32)
                        n_pre = 0 if ffn_i == 0 else 4
                        gpre = lambda c: small[:, SM_G + (l * 6 + n_pre) * 8 + c: SM_G + (l * 6 + n_pre) * 8 + c + 1]
                        gpost = lambda c: ghalf[:, (l * 2 + ffn_i) * 8 + c:(l * 2 + ffn_i) * 8 + c + 1]

                        def n_src(kind, half, blk):
                            cs = slice(half * 1024 + blk * 512, half * 1024 + (blk + 1) * 512)
                            if kind == "pre":
                                return (lambda c: xT[:, c, cs]), (lambda c: X(c, half * 2 + blk))
                            return (lambda c: yb[:, half, c, blk * 512:(blk + 1) * 512]), (lambda c: ("fy", half, c, blk))

                        def n_sq(kind, half, blk):
                            src, key = n_src(kind, half, blk)
                            for c in range(8):
                                P.op("act", lambda c=c, src=src: nc.scalar.activation(sqt[:, c, :], src(c), AF.Square),
                                     reads=[key(c)], writes=[("fsq", c)])

                        def n_stat(r):
                            b = bank("gu", [0, 1, 2, 3])
                            for c in range(8):
                                P.op("pe", lambda c=c, b=b: nc.tensor.matmul(ps[b][:, :], ones_bf, sqt[:, c, :], start=(c == 0), stop=(c == 7)),
                                     reads=[("fsq", c), "cb"], writes=[PK(b)])
                            P.op("act", lambda b=b: nc.scalar.activation(rst[:, r, :], ps[b][:, :], AF.Ln, bias=EPS, scale=1.0 / D),
                                 reads=[PK(b)], writes=[("frs", r)])
                            P.op("act", lambda: nc.scalar.activation(rst[:, r, :], rst[:, r, :], AF.Exp, scale=-0.5),
                                 reads=[("frs", r)], writes=[("frs", r)])

                        def n_apply(kind, half, blk, r):
                            cs = slice(half * 1024 + blk * 512, half * 1024 + (blk + 1) * 512)
                            for c in range(8):
                                if kind == "pre":
                                    P.op("dve", lambda c=c: nc.vector.scalar_tensor_tensor(
                                        out=hT[:, 0, c, blk * 512:(blk + 1) * 512], in0=xT[:, c, cs], scalar=gpre(c), in1=rst[:, r, :],
                                        op0=ALU.mult, op1=ALU.mult),
                                        reads=[X(c, half * 2 + blk), ("frs", r), "small"], writes=[("fh", 0, c, blk)])
                                else:
                                    ti = c % 2
                                    P.op("dve", lambda c=c, ti=ti: nc.vector.scalar_tensor_tensor(
                                        out=tmp[:, ti, :], in0=yb[:, half, c, blk * 512:(blk + 1) * 512], scalar=gpost(c), in1=rst[:, r, :],
                                        op0=ALU.mult, op1=ALU.mult), reads=[("fy", half, c, blk), ("frs", r), "ghalf"], writes=[("ftmp", ti)])
                                    if c % 2:
                                        P.op("pool", lambda c=c, ti=ti: nc.gpsimd.tensor_tensor(xT[:, c, cs], xT[:, c, cs], tmp[:, ti, :], ALU.add),
                                             reads=[("ftmp", ti), X(c, half * 2 + blk)], writes=[X(c, half * 2 + blk)])
                                    else:
                                        P.op("dve", lambda c=c, ti=ti: nc.vector.tensor_tensor(xT[:, c, cs], xT[:, c, cs], tmp[:, ti, :], ALU.add),
                                             reads=[("ftmp", ti), X(c, half * 2 + blk)], writes=[X(c, half * 2 + blk)])

                        def load_group(gidx):
                            fg, slot = gidx % 11, gidx % 2
                            P.op("pool", lambda s, fg=fg, slot=slot: nc.gpsimd.dma_start(out=wgt[:, slot, :], in_=wg_d[ffn_i][l, fg]).then_inc(s, 16),
                                 writes=[("fwg", slot)], dma=1)
                            P.op("pool", lambda s, fg=fg, slot=slot: nc.gpsimd.dma_start(out=wut[:, slot, :], in_=wu_d[ffn_i][l, fg]).then_inc(s, 16),
                                 writes=[("fwu", slot)], dma=1)
                            P.op("pool", lambda s, fg=fg, slot=slot: nc.gpsimd.dma_start(out=wdt[:, slot, :], in_=wd_d[ffn_i][l, fg]).then_inc(s, 16),
                                 writes=[("fwd", slot)], dma=1)

                        def compute_group(gidx):
                            half, fg = divmod(gidx, 11)
                            slot = gidx % 2
                            for blk in range(2):
                                for j in range(2):
                                    bg = bank("gu", [0, 1, 2, 3])
                                    bu = bank("gu", [0, 1, 2, 3])
                                    for c in range(8):
                                        P.op("pe", lambda c=c, j=j, blk=blk, bg=bg: nc.tensor.matmul(
                                            ps[bg][:, :], wgt[:, slot, c * 256 + j * 128: c * 256 + (j + 1) * 128], hT[:, 0, c, blk * 512:(blk + 1) * 512],
                                            start=(c == 0), stop=(c == 7)), reads=[("fwg", slot), ("fh", 0, c, blk)], writes=[PK(bg)])
                                    for c in range(8):
                                        P.op("pe", lambda c=c, j=j, blk=blk, bu=bu: nc.tensor.matmul(
                                            ps[bu][:, :], wut[:, slot, c * 256 + j * 128: c * 256 + (j + 1) * 128], hT[:, 0, c, blk * 512:(blk + 1) * 512],
                                            start=(c == 0), stop=(c == 7)), reads=[("fwu", slot), ("fh", 0, c, blk)], writes=[PK(bu)])
                                    si = bank("sg", [0, 1])
                                    P.op("act", lambda bg=bg, si=si: nc.scalar.activation(sgt[:, si, :], ps[bg][:, :], AF.Silu),
                                         reads=[PK(bg)], writes=[("fsg", si)])
                                    P.op("dve", lambda bu=bu, si=si, j=j, blk=blk: nc.vector.tensor_tensor(
                                        At[:, slot, j, blk * 512:(blk + 1) * 512], ps[bu][:, :], sgt[:, si, :], ALU.mult),
                                        reads=[PK(bu), ("fsg", si)], writes=[("fA", slot, j, blk)])
                            for blk in range(2):
                                for dm in range(8):
                                    by = bank("yp", [4, 5, 6, 7])
                                    for j in range(2):
                                        P.op("pe", lambda j=j, dm=dm, blk=blk, by=by: nc.tensor.matmul(
                                            ps[by][:, :], wdt[:, slot, j * 1024 + dm * 128: j * 1024 + (dm + 1) * 128],
                                            At[:, slot, j, blk * 512:(blk + 1) * 512], start=(j == 0), stop=(j == 1)),
                                            reads=[("fwd", slot), ("fA", slot, j, blk)], writes=[PK(by)])
                                    ysl = yb[:, half, dm, blk * 512:(blk + 1) * 512]
                                    if fg == 0:
                                        P.op("act", lambda by=by, ysl=ysl: nc.scalar.copy(ysl, ps[by][:, :]),
                                             reads=[PK(by)], writes=[("fy", half, dm, blk)])
                                    else:
                                        P.op("dve", lambda by=by, ysl=ysl: nc.vector.tensor_tensor(ysl, ps[by][:, :], ysl, ALU.add),
                                             reads=[PK(by), ("fy", half, dm, blk)], writes=[("fy", half, dm, blk)])

                        for blk in range(2):
                            n_sq("pre", 0, blk)
                            n_stat(blk)
                            n_apply("pre", 0, blk, blk)
                        hooks = {}
                        load_group(0)
                        for gidx in range(22):
                            if gidx + 1 < 22:
                                load_group(gidx + 1)
                            if gidx == 11:
                                for blk in range(2):
                                    n_sq("post", 0, blk)
                                    n_stat(blk)
                                    n_apply("post", 0, blk, blk)
                                for blk in range(2):
                                    n_sq("pre", 1, blk)
                                    n_stat(blk)
                                    n_apply("pre", 1, blk, blk)
                            compute_group(gidx)
                            for hk in hooks.get(gidx, ()):
                                hk()
                        for blk in range(2):
                            n_sq("post", 1, blk)
                            n_stat(blk)
                            n_apply("post", 1, blk, blk)
                        P.flush()
                    if debug and sq == 0:
                        dump(l * 3 + (0 if ffn_i == 0 else 2), xT[:, :, :], [X(c, b) for c in range(8) for b in range(4)])
                        P.flush()
                    if ffn_i == 1 or stop == "ffn1":
                        continue
                    mixer(nc, P, top, sbt, ps, bank, PK, X, xT, small, cb, identf, lamt, l, sq,
                          winfm_d, winv_d, wo_d, qrows_d, krows_d, bind_d, rms_stats, dump if (debug and sq == 0) else None, stop, dump2)
                    if debug and sq == 0:
                        dump(l * 3 + 1, xT[:, :, :], [X(c, b) for c in range(8) for b in range(4)])
                        P.flush()

            with contextlib.ExitStack() as es:
                xo = sbt(es, "xo", [128, 2, D], F32)
                for tt in range(NT):
                    for c in range(8):
                        b = bank("tr", [0, 1, 2, 3])
                        P.op("pe", lambda tt=tt, c=c, b=b: nc.tensor.transpose(ps[b][:, 0:128], xT[:, c, tt * 128:(tt + 1) * 128], identf[:]),
                             reads=[X(c, tt // 4), "identf"], writes=[PK(b)])
                        if c % 2:
                            P.op("act", lambda tt=tt, c=c, b=b: nc.scalar.copy(xo[:, tt % 2, c * 128:(c + 1) * 128], ps[b][:, 0:128]),
                                 reads=[PK(b)], writes=[("xo", tt % 2, c)])
                        else:
                            P.op("dve", lambda tt=tt, c=c, b=b: nc.vector.tensor_copy(xo[:, tt % 2, c * 128:(c + 1) * 128], ps[b][:, 0:128]),
                                 reads=[PK(b)], writes=[("xo", tt % 2, c)])
                    P.op("sp", lambda s, tt=tt: nc.sync.dma_start(out=out_d[sq, tt * 128:(tt + 1) * 128, :], in_=xo[:, tt % 2, :]).then_inc(s, 16),
                         reads=[("xo", tt % 2, c) for c in range(8)], dma=1)
                P.flush(final=(sq == nseq - 1))
    return nc


def mixer(nc, P, top, sbt, ps, bank, PK, X, xT, small, cb, identf, lamt, l, sq,
          winfm_d, winv_d, wo_d, qrows_d, krows_d, bind_d, rms_stats, dump, stop=None, dump2=None):
    ident_bf = cb[:, CB_ID:CB_ID + 128]
    tri_bf = cb[:, CB_TRI:CB_TRI + 128]
    ones_bf = cb[:, CB_ONES:CB_ONES + 128]
    with contextlib.ExitStack() as mes:
        hT = sbt(mes, "m_hT", [128, 8, S], BF16)
        mixT = sbt(mes, "m_mix", [128, 8, S], BF16)
        wsl = sbt(mes, "m_wsl", [128, 3, 1024], BF16)
        wv = sbt(mes, "m_wv", [128, 8, 512], BF16)
        pt = sbt(mes, "m_pt", [128, 3, 512], BF16)
        sqt = sbt(mes, "m_sq", [128, 2, 512], BF16)
        rst = sbt(mes, "m_rs", [128, 512], F32)
        H = lambda c, blk: ("mh", c, blk)
        MX = lambda c, blk: ("mix", c, blk)
        gpre = lambda c: small[:, SM_G + (l * 6 + 2) * 8 + c: SM_G + (l * 6 + 2) * 8 + c + 1]
        gpost = lambda c: small[:, SM_G + (l * 6 + 3) * 8 + c: SM_G + (l * 6 + 3) * 8 + c + 1]

        for blk in range(NBLK):
            cs = slice(blk * 512, (blk + 1) * 512)
            rms_stats(mes, "m", lambda c, cs=cs: xT[:, c, cs], lambda c, blk=blk: X(c, blk), 8, 512, 1.0 / D, sqt, rst, [0, 1, 2, 3])
            for c in range(8):
                P.op("dve", lambda c=c, cs=cs: nc.vector.scalar_tensor_tensor(
                    out=hT[:, c, cs], in0=xT[:, c, cs], scalar=gpre(c), in1=rst[:, :], op0=ALU.mult, op1=ALU.mult),
                    reads=[X(c, blk), "mstat", "small"], writes=[H(c, blk)])

        def load_slab(u):
            si = bank("wsl", [0, 1, 2])
            P.op("pool", lambda s, u=u, si=si: nc.gpsimd.dma_start(out=wsl[:, si, :], in_=winfm_d[l, u]).then_inc(s, 16),
                 writes=[("wsl", si)], dma=1)
            return si

        def proj_fm(si, blk, b, M=128):
            for c in range(8):
                P.op("pe", lambda c=c: nc.tensor.matmul(ps[b][0:M, :], wsl[:, si, c * 128: c * 128 + M], hT[:, c, blk * 512:(blk + 1) * 512],
                                                        start=(c == 0), stop=(c == 7)),
                     reads=[("wsl", si), H(c, blk)], writes=[PK(b)])

        def proj_v(voff, ncol, dst_fn, es):
            for c in range(8):
                P.op("pool", lambda s, c=c: nc.gpsimd.dma_start(out=wv[:, c, 0:ncol], in_=winv_d[l, :, c * 1024 + voff: c * 1024 + voff + ncol]).then_inc(s, 16),
                     writes=[("wv", c)], dma=1)
            for tt in range(NT):
                b = bank("pj", [0, 1, 2, 3])
                for c in range(8):
                    P.op("pe", lambda c=c, tt=tt, b=b: nc.tensor.matmul(ps[b][:, 0:ncol], hT[:, c, tt * 128:(tt + 1) * 128], wv[:, c, 0:ncol],
                                                                      start=(c == 0), stop=(c == 7)),
                         reads=[("wv", c), H(c, tt // 4)], writes=[PK(b)])
                dst_fn(tt, b)

        def attn_pair(streams, pv_fn, final_fn, score_fn, look=2):
            steps = []
            for J in range(NBLK):
                ni = 4 * J + 4
                for i in range(ni):
                    c0 = max(0, i - 4 * J) * 128
                    for st in streams:
                        steps.append((st, i, J, c0, i == 0, i == ni - 1))
            ptis = []
            for k in range(len(steps)):
                while len(ptis) < min(len(steps), k + look + 1):
                    st, i, J, c0, f_, l_ = steps[len(ptis)]
                    ptis.append(score_fn(st, i, J, c0))
                st, i, J, c0, f_, l_ = steps[k]
                pv_fn(st, i, J, c0, ptis[k], f_, l_)
                if l_ and st == streams[-1]:
                    final_fn(J)

        def diag_mask(b, c0, i, J):
            if i >= 4 * J:
                P.op("pe", lambda: nc.tensor.matmul(ps[b][:, c0:c0 + 128], ident_bf, tri_bf, start=False, stop=True),
                     reads=["cb"], writes=[PK(b)])

        with contextlib.ExitStack() as es:
            Fs = sbt(es, "fs", [128, S], BF16)
            Qm = sbt(es, "qm", [128, 2, S], BF16)
            Km = sbt(es, "km", [128, 2, S], BF16)
            bcol = sbt(es, "bcol", [128, 68], F32)
            eM = sbt(es, "eM", [128, 4], F32)
            Mx = sbt(es, "Mx", [128, 2], F32)
            Mrep = sbt(es, "Mrep", [128, 128], F32)
            ges = contextlib.ExitStack()
            G0 = sbt(ges, "g0", [128, S + 3], F32)
            G1 = sbt(ges, "g1", [128, S + 3], F32)
            G2 = sbt(ges, "g2", [128, S + 3], F32)
            B0 = sbt(ges, "b0", [128, S], BF16)
            B1 = sbt(ges, "b1", [128, S], BF16)
            nbf = small[:, SM_BF + l:SM_BF + l + 1]
            sgf = load_slab(U_GF)
            for blk in range(NBLK):
                b = bank("pj", [0, 1, 2, 3])
                proj_fm(sgf, blk, b)
                P.op("dve", lambda: nc.vector.tensor_scalar(Mx[:, 1:2], nbf, -1.0, None, op0=ALU.mult), reads=["small"], writes=["negbf"])
                P.op("act", lambda b=b, blk=blk: nc.scalar.activation(G0[:, blk * 512:(blk + 1) * 512], ps[b][:, :], AF.Exp, bias=Mx[:, 1:2], scale=-1.0),
                     reads=[PK(b), "negbf"], writes=[("G0", blk)])
            P.op("act", lambda: nc.scalar.activation(G0[:, 0:S], G0[:, 0:S], AF.Ln, bias=1.0),
                 reads=[("G0", k) for k in range(4)], writes=[("G0", k) for k in range(4)])
            P.op("dve", lambda: nc.vector.memset(G2[:, 0:S], 1.0), writes=["G2"])
            P.op("dve", lambda: nc.vector.tensor_tensor_scan(G1[:, 0:S], G2[:, 0:S], G0[:, 0:S], 0.0, ALU.mult, ALU.add),
                 reads=["G2"] + [("G0", k) for k in range(4)], writes=["G1"])
            sgi = load_slab(U_GI)
            bi = small[:, SM_BI + l:SM_BI + l + 1]
            for blk in range(NBLK):
                b = bank("pj", [0, 1, 2, 3])
                proj_fm(sgi, blk, b)
                P.op("act", lambda b=b, blk=blk: nc.scalar.activation(G0[:, blk * 512:(blk + 1) * 512], ps[b][:, :], AF.Identity, bias=bi, scale=1.0),
                     reads=[PK(b), "small", "G1"], writes=[("G0", blk)])
            P.op("dve", lambda: nc.vector.reduce_max(out=Mx[:, 0:1], in_=G0[:, 0:S], axis=AX.X),
                 reads=[("G0", k) for k in range(4)], writes=["Mx"])
            P.op("dve", lambda: nc.vector.scalar_tensor_tensor(out=G2[:, 0:S], in0=G0[:, 0:S], scalar=Mx[:, 0:1], in1=G1[:, 0:S],
                                                               op0=ALU.subtract, op1=ALU.add),
                 reads=[("G0", k) for k in range(4)] + ["Mx", "G1", "G2"], writes=["G2"])
            sel4 = small[:, SM_SEL4:SM_SEL4 + 4]
            bb = bank("pj", [0, 1, 2, 3])
            for tt in range(NT):
                P.op("pe", lambda tt=tt: nc.tensor.matmul(ps[bb][:, tt * 4:tt * 4 + 4], G2[:, tt * 128:(tt + 1) * 128], sel4, start=True, stop=True),
                     reads=["G2", "small"], writes=[PK(bb)])
            P.op("dve", lambda: nc.vector.memset(Mrep[:], 0.0), writes=["Mrep"])
            P.op("dve", lambda: nc.vector.tensor_scalar(Mrep[:], Mrep[:], Mx[:, 0:1], None, op0=ALU.add), reads=["Mrep", "Mx"], writes=["Mrep"])
            P.op("pe", lambda: nc.tensor.matmul(ps[bb][:, 64:68], Mrep[:], sel4, start=True, stop=True), reads=["Mrep", "small"], writes=[PK(bb)])
            P.op("dve", lambda: nc.vector.tensor_copy(bcol[:, 0:68], ps[bb][:, 0:68]), reads=[PK(bb)], writes=["bcol"])
            P.op("act", lambda: nc.scalar.activation(eM[:, 0:4], bcol[:, 64:68], AF.Exp, scale=-1.0), reads=["bcol"], writes=["eM"])
            c1 = small[:, SM_C123:SM_C123 + 1]
            c2 = small[:, SM_C123 + 1:SM_C123 + 2]
            c3 = small[:, SM_C123 + 2:SM_C123 + 3]
            P.op("dve", lambda: nc.vector.tensor_scalar(B0[:], G1[:, 0:S], -1.0, None, op0=ALU.mult), reads=["G1"], writes=["B0"])
            P.op("dve", lambda: nc.vector.scalar_tensor_tensor(out=G0[:, 0:S], in0=G1[:, 0:S], scalar=-1.0, in1=B0[:], op0=ALU.mult, op1=ALU.subtract),
                 reads=["G1", "B0", "G2"] + [("G0", k) for k in range(4)], writes=[("G0", k) for k in range(4)])
            P.op("dve", lambda: nc.vector.tensor_copy(B1[:], G0[:, 0:S]), reads=[("G0", k) for k in range(4)], writes=["B1"])
            P.op("dve", lambda: nc.vector.tensor_scalar(Fs[:], B0[:], c1, None, op0=ALU.mult), reads=["B0", "small"], writes=["Fs"])
            P.op("dve", lambda: nc.vector.scalar_tensor_tensor(out=Fs[:], in0=B1[:], scalar=c2, in1=Fs[:], op0=ALU.mult, op1=ALU.add),
                 reads=["B1", "Fs", "small"], writes=["Fs"])
            P.op("dve", lambda: nc.vector.tensor_tensor(G0[:, 0:S], G0[:, 0:S], B1[:], ALU.subtract),
                 reads=[("G0", k) for k in range(4)] + ["B1"], writes=[("G0", k) for k in range(4)])
            P.op("dve", lambda: nc.vector.tensor_copy(B0[:], G0[:, 0:S]), reads=[("G0", k) for k in range(4)] + ["Fs"], writes=["B0"])
            P.op("dve", lambda: nc.vector.scalar_tensor_tensor(out=Fs[:], in0=B0[:], scalar=c3, in1=Fs[:], op0=ALU.mult, op1=ALU.add),
                 reads=["B0", "Fs", "small"], writes=["Fs"])
            P.op("dve", lambda: nc.vector.memset(G0[:, 0:3], 0.0), reads=["B0"] + [("G0", k) for k in range(4)], writes=["G0pad"])
            for cc in range(4):
                su = load_slab((U_MQ if cc < 2 else U_MK) + cc % 2)
                for blk in range(NBLK):
                    b = bank("pj", [0, 1, 2, 3])
                    proj_fm(su, blk, b)
                    P.op("act", lambda b=b, blk=blk: nc.scalar.copy(G0[:, 3 + blk * 512: 3 + (blk + 1) * 512], ps[b][:, :]),
                         reads=[PK(b), "G0pad"], writes=[("G0", blk)])
                wc = lambda j, cc=cc: small[:, SM_CONV + (l * 4 + cc) * 4 + j: SM_CONV + (l * 4 + cc) * 4 + j + 1]
                allg0 = [("G0", k) for k in range(4)] + ["G0pad"]
                P.op("dve", lambda wc=wc: nc.vector.tensor_scalar(G2[:, 0:S], G0[:, 0:S], wc(0), None, op0=ALU.mult),
                     reads=allg0 + ["small", "G2"], writes=["G2"])
                for j in (1, 2, 3):
                    P.op("dve", lambda wc=wc, j=j: nc.vector.scalar_tensor_tensor(out=G2[:, 0:S], in0=G0[:, j:j + S], scalar=wc(j), in1=G2[:, 0:S],
                                                                                  op0=ALU.mult, op1=ALU.add),
                         reads=allg0 + ["small", "G2"], writes=["G2"])
                dstt = Qm if cc < 2 else Km
                P.op("act", lambda dstt=dstt, cc=cc: nc.scalar.activation(dstt[:, cc % 2, :], G2[:, 0:S], AF.Silu),
                     reads=["G2"], writes=[("qk", cc)])
            P.flush()
            if dump is not None:
                dump2(3, 0, Fs[:, :], S)
                dump2(3, 1, G1[:, 0:S], S)
                dump2(3, 2, bcol[:, 0:68], 68)
                dump2(3, 3, eM[:, 0:4], 4)
                dump2(4, 0, Qm[:, 0, :], S)
                dump2(4, 1, Qm[:, 1, :], S)
                dump2(4, 2, Km[:, 0, :], S)
                dump2(4, 3, Km[:, 1, :], S)
                P.flush()
            ges.close()
            if stop == "mlstm_prep":
                return
            Vb = sbt(es, "m_V", [128, NT, 512], BF16)
            wt = sbt(es, "wt", [128, 3, 512], F32)
            ft = sbt(es, "ft", [128, 4, 512], F32)
            P.op("dve", lambda: nc.vector.memset(Vb[:, :, :].rearrange("p t (q c) -> p t q c", q=2)[:, :, :, 64:192], 1.0),
                 writes=[("V", tt) for tt in range(NT)])
            def vdst(tt, b):
                src = ps[b][:, 0:256].rearrange("p (q s e) -> p q s e", q=2, s=2)
                dst = Vb[:, tt, :].rearrange("p (q c) -> p q c", q=2)
                P.op("act", lambda src=src, dst=dst: nc.scalar.copy(dst[:, :, 0:64], src[:, :, 0, :]), reads=[PK(b)], writes=[("V", tt)])
                P.op("act", lambda src=src, dst=dst: nc.scalar.copy(dst[:, :, 192:256], src[:, :, 1, :]), reads=[PK(b), ("V", tt)], writes=[("V", tt)])
            proj_v(0, 256, vdst, es)
            if dump is not None:
                P.flush()
                for tt in range(4):
                    dump2(5, 0, Vb[:, tt, :], 512) if tt == 0 else dump2(5, tt, Vb[:, tt, :], 512)
                P.flush()
            for pr in range(2):
                def score(st, i, J, c0, pr=pr):
                    h = 2 * pr + st
                    rows = slice(64 * st, 64 * st + 64)
                    be = bank("scm", [0, 1, 2, 3, 6, 7])
                    bs_ = bank("scm", [0, 1, 2, 3, 6, 7])
                    P.op("pe", lambda: nc.tensor.matmul(ps[be][:, c0:512], cb[:, CB_ROWSEL + h * 128: CB_ROWSEL + (h + 1) * 128],
                                                        Fs[:, J * 512 + c0:(J + 1) * 512], start=True, stop=(i < 4 * J)),
                         reads=["cb", "Fs"], writes=[PK(be)])
                    diag_mask(be, c0, i, J)
                    wi = bank("wt", [0, 1, 2])
                    P.op("act", lambda: nc.scalar.activation(wt[:, wi, c0:512], ps[be][:, c0:512], AF.Exp, bias=bcol[:, i * 4 + h:i * 4 + h + 1], scale=1.0),
                         reads=[PK(be), "bcol"], writes=[("wt", wi)])
                    P.op("pe", lambda: nc.tensor.matmul(ps[bs_][:, c0:512], Km[rows, pr, i * 128:(i + 1) * 128], Qm[rows, pr, J * 512 + c0:(J + 1) * 512],
                                                        start=True, stop=True),
                         reads=[("qk", 2 + pr), ("qk", pr)], writes=[PK(bs_)])
                    pti = bank("pt", [0, 1, 2])
                    P.op("dve", lambda: nc.vector.scalar_tensor_tensor(out=pt[:, pti, c0:512], in0=ps[bs_][:, c0:512], scalar=0.125, in1=wt[:, wi, c0:512],
                                                                       op0=ALU.mult, op1=ALU.mult),
                         reads=[PK(bs_), ("wt", wi)], writes=[("pt", pti)])
                    return pti

                def pv(st, i, J, c0, pti, first, last, pr=pr):
                    h = 2 * pr + st
                    P.op("pe", lambda: nc.tensor.matmul(ps[4 + st][:, c0:512], Vb[:, i, h * 128:(h + 1) * 128], pt[:, pti, c0:512], start=first, stop=last),
                         reads=[("V", i), ("pt", pti)], writes=[PK(4 + st)])

                smo = load_slab(U_MO + pr)

                def final(J, pr=pr, smo=smo):
                    bo = bank("scm", [0, 1, 2, 3, 6, 7])
                    proj_fm(smo, J, bo)
                    P.op("act", lambda: nc.scalar.activation(ft[:, 0, :], ps[bo][:, :], AF.Exp, scale=-1.0), reads=[PK(bo), "ft0"], writes=["ft0"])
                    P.op("act", lambda: nc.scalar.activation(ft[:, 0, :], ft[:, 0, :], AF.Ln, bias=1.0), reads=["ft0"], writes=["ft0"])
                    P.op("act", lambda: nc.scalar.activation(ft[:, 0, :], ft[:, 0, :], AF.Exp, scale=-1.0), reads=["ft0"], writes=["ft0"])
                    for st in range(2):
                        h = 2 * pr + st
                        o_ps = ps[4 + st]
                        wr = slice(64 * st, 64 * st + 64)
                        dr = slice(64 * (1 - st), 64 * (1 - st) + 64)
                        k1, k2 = ("ft1", st), ("ft2", st)
                        P.op("act", lambda o_ps=o_ps, wr=wr, dr=dr: nc.scalar.activation(ft[wr, 1, :], o_ps[dr, :], AF.Abs), reads=[PK(4 + st), k1], writes=[k1])
                        P.op("dve", lambda h=h, wr=wr: nc.vector.tensor_scalar(ft[wr, 1, :], ft[wr, 1, :], eM[wr, h:h + 1], None, op0=ALU.max),
                             reads=[k1, "eM"], writes=[k1])
                        P.op("act", lambda wr=wr: nc.scalar.activation(ft[wr, 1, :], ft[wr, 1, :], AF.Ln), reads=[k1], writes=[k1])
                        P.op("act", lambda wr=wr: nc.scalar.activation(ft[wr, 1, :], ft[wr, 1, :], AF.Exp, scale=-1.0), reads=[k1], writes=[k1])
                        P.op("dve", lambda o_ps=o_ps, wr=wr: nc.vector.tensor_tensor(ft[wr, 2, :], o_ps[wr, :], ft[wr, 1, :], ALU.mult),
                             reads=[PK(4 + st), k1, k2], writes=[k2])
                        P.op("dve", lambda wr=wr: nc.vector.tensor_tensor(mixT[wr, pr, J * 512:(J + 1) * 512], ft[wr, 2, :], ft[wr, 0, :], ALU.mult),
                             reads=[k2, "ft0"], writes=[("mixm%d" % st, pr, J)])

                attn_pair([0, 1], pv, final, score)
            P.flush()
            if dump is not None:
                for k in range(4):
                    dump2(5, 4 + k, ft[:, k, :], 512)
                dump2(3, 4, wt[:, 0, :], 512)
                dump2(3, 5, pt[:, 0, :], 512)
                P.flush()
        if dump is not None:
            dump(6, mixT[:, :, :], [])
            P.flush()
        if stop == "mlstm":
            return

        with contextlib.ExitStack() as es:
            Vb = sbt(es, "s_V", [128, NT, 512], BF16)
            P.op("dve", lambda: nc.vector.memset(Vb[:, :, :].rearrange("p t (h e) -> p t h e", h=4)[:, :, :, 64:128], 1.0),
                 writes=[("V", tt) for tt in range(NT)])
            Qa = [sbt(es, "qa%d" % i, [128, S], BF16) for i in range(2)]
            Ka = [sbt(es, "ka%d" % i, [128, S], BF16) for i in range(2)]
            ksq = sbt(es, "ksq", [128, 4, 512], BF16)
            kst = sbt(es, "kst", [128, 16], F32)
            kmf = sbt(es, "kmf", [128, 8], F32)
            kmb = [sbt(es, "kmb%d" % i, [64, 8], BF16) for i in range(2)]
            gw = sbt(es, "gw", [128, 64], F32)
            top8 = sbt(es, "top8", [128, 64], F32)
            selb = sbt(es, "selb", [128, 64], F32)
            ft = sbt(es, "sft", [128, 6, 512], F32)
            for i in range(2):
                P.op("dve", lambda i=i: nc.vector.memset(Qa[i][64:128, :], 0.0), writes=[("Qa", i, k) for k in range(4)] + [("Qrow", i)])
                P.op("dve", lambda i=i: nc.vector.memset(Ka[i][64:128, :], 0.0), writes=[("Ka", i)])
                P.op("pool", lambda s, i=i: nc.gpsimd.dma_start(out=Qa[i][72:76, :], in_=qrows_d).then_inc(s, 16),
                     reads=[("Qrow", i)], writes=[("Qrow", i)] + [("Qa", i, k) for k in range(4)], dma=1)
                P.op("dve", lambda i=i: nc.vector.memset(Ka[i][96:97, :], 1.0), reads=[("Ka", i)], writes=[("Ka", i)])

            def softmax_pair(uq, uk, slope_idx, moba, vcol_fn, pv_m, nacc, clear_sel=False):
                for st in range(2):
                    P.op("pool", lambda s, st=st: nc.gpsimd.dma_start(out=Ka[st][72:76, :], in_=krows_d[slope_idx[st]]).then_inc(s, 16),
                         reads=[("Ka", st)], writes=[("Ka", st)], dma=1)
                    if moba:
                        P.op("pool", lambda s, st=st: nc.gpsimd.dma_start(out=Ka[st][64:72, :], in_=bind_d).then_inc(s, 16),
                             reads=[("Ka", st)], writes=[("Ka", st)], dma=1)
                    elif clear_sel:
                        P.op("dve", lambda st=st: nc.vector.memset(Ka[st][64:72, :], 0.0), reads=[("Ka", st)], writes=[("Ka", st)])
                        P.op("dve", lambda st=st: nc.vector.memset(Qa[st][64:72, :], 0.0), reads=[("Qa", st, k) for k in range(4)],
                             writes=[("Qa", st, k) for k in range(4)])
                sk = load_slab(uk)
                sq_ = load_slab(uq)
                for blk in range(NBLK):
                    b = bank("pj", [0, 1, 2, 3])
                    proj_fm(sk, blk, b)
                    cs = slice(blk * 512, (blk + 1) * 512)
                    P.op("dve", lambda b=b, cs=cs: nc.vector.tensor_copy(Ka[0][0:64, cs], ps[b][0:64, :]), reads=[PK(b), ("Ka", 0)], writes=[("Ka", 0)])
                    P.op("dve", lambda b=b, cs=cs: nc.vector.tensor_copy(Ka[1][0:64, cs], ps[b][64:128, :]), reads=[PK(b), ("Ka", 1)], writes=[("Ka", 1)])
                    P.op("act", lambda b=b, blk=blk: nc.scalar.activation(ksq[:, blk, :], ps[b][:, :], AF.Square), reads=[PK(b)], writes=[("ksq", blk)])
                    if moba:
                        P.op("dve", lambda b=b, blk=blk: nc.vector.reduce_sum(out=kmf[:, 2 * blk:2 * blk + 2],
                                                                             in_=ps[b][:, :].rearrange("p (n k) -> p n k", n=2), axis=AX.X),
                             reads=[PK(b), "kmf"], writes=["kmf"])
                qbanks = []
                for blk in range(NBLK):
                    b = bank("pj", [0, 1, 2, 3])
                    qbanks.append(b)
                    proj_fm(sq_, blk, b)
                    cs = slice(blk * 512, (blk + 1) * 512)
                    P.op("act", lambda b=b, cs=cs: nc.scalar.activation(Qa[0][0:64, cs], ps[b][0:64, :], AF.Copy, scale=0.125),
                         reads=[PK(b), ("Qa", 0, blk)], writes=[("Qa", 0, blk)])
                    P.op("act", lambda b=b, cs=cs: nc.scalar.activation(Qa[1][0:64, cs], ps[b][64:128, :], AF.Copy, scale=0.125),
                         reads=[PK(b), ("Qa", 1, blk)], writes=[("Qa", 1, blk)])
                for blk in range(NBLK):
                    for st in range(2):
                        bn = bank("fin4", [4, 5, 6, 7])
                        rows = slice(64 * st, 64 * st + 64)
                        P.op("pe", lambda rows=rows, blk=blk, bn=bn: nc.tensor.matmul(ps[bn][0:1, :], cb[rows, CB_ONES:CB_ONES + 1], ksq[rows, blk, :],
                                                                                   start=True, stop=True),
                             reads=[("ksq", blk), "cb"], writes=[PK(bn)])
                        P.op("dve", lambda bn=bn, st=st, blk=blk: nc.vector.reduce_max(out=kst[0:1, st * 4 + blk: st * 4 + blk + 1], in_=ps[bn][0:1, :], axis=AX.X),
                             reads=[PK(bn), "kst"], writes=["kst"])
                for st in range(2):
                    P.op("dve", lambda st=st: nc.vector.reduce_max(out=kst[0:1, 8 + st:9 + st], in_=kst[0:1, st * 4:st * 4 + 4], axis=AX.X),
                         reads=["kst"], writes=["kst"])
                    P.op("dve", lambda st=st: nc.vector.tensor_scalar(kst[0:1, 10 + st:11 + st], kst[0:1, 8 + st:9 + st], -1.0 / 16, None, op0=ALU.mult),
                         reads=["kst"], writes=["kst"])
                if moba:
                    P.op("act", lambda: nc.scalar.activation(kmb[0][0:64, :], kmf[0:64, :], AF.Copy, scale=1.0 / 256), reads=["kmf"], writes=["kmb0"])
                    P.op("act", lambda: nc.scalar.activation(kmb[1][0:64, :], kmf[64:128, :], AF.Copy, scale=1.0 / 256), reads=["kmf"], writes=["kmb1"])
                for blk in range(NBLK):
                    b = qbanks[blk]
                    cs = slice(blk * 512, (blk + 1) * 512)
                    P.op("act", lambda b=b, blk=blk: nc.scalar.activation(ksq[:, blk, :], ps[b][:, :], AF.Square), reads=[PK(b)], writes=[("ksq", blk)])
                    for st in range(2):
                        bn = bank("fin4", [4, 5, 6, 7])
                        rows = slice(64 * st, 64 * st + 64)
                        P.op("pe", lambda rows=rows, blk=blk, bn=bn: nc.tensor.matmul(ps[bn][0:1, :], cb[rows, CB_ONES:CB_ONES + 1], ksq[rows, blk, :],
                                                                                   start=True, stop=True),
                             reads=[("ksq", blk), "cb"], writes=[PK(bn)])
                        P.op("act", lambda bn=bn, st=st, cs=cs: nc.scalar.activation(Qa[st][96:97, cs], ps[bn][0:1, :], AF.Identity,
                                                                                  bias=kst[0:1, 10 + st:11 + st], scale=-1.0 / 16),
                             reads=[PK(bn), "kst", ("Qa", st, blk)], writes=[("Qa", st, blk)])
                if moba:
                    for st in range(2):
                        bg = bank("fin", [6, 7])
                        for k in range(8):
                            qt = 8 + k
                            P.op("pe", lambda st=st, qt=qt, k=k, bg=bg: nc.tensor.matmul(ps[bg][:, k * 8:k * 8 + 8], Qa[st][0:64, qt * 128:(qt + 1) * 128],
                                                                                      kmb[st][0:64, :], start=True, stop=True),
                                 reads=[("Qa", st, qt // 4), "kmb%d" % st], writes=[PK(bg)])
                        P.op("dve", lambda: nc.vector.memset(gw[:], -1e30), reads=["gw"], writes=["gw"])
                        P.op("dve", lambda: nc.vector.memset(selb[:], 0.0), reads=["selb"], writes=["selb"])
                        for k in range(8):
                            j = (8 + k) // 2
                            P.op("dve", lambda bg=bg, j=j, k=k: nc.vector.tensor_copy(gw[:, k * 8:k * 8 + j], ps[bg][:, k * 8:k * 8 + j]),
                                 reads=[PK(bg), "gw"], writes=[("gw", k)])
                        for k in range(8):
                            P.op("dve", lambda k=k: nc.vector.max(out=top8[:, k * 8:k * 8 + 8], in_=gw[:, k * 8:k * 8 + 8]),
                                 reads=[("gw", k), "gw", "top8"], writes=[("top8", k)])
                        for k in range(8):
                            j = (8 + k) // 2
                            P.op("dve", lambda j=j, k=k: nc.vector.tensor_scalar(selb[:, k * 8:k * 8 + j], gw[:, k * 8:k * 8 + j], top8[:, k * 8 + 2:k * 8 + 3], 1.0,
                                                                                 op0=ALU.is_ge, op1=ALU.subtract),
                                 reads=[("gw", k), ("top8", k), "selb"], writes=[("selb", k)])
                        for hb in range(2):
                            bt = bank("pj", [0, 1, 2, 3])
                            for kk in range(4):
                                k = hb * 4 + kk
                                P.op("pe", lambda bt=bt, k=k, kk=kk: nc.tensor.transpose(ps[bt][0:8, kk * 128:(kk + 1) * 128], selb[:, k * 8:k * 8 + 8], identf[:]),
                                     reads=[("selb", k), "selb", "identf"], writes=[PK(bt)])
                            P.op("act", lambda st=st, hb=hb, bt=bt: nc.scalar.activation(Qa[st][64:72, 1024 + hb * 512:1024 + (hb + 1) * 512], ps[bt][0:8, :],
                                                                                     AF.Copy, scale=-NEG),
                                 reads=[PK(bt), ("Qa", st, 2 + hb)], writes=[("Qa", st, 2 + hb)])
                        P.op("dve", lambda: nc.vector.memset(top8[:, 0:1], 0.0), reads=[("gw", k) for k in range(8)] + [("top8", k) for k in range(8)] + [("selb", k) for k in range(8)],
                             writes=["gw", "top8", "selb"])

                def score(st, i, J, c0):
                    b = bank("sc", [0, 1, 2, 3])
                    P.op("pe", lambda: nc.tensor.matmul(ps[b][:, c0:512], Ka[st][0:97, i * 128:(i + 1) * 128], Qa[st][0:97, J * 512 + c0:(J + 1) * 512],
                                                        start=True, stop=(i < 4 * J)),
                         reads=[("Ka", st), ("Qa", st, J)], writes=[PK(b)])
                    diag_mask(b, c0, i, J)
                    pti = bank("pt", [0, 1, 2])
                    P.op("act", lambda: nc.scalar.activation(pt[:, pti, c0:512], ps[b][:, c0:512], AF.Exp), reads=[PK(b)], writes=[("pt", pti)])
                    return pti

                def pv(st, i, J, c0, pti, first, last):
                    for a in range(nacc):
                        ba = 4 + st * nacc + a
                        lhs = vcol_fn(st, i) if a == 0 else ones_bf
                        P.op("pe", lambda ba=ba, lhs=lhs: nc.tensor.matmul(ps[ba][:, c0:512], lhs, pt[:, pti, c0:512], start=first, stop=last),
                             reads=[("V", i), ("pt", pti), "cb"], writes=[PK(ba)])

                attn_pair([0, 1], pv, pv_m, score)

            def vdst_a(tt, b):
                P.op("act", lambda tt=tt, b=b: nc.scalar.copy(
                    Vb[:, tt, :].rearrange("p (h e) -> p h e", h=4)[:, :, 0:64], ps[b][:, 0:256].rearrange("p (h e) -> p h e", h=4)),
                    reads=[PK(b)], writes=[("V", tt)])
            proj_v(256, 256, vdst_a, es)
            for pr in range(2):
                def fin_moba(J, pr=pr):
                    for st in range(2):
                        o_ps = ps[4 + st]
                        P.op("act", lambda o_ps=o_ps: nc.scalar.activation(ft[0:64, 0, :], o_ps[64:128, :], AF.Ln), reads=[PK(4 + st), "sft0"], writes=["sft0"])
                        P.op("act", lambda: nc.scalar.activation(ft[0:64, 0, :], ft[0:64, 0, :], AF.Exp, scale=-1.0), reads=["sft0"], writes=["sft0"])
                        P.op("dve", lambda o_ps=o_ps, st=st: nc.vector.tensor_tensor(mixT[64 * st:64 * st + 64, 2 + pr, J * 512:(J + 1) * 512],
                                                                                     o_ps[0:64, :], ft[0:64, 0, :], ALU.mult),
                             reads=[PK(4 + st), "sft0"], writes=[("mixm", st, pr, J)])
                softmax_pair(U_AQ + pr, U_AK + pr, [2 * pr, 2 * pr + 1], True,
                             lambda st, i, pr=pr: Vb[:, i, (2 * pr + st) * 128:(2 * pr + st + 1) * 128], fin_moba, 1)
            if stop == "moba":
                P.flush()
                if dump is not None:
                    dump(7, mixT[:, :, :], [])
                    P.flush()
                return
            def vdst_d(tt, b):
                P.op("act", lambda tt=tt, b=b: nc.scalar.copy(Vb[:, tt, :], ps[b][:, :]), reads=[PK(b)], writes=[("V", tt)])
            proj_v(512, 512, vdst_d, es)
            neglam = lamt[:, 8 * l:8 * l + 1]
            gsub = lamt[:, 8 * l + 1:8 * l + 2]
            for h in range(4):
                def fin_diff(J, h=h):
                    P.op("act", lambda: nc.scalar.activation(ft[:, 0, :], ps[5][:, :], AF.Ln), reads=[PK(5), "sft0"], writes=["sft0"])
                    P.op("act", lambda: nc.scalar.activation(ft[:, 1, :], ps[7][:, :], AF.Ln), reads=[PK(7), "sft1"], writes=["sft1"])
                    P.op("act", lambda: nc.scalar.activation(ft[:, 0, :], ft[:, 0, :], AF.Exp, scale=-1.0), reads=["sft0"], writes=["sft0"])
                    P.op("act", lambda: nc.scalar.activation(ft[:, 1, :], ft[:, 1, :], AF.Exp, scale=-1.0), reads=["sft1"], writes=["sft1"])
                    P.op("dve", lambda: nc.vector.tensor_tensor(ft[:, 2, :], ps[4][:, :], ft[:, 0, :], ALU.mult), reads=[PK(4), "sft0", "sft2"], writes=["sft2"])
                    P.op("dve", lambda: nc.vector.tensor_tensor(ft[:, 3, :], ps[6][:, :], ft[:, 1, :], ALU.mult), reads=[PK(6), "sft1", "sft3"], writes=["sft3"])
                    P.op("dve", lambda: nc.vector.scalar_tensor_tensor(out=ft[:, 4, :], in0=ft[:, 3, :], scalar=neglam, in1=ft[:, 2, :], op0=ALU.mult, op1=ALU.add),
                         reads=["sft2", "sft3", "lamt", "sft4"], writes=["sft4"])
                    qi = bank("ksq", [0, 1])
                    P.op("act", lambda qi=qi: nc.scalar.activation(ksq[:, qi, :], ft[:, 4, :], AF.Square), reads=["sft4"], writes=[("ksq", qi)])
                    bn = bank("sc", [0, 1, 2, 3])
                    P.op("pe", lambda qi=qi, bn=bn: nc.tensor.matmul(ps[bn][:, :], ones_bf, ksq[:, qi, :], start=True, stop=True),
                         reads=[("ksq", qi), "cb"], writes=[PK(bn)])
                    P.op("act", lambda bn=bn: nc.scalar.activation(ft[:, 5, :], ps[bn][:, :], AF.Ln, bias=EPS, scale=1.0 / 128), reads=[PK(bn), "sft5"], writes=["sft5"])
                    P.op("act", lambda: nc.scalar.activation(ft[:, 5, :], ft[:, 5, :], AF.Exp, scale=-0.5), reads=["sft5"], writes=["sft5"])
                    P.op("dve", lambda: nc.vector.scalar_tensor_tensor(out=mixT[:, 4 + h, J * 512:(J + 1) * 512], in0=ft[:, 4, :], scalar=gsub, in1=ft[:, 5, :],
                                                                       op0=ALU.mult, op1=ALU.mult),
                         reads=["sft4", "sft5", "lamt"], writes=[("mixd", h, J)])
                softmax_pair(U_DQ + h, U_DK + h, [4 + h, 4 + h], False,
                             lambda st, i, h=h: Vb[:, i, h * 128:(h + 1) * 128], fin_diff, 2, clear_sel=(h == 0))
            P.flush()
        if dump is not None:
            dump(7, mixT[:, :, :], [])
            P.flush()
        if stop == "diff":
            return

        with contextlib.ExitStack() as es:
            wo = sbt(es, "wo", [128, 8, 1024], BF16)
            yb = sbt(es, "o_y", [128, 2, 8, 512], F32)
            tmp = sbt(es, "o_tmp", [128, 2, 512], F32)
            for dm in range(8):
                P.op("pool", lambda s, dm=dm: nc.gpsimd.dma_start(out=wo[:, dm, :], in_=wo_d[l, dm]).then_inc(s, 16), writes=[("wo", dm)], dma=1)

            def o_proj(blk):
                cs = slice(blk * 512, (blk + 1) * 512)
                ys = blk % 2
                for dm in range(8):
                    b = bank("yp", [4, 5, 6, 7])
                    for c in range(8):
                        P.op("pe", lambda c=c, dm=dm, b=b: nc.tensor.matmul(ps[b][:, :], wo[:, dm, c * 128:(c + 1) * 128], mixT[:, c, cs],
                                                                         start=(c == 0), stop=(c == 7)),
                             reads=[("wo", dm)], writes=[PK(b)])
                    if dm % 2:
                        P.op("act", lambda dm=dm, b=b: nc.scalar.copy(yb[:, ys, dm, :], ps[b][:, :]), reads=[PK(b)], writes=[("oy", ys, dm)])
                    else:
                        P.op("dve", lambda dm=dm, b=b: nc.vector.tensor_copy(yb[:, ys, dm, :], ps[b][:, :]), reads=[PK(b)], writes=[("oy", ys, dm)])

            def o_norm(blk):
                cs = slice(blk * 512, (blk + 1) * 512)
                ys = blk % 2
                rms_stats(es, "m", lambda c: yb[:, ys, c, :], lambda c: ("oy", ys, c), 8, 512, 1.0 / D, sqt, rst, [0, 1, 2, 3])
                for c in range(8):
                    ti = c % 2
                    P.op("dve", lambda c=c, ti=ti: nc.vector.scalar_tensor_tensor(out=tmp[:, ti, :], in0=yb[:, ys, c, :], scalar=gpost(c), in1=rst[:, :],
                                                                                  op0=ALU.mult, op1=ALU.mult),
                         reads=[("oy", ys, c), "mstat", "small"], writes=[("otmp", ti)])
                    P.op("pool", lambda c=c, ti=ti: nc.gpsimd.tensor_tensor(xT[:, c, cs], xT[:, c, cs], tmp[:, ti, :], ALU.add),
                         reads=[("otmp", ti), X(c, blk)], writes=[X(c, blk)])

            o_proj(0)
            for blk in range(NBLK):
                if blk + 1 < NBLK:
                    o_proj(blk + 1)
                o_norm(blk)
            P.flush()


_CACHE = {}


def _prep_inputs(inputs):
    small, cbt, qrows, krows, blockind, identf = _host_tables(inputs)
    w = _host_weights(inputs)
    shared = {"small": small, "cb": cbt, "qrows": qrows, "krows": krows, "blockind": blockind, "identf": identf}
    shared.update(w)
    return shared


def kernel(**inputs):
    x = np.ascontiguousarray(np.asarray(inputs["x"], np.float32))
    shared = _prep_inputs(inputs)
    if "nc" not in _CACHE:
        _CACHE["nc"] = build_program()
    nc = _CACHE["nc"]
    in_maps = []
    for c in range(NCORES):
        m = {"x": x[c * SEQ_PER_CORE:(c + 1) * SEQ_PER_CORE]}
        m.update(shared)
        in_maps.append(m)
    res = run_bass_kernel_spmd(nc, in_maps, core_ids=list(range(NCORES)))
    out = np.concatenate([np.asarray(r["out"], np.float32) for r in res.results], axis=0)
    return out
```

```python
import contextlib
import math
import numpy as np
import concourse.bass as bass
import concourse.mybir as mybir
from concourse.bass_utils import run_bass_kernel_spmd

F32 = mybir.dt.float32
BF16 = mybir.dt.bfloat16
AF = mybir.ActivationFunctionType
ALU = mybir.AluOpType
AX = mybir.AxisListType

NCORES = 8
SEQ_PER_CORE = 2
S = 2048
D = 1024
DFF = 2816
L = 2
NBLK = S // 512
NT = S // 128
NEG = -30000.0
EPS = 1e-6
COMPUTE = ("pe", "act", "dve", "pool")


class _Op:
    __slots__ = ("eng", "fn", "waits", "signal", "pos", "dma", "dsem", "dval", "sigval", "uid")
    _n = 0

    def __init__(self, eng, fn, dma):
        _Op._n += 1
        self.uid = _Op._n
        self.eng = eng
        self.fn = fn
        self.waits = []
        self.signal = False
        self.pos = -1
        self.dma = dma
        self.dsem = None
        self.dval = 0
        self.sigval = None


class Prog:
    def __init__(self, nc, es, n_dma_sems=24):
        self.nc = nc
        self.ops = []
        self.state = {}
        self.default = None
        self.eng_obj = {"pe": nc.tensor, "act": nc.scalar, "dve": nc.vector,
                        "pool": nc.gpsimd, "sp": nc.sync}
        self.esem = {e: es.enter_context(nc.semaphore("c_" + e)) for e in COMPUTE}
        self.dsems = [es.enter_context(nc.semaphore("d%d" % i)) for i in range(n_dma_sems)]
        self.nd = n_dma_sems
        self.dsem_val = [0] * n_dma_sems
        self.di = 0
        self.di_sp = 0
        self.nd_sp = 6
        self.ecount = {e: 0 for e in COMPUTE}
        self.epos = {}
        self.waited = {}
        self.dma_waited = {}
        self.bar_tile = es.enter_context(nc.sbuf_tensor("bar_tile", [1, 8], F32))
        self.n_inst = 0

    def op(self, eng, fn, reads=(), writes=(), dma=0):
        o = _Op(eng, fn, dma)
        deps = []
        for k in reads:
            st = self.state.get(k)
            if st is None:
                if self.default is not None:
                    deps.append((self.default, "raw"))
            else:
                if st[0] is not None:
                    deps.append((st[0], "raw"))
                if isinstance(k, tuple) and k[0] == "ps":
                    for r in st[1]:
                        if r.eng != eng:
                            deps.append((r, "war"))
        for k in writes:
            st = self.state.get(k)
            if st is None:
                if self.default is not None:
                    deps.append((self.default, "waw"))
            else:
                if st[0] is not None:
                    deps.append((st[0], "waw"))
                for r in st[1]:
                    deps.append((r, "war"))
        for k in reads:
            st = self.state.get(k)
            if st is None:
                st = self.state[k] = [self.default, []]
            st[1].append(o)
        for k in writes:
            self.state[k] = [o, []]
        o.waits = deps
        self.ops.append(o)
        return o

    def flush(self, final=False):
        nc = self.nc
        allkeys = list(self.state.keys())
        bt = self.bar_tile
        bar = self.op("dve", lambda: nc.vector.memset(bt[0:1, 0:8], 0.0), reads=(), writes=allkeys)
        bar.signal = True
        for o in self.ops:
            self.epos[o.eng] = self.epos.get(o.eng, 0) + 1
            o.pos = self.epos[o.eng]
        plan = []
        for o in self.ops:
            need = {}
            dneed = []
            for (p, kind) in o.waits:
                if p is o:
                    continue
                if p.dma:
                    s = self.dma_waited.setdefault(o.eng, set())
                    if p.uid not in s:
                        s.add(p.uid)
                        dneed.append(p)
                    continue
                if p.eng == o.eng:
                    if o.eng == "pe" or o.eng == "sp":
                        continue
                if p.pos <= self.waited.get((o.eng, p.eng), 0):
                    continue
                if p.eng not in need or need[p.eng].pos < p.pos:
                    need[p.eng] = p
            for pe_, p in need.items():
                self.waited[(o.eng, pe_)] = p.pos
                p.signal = True
            plan.append((list(need.values()), dneed))
        for o, (cw, dw) in zip(self.ops, plan):
            eng = self.eng_obj[o.eng]
            for p in cw:
                eng.wait_ge(self.esem[p.eng], p.sigval)
            for p in dw:
                eng.wait_ge(self.dsems[p.dsem], p.dval)
            if o.dma:
                if o.eng == "sp":
                    k = self.di_sp % self.nd_sp
                    self.di_sp += 1
                else:
                    k = self.nd_sp + self.di % (self.nd - self.nd_sp)
                    self.di += 1
                if self.dsem_val[k]:
                    eng.wait_ge(self.dsems[k], self.dsem_val[k])
                o.dsem = k
                self.dsem_val[k] += 16 * o.dma
                o.dval = self.dsem_val[k]
                o.fn(self.dsems[k])
            else:
                ins = o.fn()
                if o.signal:
                    self.ecount[o.eng] += 1
                    o.sigval = self.ecount[o.eng]
                    ins.then_inc(self.esem[o.eng], 1)
            self.n_inst += 1
        self.ops = []
        self.state = {}
        self.default = bar
        if final:
            for k in range(self.nd):
                if self.dsem_val[k]:
                    nc.sync.wait_ge(self.dsems[k], self.dsem_val[k])
            for e in COMPUTE:
                if e != "dve":
                    self.eng_obj[e].wait_ge(self.esem["dve"], bar.sigval)


M_QK = 256
SPLIT = (256, 256, 256, 256, 4, 4, 256, 256, 256, 512, 512, 512)
OFF = np.cumsum((0,) + SPLIT)
(O_MQ, O_MK, O_MV, O_MO, O_MI, O_MF, O_AQ, O_AK, O_AV, O_DQ, O_DK, O_DV, _) = OFF
NFM = 20
U_MQ, U_MK, U_MO, U_GF, U_GI, U_AQ, U_AK, U_DQ, U_DK = 0, 2, 4, 6, 7, 8, 10, 12, 16

SM_G = 0
SM_CONV = SM_G + L * 6 * 8
SM_BF = SM_CONV + L * 4 * 4
SM_BI = SM_BF + L
SM_GSUB = SM_BI + L
SM_LAM = SM_GSUB + L
SM_SEL4 = SM_LAM + L * 4 * 64
SM_C123 = SM_SEL4 + 4
NSM = SM_C123 + 3

CB_ID = 0
CB_TRI = 128
CB_ONES = 256
CB_ROWSEL = 384
NCB = CB_ROWSEL + 4 * 128


def _host_tables(inp):
    f = np.float32
    small = np.zeros((128, NSM), f)
    names = ["ffn1_pre_norm", "ffn1_post_norm", "mix_pre_norm", "mix_post_norm", "ffn2_pre_norm", "ffn2_post_norm"]
    for l in range(L):
        for n, nm in enumerate(names):
            small[:, SM_G + (l * 6 + n) * 8: SM_G + (l * 6 + n) * 8 + 8] = np.asarray(inp[nm][l], f).reshape(8, 128).T
        cq = np.asarray(inp["conv_qk"][l], f)
        for cc in range(4):
            small[:, SM_CONV + (l * 4 + cc) * 4: SM_CONV + (l * 4 + cc) * 4 + 4] = cq[:, cc * 128:(cc + 1) * 128].T
        for h in range(4):
            small[32 * h:32 * h + 3, SM_BF + l] = np.asarray(inp["fgate_bias"], f)[l, h]
            small[32 * h, SM_BI + l] = np.asarray(inp["igate_bias"], f)[l, h]
        small[:, SM_GSUB + l] = np.asarray(inp["diff_subln"][l], f)
        for v, nm in enumerate(["lambda_q1", "lambda_k1", "lambda_q2", "lambda_k2"]):
            small[:, SM_LAM + (l * 4 + v) * 64: SM_LAM + (l * 4 + v) * 64 + 64] = np.asarray(inp[nm][l], f)[None, :]
    for h in range(4):
        small[32 * h, SM_SEL4 + h] = 1.0
        small[32 * h, SM_C123 + 0] = 1.0
        small[32 * h + 1, SM_C123 + 1] = 1.0
        small[32 * h + 2, SM_C123 + 2] = 1.0
    cb = np.zeros((128, NCB), f)
    cb[:, CB_ID:CB_ID + 128] = np.eye(128, dtype=f)
    pk = np.arange(128)[:, None]
    pq = np.arange(128)[None, :]
    cb[:, CB_TRI:CB_TRI + 128] = np.where(pk > pq, NEG, 0.0)
    cb[:, CB_ONES:CB_ONES + 128] = 1.0
    for h in range(4):
        cb[32 * h:32 * h + 3, CB_ROWSEL + h * 128: CB_ROWSEL + (h + 1) * 128] = 1.0
    t = np.arange(S)
    thi = (t // 128 * 128).astype(f)
    tlo = (t % 128).astype(f)
    qrows = np.stack([np.ones(S, f), np.ones(S, f), thi, tlo]).astype(f)
    slopes = np.exp2(-8.0 * np.arange(1, 9, dtype=np.float64) / 8).astype(f)
    head_slopes = list(slopes[0::2]) + list(slopes[1::2])
    krows = np.zeros((8, 4, S), f)
    for i, sg in enumerate(head_slopes):
        krows[i, 0] = sg * thi
        krows[i, 1] = sg * tlo
        krows[i, 2] = -sg
        krows[i, 3] = -sg
    blockind = (t[None, :] // 256 == np.arange(8)[:, None]).astype(f)
    return small, cb, qrows, krows, blockind, np.eye(128, dtype=f)


def _host_weights(inp):
    f = np.float32
    out = {}
    for k, (g, u, d) in enumerate([("ffn1_w_gate", "ffn1_w_up", "ffn1_w_down"), ("ffn2_w_gate", "ffn2_w_up", "ffn2_w_down")]):
        wg = np.asarray(inp[g], f).reshape(L, 8, 128, 11, 256).transpose(0, 3, 2, 1, 4)
        wu = np.asarray(inp[u], f).reshape(L, 8, 128, 11, 256).transpose(0, 3, 2, 1, 4)
        wd = np.asarray(inp[d], f).reshape(L, 11, 2, 128, 1024).transpose(0, 1, 3, 2, 4)
        out["wg%d" % k] = np.ascontiguousarray(wg).reshape(L, 11, 128, 2048)
        out["wu%d" % k] = np.ascontiguousarray(wu).reshape(L, 11, 128, 2048)
        out["wd%d" % k] = np.ascontiguousarray(wd).reshape(L, 11, 128, 2048)
    w_in = np.asarray(inp["w_in"], f)
    fm = np.zeros((L, NFM, 1024, 128), f)
    for l in range(L):
        w = w_in[l]
        for i in range(2):
            fm[l, U_MQ + i] = w[:, O_MQ + i * 128: O_MQ + (i + 1) * 128]
            fm[l, U_MK + i] = w[:, O_MK + i * 128: O_MK + (i + 1) * 128]
            fm[l, U_MO + i] = w[:, O_MO + i * 128: O_MO + (i + 1) * 128]
            fm[l, U_AQ + i] = w[:, O_AQ + i * 128: O_AQ + (i + 1) * 128]
            fm[l, U_AK + i] = w[:, O_AK + i * 128: O_AK + (i + 1) * 128]
        for h in range(4):
            for r in range(3):
                fm[l, U_GF, :, 32 * h + r] = w[:, O_MF + h]
            fm[l, U_GI, :, 32 * h] = w[:, O_MI + h]
            fm[l, U_DQ + h] = w[:, O_DQ + h * 128: O_DQ + (h + 1) * 128]
            fm[l, U_DK + h] = w[:, O_DK + h * 128: O_DK + (h + 1) * 128]
    out["win_fm"] = np.ascontiguousarray(fm.reshape(L, NFM, 8, 128, 128).transpose(0, 1, 3, 2, 4)).reshape(L, NFM, 128, 1024)
    wv = np.concatenate([w_in[:, :, O_MV:O_MV + 256], w_in[:, :, O_AV:O_AV + 256], w_in[:, :, O_DV:O_DV + 512]], axis=2)
    out["win_v"] = np.ascontiguousarray(wv.reshape(L, 8, 128, 1024).transpose(0, 2, 1, 3)).reshape(L, 128, 8192)
    wo = np.asarray(inp["w_out"], f).reshape(L, 8, 128, 8, 128).transpose(0, 3, 2, 1, 4)
    out["wo"] = np.ascontiguousarray(wo).reshape(L, 8, 128, 1024)
    return out


def build_program(debug=False, nseq=SEQ_PER_CORE, nlayers=L, stop=None):
    nc = bass.Bass("TRN2", target_bir_lowering=False)
    dt_in = lambda name, shape: nc.dram_tensor(name, list(shape), F32, kind="ExternalInput").ap()
    x_d = dt_in("x", (SEQ_PER_CORE, S, D))
    small_d = dt_in("small", (128, NSM))
    cb_d = dt_in("cb", (128, NCB))
    qrows_d = dt_in("qrows", (4, S))
    krows_d = dt_in("krows", (8, 4, S))
    bind_d = dt_in("blockind", (8, S))
    idf_d = dt_in("identf", (128, 128))
    wg_d = [dt_in("wg%d" % k, (L, 11, 128, 2048)) for k in range(2)]
    wu_d = [dt_in("wu%d" % k, (L, 11, 128, 2048)) for k in range(2)]
    wd_d = [dt_in("wd%d" % k, (L, 11, 128, 2048)) for k in range(2)]
    winfm_d = dt_in("win_fm", (L, NFM, 128, 1024))
    winv_d = dt_in("win_v", (L, 128, 8192))
    wo_d = dt_in("wo", (L, 8, 128, 1024))
    out_d = nc.dram_tensor("out", [SEQ_PER_CORE, S, D], F32, kind="ExternalOutput").ap()
    dbg_d = None
    if debug:
        dbg_d = nc.dram_tensor("dbg", [8, 128, 8 * S], F32, kind="ExternalOutput").ap()

    with contextlib.ExitStack() as top:
        P = Prog(nc, top)
        _cnt = [0]

        def sbt(es, name, shape, dt):
            _cnt[0] += 1
            return es.enter_context(nc.sbuf_tensor("%s_%d" % (name, _cnt[0]), shape, dt))
        ps = [top.enter_context(nc.psum_tensor("psb%d" % i, [128, 512], F32)) for i in range(8)]
        pools = {}

        def bank(pool, banks):
            i = pools.get(pool, 0)
            pools[pool] = i + 1
            return banks[i % len(banks)]

        def PK(b):
            return ("ps", b)

        xT = sbt(top, "xT", [128, 8, S], F32)
        small = sbt(top, "small", [128, NSM], F32)
        cb = sbt(top, "cbf", [128, NCB], BF16)
        identf = sbt(top, "identf", [128, 128], F32)
        ghalf = sbt(top, "ghalf", [128, L * 2 * 8], F32)
        lamt = sbt(top, "lamt", [128, 8 * L], F32)
        ident_bf = cb[:, CB_ID:CB_ID + 128]
        tri_bf = cb[:, CB_TRI:CB_TRI + 128]
        ones_bf = cb[:, CB_ONES:CB_ONES + 128]

        def X(c, blk):
            return ("x", c, blk)

        P.op("sp", lambda s: nc.sync.dma_start(out=small[:], in_=small_d).then_inc(s, 16), writes=["small"], dma=1)
        P.op("sp", lambda s: nc.sync.dma_start(out=identf[:], in_=idf_d).then_inc(s, 16), writes=["identf"], dma=1)
        P.op("pool", lambda s: nc.gpsimd.dma_start(out=cb[:], in_=cb_d).then_inc(s, 16), writes=["cb"], dma=1)
        for l in range(L):
            for k, n in enumerate((1, 5)):
                src = small[:, SM_G + (l * 6 + n) * 8: SM_G + (l * 6 + n) * 8 + 8]
                dst = ghalf[:, (l * 2 + k) * 8:(l * 2 + k) * 8 + 8]
                P.op("dve", lambda src=src, dst=dst: nc.vector.tensor_scalar(dst, src, 0.5, None, op0=ALU.mult),
                     reads=["small"], writes=["ghalf"])
        with contextlib.ExitStack() as es0:
            ltmp = sbt(es0, "ltmp", [128, 64], F32)
            lsum = sbt(es0, "lsum", [128, 4], F32)
            for l in range(L):
                lam_init = 0.8 - 0.6 * math.exp(-0.3 * l)
                for j in range(2):
                    a = small[:, SM_LAM + (l * 4 + 2 * j) * 64: SM_LAM + (l * 4 + 2 * j) * 64 + 64]
                    b = small[:, SM_LAM + (l * 4 + 2 * j + 1) * 64: SM_LAM + (l * 4 + 2 * j + 1) * 64 + 64]
                    P.op("dve", lambda a=a, b=b: nc.vector.tensor_tensor(ltmp[:], a, b, ALU.mult), reads=["small", "ltmp"], writes=["ltmp"])
                    P.op("dve", lambda j=j: nc.vector.reduce_sum(out=lsum[:, j:j + 1], in_=ltmp[:], axis=AX.X), reads=["ltmp"], writes=["lsum"])
                P.op("act", lambda: nc.scalar.activation(lsum[:, 2:4], lsum[:, 0:2], AF.Exp), reads=["lsum"], writes=["lsum"])
                P.op("dve", lambda l=l, li=lam_init: nc.vector.scalar_tensor_tensor(
                    out=lamt[:, 8 * l:8 * l + 1], in0=lsum[:, 3:4], scalar=-li, in1=lsum[:, 2:3], op0=ALU.add, op1=ALU.subtract),
                    reads=["lsum"], writes=["lamt"])
                P.op("dve", lambda l=l, li=lam_init: nc.vector.tensor_scalar(
                    lamt[:, 8 * l + 1:8 * l + 2], small[:, SM_GSUB + l:SM_GSUB + l + 1], 1.0 - li, None, op0=ALU.mult),
                    reads=["small", "lamt"], writes=["lamt"])
            P.flush()

        def rms_stats(es, tag, src_fn, src_keys, nchunk, ncols, inv_n, sq_tile, stat_tile, bankset):
            b = bank("gu", bankset)
            for c in range(nchunk):
                P.op("act", lambda c=c: nc.scalar.activation(sq_tile[:, c % 2, :ncols], src_fn(c), AF.Square),
                     reads=[src_keys(c)], writes=[(tag + "sq", c % 2)])
                P.op("pe", lambda c=c, b=b: nc.tensor.matmul(ps[b][:, :ncols], ones_bf, sq_tile[:, c % 2, :ncols],
                                                            start=(c == 0), stop=(c == nchunk - 1)),
                     reads=[(tag + "sq", c % 2), "cb"], writes=[PK(b)])
            P.op("act", lambda b=b: nc.scalar.activation(stat_tile[:, :ncols], ps[b][:, :ncols], AF.Ln, bias=EPS, scale=inv_n),
                 reads=[PK(b)], writes=[tag + "stat"])
            P.op("act", lambda: nc.scalar.activation(stat_tile[:, :ncols], stat_tile[:, :ncols], AF.Exp, scale=-0.5),
                 reads=[tag + "stat"], writes=[tag + "stat"])

        def dump2(idx, c, ap2, n):
            P.op("pool", lambda s: nc.gpsimd.dma_start(out=dbg_d[idx, 0:ap2.shape[0], c * S:c * S + n], in_=ap2).then_inc(s, 16), reads=["__dump"], dma=1)

        def dump(idx, src3, keys):
            if dbg_d is None:
                return
            for c in range(8):
                P.op("pool", lambda s, c=c: nc.gpsimd.dma_start(out=dbg_d[idx, :, c * S:(c + 1) * S], in_=src3[:, c, :]).then_inc(s, 16),
                     reads=list(keys) + ["__dump"], dma=1)

        for sq in range(nseq):
            with contextlib.ExitStack() as es:
                xin = sbt(es, "xin", [128, 2, D], F32)
                for tt in range(NT):
                    P.op("sp", lambda s, tt=tt: nc.sync.dma_start(out=xin[:, tt % 2, :], in_=x_d[sq, tt * 128:(tt + 1) * 128, :]).then_inc(s, 16),
                         writes=[("xin", tt % 2)], dma=1)
                    for c in range(8):
                        b = bank("tr", [0, 1, 2, 3])
                        P.op("pe", lambda tt=tt, c=c, b=b: nc.tensor.transpose(ps[b][:, 0:128], xin[:, tt % 2, c * 128:(c + 1) * 128], identf[:]),
                             reads=[("xin", tt % 2), "identf"], writes=[PK(b)])
                        eng = "act" if c % 2 else "dve"
                        if eng == "act":
                            P.op("act", lambda tt=tt, c=c, b=b: nc.scalar.copy(xT[:, c, tt * 128:(tt + 1) * 128], ps[b][:, 0:128]),
                                 reads=[PK(b)], writes=[("xtile", c, tt)])
                        else:
                            P.op("dve", lambda tt=tt, c=c, b=b: nc.vector.tensor_copy(xT[:, c, tt * 128:(tt + 1) * 128], ps[b][:, 0:128]),
                                 reads=[PK(b)], writes=[("xtile", c, tt)])
                P.flush()

            for l in range(nlayers if stop != "load" else 0):
                for ffn_i in range(2):
                    if stop in ("ffn1", "mlstm_prep", "mlstm", "moba", "diff") and ffn_i == 1:
                        continue
                    with contextlib.ExitStack() as es:
                        hT = sbt(es, "f_hT", [128, 1, 8, 1024], BF16)
                        yb = sbt(es, "f_y", [128, 2, 8, 1024], F32)
                        wgt = sbt(es, "f_wg", [128, 2, 2048], BF16)
                        wut = sbt(es, "f_wu", [128, 2, 2048], BF16)
                        wdt = sbt(es, "f_wd", [128, 2, 2048], BF16)
                        At = sbt(es, "f_A", [128, 2, 2, 1024], BF16)
                        sgt = sbt(es, "f_sg", [128, 2, 512], F32)
                        sqt = sbt(es, "f_sq", [128, 8, 512], BF16)
                        rst = sbt(es, "f_rs", [128, 2, 512], F32)
                        tmp = sbt(es, "f_tmp", [128, 2, 512], F32)
                        n_pre = 0 if ffn_i == 0 else 4
                        gpre = lambda c: small[:, SM_G + (l * 6 + n_pre) * 8 + c: SM_G + (l * 6 + n_pre) * 8 + c + 1]
                        gpost = lambda c: ghalf[:, (l * 2 + ffn_i) * 8 + c:(l * 2 + ffn_i) * 8 + c + 1]

                        def n_src(kind, half, blk):
                            cs = slice(half * 1024 + blk * 512, half * 1024 + (blk + 1) * 512)
                            if kind == "pre":
                                return (lambda c: xT[:, c, cs]), (lambda c: X(c, half * 2 + blk))
                            return (lambda c: yb[:, half, c, blk * 512:(blk + 1) * 512]), (lambda c: ("fy", half, c, blk))

                        def n_sq(kind, half, blk):
                            src, key = n_src(kind, half, blk)
                            for c in range(8):
                                P.op("act", lambda c=c, src=src: nc.scalar.activation(sqt[:, c, :], src(c), AF.Square),
                                     reads=[key(c)], writes=[("fsq", c)])

                        def n_stat(r):
                            b = bank("gu", [0, 1, 2, 3])
                            for c in range(8):
                                P.op("pe", lambda c=c, b=b: nc.tensor.matmul(ps[b][:, :], ones_bf, sqt[:, c, :], start=(c == 0), stop=(c == 7)),
                                     reads=[("fsq", c), "cb"], writes=[PK(b)])
                            P.op("act", lambda b=b: nc.scalar.activation(rst[:, r, :], ps[b][:, :], AF.Ln, bias=EPS, scale=1.0 / D),
                                 reads=[PK(b)], writes=[("frs", r)])
                            P.op("act", lambda: nc.scalar.activation(rst[:, r, :], rst[:, r, :], AF.Exp, scale=-0.5),
                                 reads=[("frs", r)], writes=[("frs", r)])

                        def n_apply(kind, half, blk, r):
                            cs = slice(half * 1024 + blk * 512, half * 1024 + (blk + 1) * 512)
                            for c in range(8):
                                if kind == "pre":
                                    P.op("dve", lambda c=c: nc.vector.scalar_tensor_tensor(
                                        out=hT[:, 0, c, blk * 512:(blk + 1) * 512], in0=xT[:, c, cs], scalar=gpre(c), in1=rst[:, r, :],
                                        op0=ALU.mult, op1=ALU.mult),
                                        reads=[X(c, half * 2 + blk), ("frs", r), "small"], writes=[("fh", 0, c, blk)])
                                else:
                                    ti = c % 2
                                    P.op("dve", lambda c=c, ti=ti: nc.vector.scalar_tensor_tensor(
                                        out=tmp[:, ti, :], in0=yb[:, half, c, blk * 512:(blk + 1) * 512], scalar=gpost(c), in1=rst[:, r, :],
                                        op0=ALU.mult, op1=ALU.mult), reads=[("fy", half, c, blk), ("frs", r), "ghalf"], writes=[("ftmp", ti)])
                                    if c % 2:
                                        P.op("pool", lambda c=c, ti=ti: nc.gpsimd.tensor_tensor(xT[:, c, cs], xT[:, c, cs], tmp[:, ti, :], ALU.add),
                                             reads=[("ftmp", ti), X(c, half * 2 + blk)], writes=[X(c, half * 2 + blk)])
                                    else:
                                        P.op("dve", lambda c=c, ti=ti: nc.vector.tensor_tensor(xT[:, c, cs], xT[:, c, cs], tmp[:, ti, :], ALU.add),
                                             reads=[("ftmp", ti), X(c, half * 2 + blk)], writes=[X(c, half * 2 + blk)])

                        def load_group(gidx):
                            fg, slot = gidx % 11, gidx % 2
                            P.op("pool", lambda s, fg=fg, slot=slot: nc.gpsimd.dma_start(out=wgt[:, slot, :], in_=wg_d[ffn_i][l, fg]).then_inc(s, 16),
                                 writes=[("fwg", slot)], dma=1)
                            P.op("pool", lambda s, fg=fg, slot=slot: nc.gpsimd.dma_start(out=wut[:, slot, :], in_=wu_d[ffn_i][l, fg]).then_inc(s, 16),
                                 writes=[("fwu", slot)], dma=1)
                            P.op("pool", lambda s, fg=fg, slot=slot: nc.gpsimd.dma_start(out=wdt[:, slot, :], in_=wd_d[ffn_i][l, fg]).then_inc(s, 16),
                                 writes=[("fwd", slot)], dma=1)

                        def compute_group(gidx):
                            half, fg = divmod(gidx, 11)
                            slot = gidx % 2
                            for blk in range(2):
                                for j in range(2):
                                    bg = bank("gu", [0, 1, 2, 3])
                                    bu = bank("gu", [0, 1, 2, 3])
                                    for c in range(8):
                                        P.op("pe", lambda c=c, j=j, blk=blk, bg=bg: nc.tensor.matmul(
                                            ps[bg][:, :], wgt[:, slot, c * 256 + j * 128: c * 256 + (j + 1) * 128], hT[:, 0, c, blk * 512:(blk + 1) * 512],
                                            start=(c == 0), stop=(c == 7)), reads=[("fwg", slot), ("fh", 0, c, blk)], writes=[PK(bg)])
                                    for c in range(8):
                                        P.op("pe", lambda c=c, j=j, blk=blk, bu=bu: nc.tensor.matmul(
                                            ps[bu][:, :], wut[:, slot, c * 256 + j * 128: c * 256 + (j + 1) * 128], hT[:, 0, c, blk * 512:(blk + 1) * 512],
                                            start=(c == 0), stop=(c == 7)), reads=[("fwu", slot), ("fh", 0, c, blk)], writes=[PK(bu)])
                                    si = bank("sg", [0, 1])
                                    P.op("act", lambda bg=bg, si=si: nc.scalar.activation(sgt[:, si, :], ps[bg][:, :], AF.Silu),
                                         reads=[PK(bg)], writes=[("fsg", si)])
                                    P.op("dve", lambda bu=bu, si=si, j=j, blk=blk: nc.vector.tensor_tensor(
                                        At[:, slot, j, blk * 512:(blk + 1) * 512], ps[bu][:, :], sgt[:, si, :], ALU.mult),
                                        reads=[PK(bu), ("fsg", si)], writes=[("fA", slot, j, blk)])
                            for blk in range(2):
                                for dm in range(8):
                                    by = bank("yp", [4, 5, 6, 7])
                                    for j in range(2):
                                        P.op("pe", lambda j=j, dm=dm, blk=blk, by=by: nc.tensor.matmul(
                                            ps[by][:, :], wdt[:, slot, j * 1024 + dm * 128: j * 1024 + (dm + 1) * 128],
                                            At[:, slot, j, blk * 512:(blk + 1) * 512], start=(j == 0), stop=(j == 1)),
                                            reads=[("fwd", slot), ("fA", slot, j, blk)], writes=[PK(by)])
                                    ysl = yb[:, half, dm, blk * 512:(blk + 1) * 512]
                                    if fg == 0:
                                        P.op("act", lambda by=by, ysl=ysl: nc.scalar.copy(ysl, ps[by][:, :]),
                                             reads=[PK(by)], writes=[("fy", half, dm, blk)])
                                    else:
                                        P.op("dve", lambda by=by, ysl=ysl: nc.vector.tensor_tensor(ysl, ps[by][:, :], ysl, ALU.add),
                                             reads=[PK(by), ("fy", half, dm, blk)], writes=[("fy", half, dm, blk)])

                        for blk in range(2):
                            n_sq("pre", 0, blk)
                            n_stat(blk)
                            n_apply("pre", 0, blk, blk)
                        hooks = {12: [lambda: n_sq("post", 0, 0)],
                                 13: [lambda: n_stat(0), lambda: n_apply("post", 0, 0, 0), lambda: n_sq("post", 0, 1)],
                                 14: [lambda: n_stat(1), lambda: n_apply("post", 0, 1, 1)]}
                        load_group(0)
                        for gidx in range(22):
                            if gidx + 1 < 22:
                                load_group(gidx + 1)
                            if gidx == 11:
                                for blk in range(2):
                                    n_sq("pre", 1, blk)
                                    n_stat(blk)
                                    n_apply("pre", 1, blk, blk)
                            compute_group(gidx)
                            for hk in hooks.get(gidx, ()):
                                hk()
                        for blk in range(2):
                            n_sq("post", 1, blk)
                            n_stat(blk)
                            n_apply("post", 1, blk, blk)
                        P.flush()
                    if debug and sq == 0:
                        dump(l * 3 + (0 if ffn_i == 0 else 2), xT[:, :, :], [X(c, b) for c in range(8) for b in range(4)])
                        P.flush()
                    if ffn_i == 1 or stop == "ffn1":
                        continue
                    mixer(nc, P, top, sbt, ps, bank, PK, X, xT, small, cb, identf, lamt, l, sq,
                          winfm_d, winv_d, wo_d, qrows_d, krows_d, bind_d, rms_stats, dump if (debug and sq == 0) else None, stop, dump2)
                    if debug and sq == 0:
                        dump(l * 3 + 1, xT[:, :, :], [X(c, b) for c in range(8) for b in range(4)])
                        P.flush()

            with contextlib.ExitStack() as es:
                xo = sbt(es, "xo", [128, 2, D], F32)
                for tt in range(NT):
                    for c in range(8):
                        b = bank("tr", [0, 1, 2, 3])
                        P.op("pe", lambda tt=tt, c=c, b=b: nc.tensor.transpose(ps[b][:, 0:128], xT[:, c, tt * 128:(tt + 1) * 128], identf[:]),
                             reads=[X(c, tt // 4), "identf"], writes=[PK(b)])
                        if c % 2:
                            P.op("act", lambda tt=tt, c=c, b=b: nc.scalar.copy(xo[:, tt % 2, c * 128:(c + 1) * 128], ps[b][:, 0:128]),
                                 reads=[PK(b)], writes=[("xo", tt % 2, c)])
                        else:
                            P.op("dve", lambda tt=tt, c=c, b=b: nc.vector.tensor_copy(xo[:, tt % 2, c * 128:(c + 1) * 128], ps[b][:, 0:128]),
                                 reads=[PK(b)], writes=[("xo", tt % 2, c)])
                    P.op("sp", lambda s, tt=tt: nc.sync.dma_start(out=out_d[sq, tt * 128:(tt + 1) * 128, :], in_=xo[:, tt % 2, :]).then_inc(s, 16),
                         reads=[("xo", tt % 2, c) for c in range(8)], dma=1)
                P.flush(final=(sq == nseq - 1))
    return nc


def mixer(nc, P, top, sbt, ps, bank, PK, X, xT, small, cb, identf, lamt, l, sq,
          winfm_d, winv_d, wo_d, qrows_d, krows_d, bind_d, rms_stats, dump, stop=None, dump2=None):
    ident_bf = cb[:, CB_ID:CB_ID + 128]
    tri_bf = cb[:, CB_TRI:CB_TRI + 128]
    ones_bf = cb[:, CB_ONES:CB_ONES + 128]
    with contextlib.ExitStack() as mes:
        hT = sbt(mes, "m_hT", [128, 8, S], BF16)
        mixT = sbt(mes, "m_mix", [128, 8, S], BF16)
        wsl = sbt(mes, "m_wsl", [128, 3, 1024], BF16)
        wv = sbt(mes, "m_wv", [128, 8, 512], BF16)
        pt = sbt(mes, "m_pt", [128, 3, 512], BF16)
        sqt = sbt(mes, "m_sq", [128, 2, 512], BF16)
        rst = sbt(mes, "m_rs", [128, 512], F32)
        H = lambda c, blk: ("mh", c, blk)
        MX = lambda c, blk: ("mix", c, blk)
        gpre = lambda c: small[:, SM_G + (l * 6 + 2) * 8 + c: SM_G + (l * 6 + 2) * 8 + c + 1]
        gpost = lambda c: small[:, SM_G + (l * 6 + 3) * 8 + c: SM_G + (l * 6 + 3) * 8 + c + 1]

        for blk in range(NBLK):
            cs = slice(blk * 512, (blk + 1) * 512)
            rms_stats(mes, "m", lambda c, cs=cs: xT[:, c, cs], lambda c, blk=blk: X(c, blk), 8, 512, 1.0 / D, sqt, rst, [0, 1, 2, 3])
            for c in range(8):
                P.op("dve", lambda c=c, cs=cs: nc.vector.scalar_tensor_tensor(
                    out=hT[:, c, cs], in0=xT[:, c, cs], scalar=gpre(c), in1=rst[:, :], op0=ALU.mult, op1=ALU.mult),
                    reads=[X(c, blk), "mstat", "small"], writes=[H(c, blk)])

        def load_slab(u):
            si = bank("wsl", [0, 1, 2])
            P.op("pool", lambda s, u=u, si=si: nc.gpsimd.dma_start(out=wsl[:, si, :], in_=winfm_d[l, u]).then_inc(s, 16),
                 writes=[("wsl", si)], dma=1)
            return si

        def proj_fm(si, blk, b, M=128):
            for c in range(8):
                P.op("pe", lambda c=c: nc.tensor.matmul(ps[b][0:M, :], wsl[:, si, c * 128: c * 128 + M], hT[:, c, blk * 512:(blk + 1) * 512],
                                                        start=(c == 0), stop=(c == 7)),
                     reads=[("wsl", si), H(c, blk)], writes=[PK(b)])

        def proj_v(voff, ncol, dst_fn, es):
            for c in range(8):
                P.op("pool", lambda s, c=c: nc.gpsimd.dma_start(out=wv[:, c, 0:ncol], in_=winv_d[l, :, c * 1024 + voff: c * 1024 + voff + ncol]).then_inc(s, 16),
                     writes=[("wv", c)], dma=1)
            for tt in range(NT):
                b = bank("pj", [0, 1, 2, 3])
                for c in range(8):
                    P.op("pe", lambda c=c, tt=tt, b=b: nc.tensor.matmul(ps[b][:, 0:ncol], hT[:, c, tt * 128:(tt + 1) * 128], wv[:, c, 0:ncol],
                                                                      start=(c == 0), stop=(c == 7)),
                         reads=[("wv", c), H(c, tt // 4)], writes=[PK(b)])
                dst_fn(tt, b)

        def attn_pair(streams, pv_fn, final_fn, score_fn, look=2):
            steps = []
            for J in range(NBLK):
                ni = 4 * J + 4
                for i in range(ni):
                    c0 = max(0, i - 4 * J) * 128
                    for st in streams:
                        steps.append((st, i, J, c0, i == 0, i == ni - 1))
            ptis = []
            for k in range(len(steps)):
                while len(ptis) < min(len(steps), k + look + 1):
                    st, i, J, c0, f_, l_ = steps[len(ptis)]
                    ptis.append(score_fn(st, i, J, c0))
                st, i, J, c0, f_, l_ = steps[k]
                pv_fn(st, i, J, c0, ptis[k], f_, l_)
                if l_ and st == streams[-1]:
                    final_fn(J)

        def diag_mask(b, c0, i, J):
            if i >= 4 * J:
                P.op("pe", lambda: nc.tensor.matmul(ps[b][:, c0:c0 + 128], ident_bf, tri_bf, start=False, stop=True),
                     reads=["cb"], writes=[PK(b)])

        with contextlib.ExitStack() as es:
            Fs = sbt(es, "fs", [128, S], BF16)
            Qm = sbt(es, "qm", [128, 2, S], BF16)
            Km = sbt(es, "km", [128, 2, S], BF16)
            bcol = sbt(es, "bcol", [128, 68], F32)
            eM = sbt(es, "eM", [128, 4], F32)
            Mx = sbt(es, "Mx", [128, 2], F32)
            Mrep = sbt(es, "Mrep", [128, 128], F32)
            ges = contextlib.ExitStack()
            G0 = sbt(ges, "g0", [128, S + 3], F32)
            G1 = sbt(ges, "g1", [128, S + 3], F32)
            G2 = sbt(ges, "g2", [128, S + 3], F32)
            B0 = sbt(ges, "b0", [128, S], BF16)
            B1 = sbt(ges, "b1", [128, S], BF16)
            nbf = small[:, SM_BF + l:SM_BF + l + 1]
            sgf = load_slab(U_GF)
            for blk in range(NBLK):
                b = bank("pj", [0, 1, 2, 3])
                proj_fm(sgf, blk, b)
                P.op("dve", lambda: nc.vector.tensor_scalar(Mx[:, 1:2], nbf, -1.0, None, op0=ALU.mult), reads=["small"], writes=["negbf"])
                P.op("act", lambda b=b, blk=blk: nc.scalar.activation(G0[:, blk * 512:(blk + 1) * 512], ps[b][:, :], AF.Exp, bias=Mx[:, 1:2], scale=-1.0),
                     reads=[PK(b), "negbf"], writes=[("G0", blk)])
            P.op("act", lambda: nc.scalar.activation(G0[:, 0:S], G0[:, 0:S], AF.Ln, bias=1.0),
                 reads=[("G0", k) for k in range(4)], writes=[("G0", k) for k in range(4)])
            P.op("dve", lambda: nc.vector.memset(G2[:, 0:S], 1.0), writes=["G2"])
            P.op("dve", lambda: nc.vector.tensor_tensor_scan(G1[:, 0:S], G2[:, 0:S], G0[:, 0:S], 0.0, ALU.mult, ALU.add),
                 reads=["G2"] + [("G0", k) for k in range(4)], writes=["G1"])
            sgi = load_slab(U_GI)
            bi = small[:, SM_BI + l:SM_BI + l + 1]
            for blk in range(NBLK):
                b = bank("pj", [0, 1, 2, 3])
                proj_fm(sgi, blk, b)
                P.op("act", lambda b=b, blk=blk: nc.scalar.activation(G0[:, blk * 512:(blk + 1) * 512], ps[b][:, :], AF.Identity, bias=bi, scale=1.0),
                     reads=[PK(b), "small", "G1"], writes=[("G0", blk)])
            P.op("dve", lambda: nc.vector.reduce_max(out=Mx[:, 0:1], in_=G0[:, 0:S], axis=AX.X),
                 reads=[("G0", k) for k in range(4)], writes=["Mx"])
            P.op("dve", lambda: nc.vector.scalar_tensor_tensor(out=G2[:, 0:S], in0=G0[:, 0:S], scalar=Mx[:, 0:1], in1=G1[:, 0:S],
                                                               op0=ALU.subtract, op1=ALU.add),
                 reads=[("G0", k) for k in range(4)] + ["Mx", "G1", "G2"], writes=["G2"])
            sel4 = small[:, SM_SEL4:SM_SEL4 + 4]
            bb = bank("pj", [0, 1, 2, 3])
            for tt in range(NT):
                P.op("pe", lambda tt=tt: nc.tensor.matmul(ps[bb][:, tt * 4:tt * 4 + 4], G2[:, tt * 128:(tt + 1) * 128], sel4, start=True, stop=True),
                     reads=["G2", "small"], writes=[PK(bb)])
            P.op("dve", lambda: nc.vector.memset(Mrep[:], 0.0), writes=["Mrep"])
            P.op("dve", lambda: nc.vector.tensor_scalar(Mrep[:], Mrep[:], Mx[:, 0:1], None, op0=ALU.add), reads=["Mrep", "Mx"], writes=["Mrep"])
            P.op("pe", lambda: nc.tensor.matmul(ps[bb][:, 64:68], Mrep[:], sel4, start=True, stop=True), reads=["Mrep", "small"], writes=[PK(bb)])
            P.op("dve", lambda: nc.vector.tensor_copy(bcol[:, 0:68], ps[bb][:, 0:68]), reads=[PK(bb)], writes=["bcol"])
            P.op("act", lambda: nc.scalar.activation(eM[:, 0:4], bcol[:, 64:68], AF.Exp, scale=-1.0), reads=["bcol"], writes=["eM"])
            c1 = small[:, SM_C123:SM_C123 + 1]
            c2 = small[:, SM_C123 + 1:SM_C123 + 2]
            c3 = small[:, SM_C123 + 2:SM_C123 + 3]
            P.op("dve", lambda: nc.vector.tensor_scalar(B0[:], G1[:, 0:S], -1.0, None, op0=ALU.mult), reads=["G1"], writes=["B0"])
            P.op("dve", lambda: nc.vector.scalar_tensor_tensor(out=G0[:, 0:S], in0=G1[:, 0:S], scalar=-1.0, in1=B0[:], op0=ALU.mult, op1=ALU.subtract),
                 reads=["G1", "B0", "G2"] + [("G0", k) for k in range(4)], writes=[("G0", k) for k in range(4)])
            P.op("dve", lambda: nc.vector.tensor_copy(B1[:], G0[:, 0:S]), reads=[("G0", k) for k in range(4)], writes=["B1"])
            P.op("dve", lambda: nc.vector.tensor_scalar(Fs[:], B0[:], c1, None, op0=ALU.mult), reads=["B0", "small"], writes=["Fs"])
            P.op("dve", lambda: nc.vector.scalar_tensor_tensor(out=Fs[:], in0=B1[:], scalar=c2, in1=Fs[:], op0=ALU.mult, op1=ALU.add),
                 reads=["B1", "Fs", "small"], writes=["Fs"])
            P.op("dve", lambda: nc.vector.tensor_tensor(G0[:, 0:S], G0[:, 0:S], B1[:], ALU.subtract),
                 reads=[("G0", k) for k in range(4)] + ["B1"], writes=[("G0", k) for k in range(4)])
            P.op("dve", lambda: nc.vector.tensor_copy(B0[:], G0[:, 0:S]), reads=[("G0", k) for k in range(4)] + ["Fs"], writes=["B0"])
            P.op("dve", lambda: nc.vector.scalar_tensor_tensor(out=Fs[:], in0=B0[:], scalar=c3, in1=Fs[:], op0=ALU.mult, op1=ALU.add),
                 reads=["B0", "Fs", "small"], writes=["Fs"])
            P.op("dve", lambda: nc.vector.memset(G0[:, 0:3], 0.0), reads=["B0"] + [("G0", k) for k in range(4)], writes=["G0pad"])
            for cc in range(4):
                su = load_slab((U_MQ if cc < 2 else U_MK) + cc % 2)
                for blk in range(NBLK):
                    b = bank("pj", [0, 1, 2, 3])
                    proj_fm(su, blk, b)
                    P.op("act", lambda b=b, blk=blk: nc.scalar.copy(G0[:, 3 + blk * 512: 3 + (blk + 1) * 512], ps[b][:, :]),
                         reads=[PK(b), "G0pad"], writes=[("G0", blk)])
                wc = lambda j, cc=cc: small[:, SM_CONV + (l * 4 + cc) * 4 + j: SM_CONV + (l * 4 + cc) * 4 + j + 1]
                allg0 = [("G0", k) for k in range(4)] + ["G0pad"]
                P.op("dve", lambda wc=wc: nc.vector.tensor_scalar(G2[:, 0:S], G0[:, 0:S], wc(0), None, op0=ALU.mult),
                     reads=allg0 + ["small", "G2"], writes=["G2"])
                for j in (1, 2, 3):
                    P.op("dve", lambda wc=wc, j=j: nc.vector.scalar_tensor_tensor(out=G2[:, 0:S], in0=G0[:, j:j + S], scalar=wc(j), in1=G2[:, 0:S],
                                                                                  op0=ALU.mult, op1=ALU.add),
                         reads=allg0 + ["small", "G2"], writes=["G2"])
                dstt = Qm if cc < 2 else Km
                P.op("act", lambda dstt=dstt, cc=cc: nc.scalar.activation(dstt[:, cc % 2, :], G2[:, 0:S], AF.Silu),
                     reads=["G2"], writes=[("qk", cc)])
            P.flush()
            if dump is not None:
                dump2(3, 0, Fs[:, :], S)
                dump2(3, 1, G1[:, 0:S], S)
                dump2(3, 2, bcol[:, 0:68], 68)
                dump2(3, 3, eM[:, 0:4], 4)
                dump2(4, 0, Qm[:, 0, :], S)
                dump2(4, 1, Qm[:, 1, :], S)
                dump2(4, 2, Km[:, 0, :], S)
                dump2(4, 3, Km[:, 1, :], S)
                P.flush()
            ges.close()
            if stop == "mlstm_prep":
                return
            Vb = sbt(es, "m_V", [128, NT, 512], BF16)
            wt = sbt(es, "wt", [128, 3, 512], F32)
            ft = sbt(es, "ft", [128, 4, 512], F32)
            P.op("dve", lambda: nc.vector.memset(Vb[:, :, :].rearrange("p t (q c) -> p t q c", q=2)[:, :, :, 64:192], 1.0),
                 writes=[("V", tt) for tt in range(NT)])
            def vdst(tt, b):
                src = ps[b][:, 0:256].rearrange("p (q s e) -> p q s e", q=2, s=2)
                dst = Vb[:, tt, :].rearrange("p (q c) -> p q c", q=2)
                P.op("act", lambda src=src, dst=dst: nc.scalar.copy(dst[:, :, 0:64], src[:, :, 0, :]), reads=[PK(b)], writes=[("V", tt)])
                P.op("act", lambda src=src, dst=dst: nc.scalar.copy(dst[:, :, 192:256], src[:, :, 1, :]), reads=[PK(b), ("V", tt)], writes=[("V", tt)])
            proj_v(0, 256, vdst, es)
            if dump is not None:
                P.flush()
                for tt in range(4):
                    dump2(5, 0, Vb[:, tt, :], 512) if tt == 0 else dump2(5, tt, Vb[:, tt, :], 512)
                P.flush()
            for pr in range(2):
                def score(st, i, J, c0, pr=pr):
                    h = 2 * pr + st
                    rows = slice(64 * st, 64 * st + 64)
                    be = bank("scm", [0, 1, 2, 3, 6, 7])
                    bs_ = bank("scm", [0, 1, 2, 3, 6, 7])
                    P.op("pe", lambda: nc.tensor.matmul(ps[be][:, c0:512], cb[:, CB_ROWSEL + h * 128: CB_ROWSEL + (h + 1) * 128],
                                                        Fs[:, J * 512 + c0:(J + 1) * 512], start=True, stop=(i < 4 * J)),
                         reads=["cb", "Fs"], writes=[PK(be)])
                    diag_mask(be, c0, i, J)
                    wi = bank("wt", [0, 1, 2])
                    P.op("act", lambda: nc.scalar.activation(wt[:, wi, c0:512], ps[be][:, c0:512], AF.Exp, bias=bcol[:, i * 4 + h:i * 4 + h + 1], scale=1.0),
                         reads=[PK(be), "bcol"], writes=[("wt", wi)])
                    P.op("pe", lambda: nc.tensor.matmul(ps[bs_][:, c0:512], Km[rows, pr, i * 128:(i + 1) * 128], Qm[rows, pr, J * 512 + c0:(J + 1) * 512],
                                                        start=True, stop=True),
                         reads=[("qk", 2 + pr), ("qk", pr)], writes=[PK(bs_)])
                    pti = bank("pt", [0, 1, 2])
                    P.op("dve", lambda: nc.vector.scalar_tensor_tensor(out=pt[:, pti, c0:512], in0=ps[bs_][:, c0:512], scalar=0.125, in1=wt[:, wi, c0:512],
                                                                       op0=ALU.mult, op1=ALU.mult),
                         reads=[PK(bs_), ("wt", wi)], writes=[("pt", pti)])
                    return pti

                def pv(st, i, J, c0, pti, first, last, pr=pr):
                    h = 2 * pr + st
                    P.op("pe", lambda: nc.tensor.matmul(ps[4 + st][:, c0:512], Vb[:, i, h * 128:(h + 1) * 128], pt[:, pti, c0:512], start=first, stop=last),
                         reads=[("V", i), ("pt", pti)], writes=[PK(4 + st)])

                smo = load_slab(U_MO + pr)

                def final(J, pr=pr, smo=smo):
                    bo = bank("scm", [0, 1, 2, 3, 6, 7])
                    proj_fm(smo, J, bo)
                    P.op("act", lambda: nc.scalar.activation(ft[:, 0, :], ps[bo][:, :], AF.Exp, scale=-1.0), reads=[PK(bo), "ft0"], writes=["ft0"])
                    P.op("act", lambda: nc.scalar.activation(ft[:, 0, :], ft[:, 0, :], AF.Ln, bias=1.0), reads=["ft0"], writes=["ft0"])
                    P.op("act", lambda: nc.scalar.activation(ft[:, 0, :], ft[:, 0, :], AF.Exp, scale=-1.0), reads=["ft0"], writes=["ft0"])
                    for st in range(2):
                        h = 2 * pr + st
                        o_ps = ps[4 + st]
                        wr = slice(64 * st, 64 * st + 64)
                        dr = slice(64 * (1 - st), 64 * (1 - st) + 64)
                        k1, k2 = ("ft1", st), ("ft2", st)
                        P.op("act", lambda o_ps=o_ps, wr=wr, dr=dr: nc.scalar.activation(ft[wr, 1, :], o_ps[dr, :], AF.Abs), reads=[PK(4 + st), k1], writes=[k1])
                        P.op("dve", lambda h=h, wr=wr: nc.vector.tensor_scalar(ft[wr, 1, :], ft[wr, 1, :], eM[wr, h:h + 1], None, op0=ALU.max),
                             reads=[k1, "eM"], writes=[k1])
                        P.op("act", lambda wr=wr: nc.scalar.activation(ft[wr, 1, :], ft[wr, 1, :], AF.Ln), reads=[k1], writes=[k1])
                        P.op("act", lambda wr=wr: nc.scalar.activation(ft[wr, 1, :], ft[wr, 1, :], AF.Exp, scale=-1.0), reads=[k1], writes=[k1])
                        P.op("dve", lambda o_ps=o_ps, wr=wr: nc.vector.tensor_tensor(ft[wr, 2, :], o_ps[wr, :], ft[wr, 1, :], ALU.mult),
                             reads=[PK(4 + st), k1, k2], writes=[k2])
                        P.op("dve", lambda wr=wr: nc.vector.tensor_tensor(mixT[wr, pr, J * 512:(J + 1) * 512], ft[wr, 2, :], ft[wr, 0, :], ALU.mult),
                             reads=[k2, "ft0"], writes=[("mixm%d" % st, pr, J)])

                attn_pair([0, 1], pv, final, score)
            P.flush()
            if dump is not None:
                for k in range(4):
                    dump2(5, 4 + k, ft[:, k, :], 512)
                dump2(3, 4, wt[:, 0, :], 512)
                dump2(3, 5, pt[:, 0, :], 512)
                P.flush()
        if dump is not None:
            dump(6, mixT[:, :, :], [])
            P.flush()
        if stop == "mlstm":
            return

        with contextlib.ExitStack() as es:
            Vb = sbt(es, "s_V", [128, NT, 512], BF16)
            P.op("dve", lambda: nc.vector.memset(Vb[:, :, :].rearrange("p t (h e) -> p t h e", h=4)[:, :, :, 64:128], 1.0),
                 writes=[("V", tt) for tt in range(NT)])
            Qa = [sbt(es, "qa%d" % i, [128, S], BF16) for i in range(2)]
            Ka = [sbt(es, "ka%d" % i, [128, S], BF16) for i in range(2)]
            ksq = sbt(es, "ksq", [128, 4, 512], BF16)
            kst = sbt(es, "kst", [128, 16], F32)
            kmf = sbt(es, "kmf", [128, 8], F32)
            kmb = [sbt(es, "kmb%d" % i, [64, 8], BF16) for i in range(2)]
            gw = sbt(es, "gw", [128, 64], F32)
            top8 = sbt(es, "top8", [128, 64], F32)
            selb = sbt(es, "selb", [128, 64], F32)
            ft = sbt(es, "sft", [128, 6, 512], F32)
            for i in range(2):
                P.op("dve", lambda i=i: nc.vector.memset(Qa[i][64:128, :], 0.0), writes=[("Qa", i, k) for k in range(4)] + [("Qrow", i)])
                P.op("dve", lambda i=i: nc.vector.memset(Ka[i][64:128, :], 0.0), writes=[("Ka", i)])
                P.op("pool", lambda s, i=i: nc.gpsimd.dma_start(out=Qa[i][72:76, :], in_=qrows_d).then_inc(s, 16),
                     reads=[("Qrow", i)], writes=[("Qrow", i)] + [("Qa", i, k) for k in range(4)], dma=1)
                P.op("dve", lambda i=i: nc.vector.memset(Ka[i][96:97, :], 1.0), reads=[("Ka", i)], writes=[("Ka", i)])

            def softmax_pair(uq, uk, slope_idx, moba, vcol_fn, pv_m, nacc, clear_sel=False):
                for st in range(2):
                    P.op("pool", lambda s, st=st: nc.gpsimd.dma_start(out=Ka[st][72:76, :], in_=krows_d[slope_idx[st]]).then_inc(s, 16),
                         reads=[("Ka", st)], writes=[("Ka", st)], dma=1)
                    if moba:
                        P.op("pool", lambda s, st=st: nc.gpsimd.dma_start(out=Ka[st][64:72, :], in_=bind_d).then_inc(s, 16),
                             reads=[("Ka", st)], writes=[("Ka", st)], dma=1)
                    elif clear_sel:
                        P.op("dve", lambda st=st: nc.vector.memset(Ka[st][64:72, :], 0.0), reads=[("Ka", st)], writes=[("Ka", st)])
                        P.op("dve", lambda st=st: nc.vector.memset(Qa[st][64:72, :], 0.0), reads=[("Qa", st, k) for k in range(4)],
                             writes=[("Qa", st, k) for k in range(4)])
                sk = load_slab(uk)
                sq_ = load_slab(uq)
                for blk in range(NBLK):
                    b = bank("pj", [0, 1, 2, 3])
                    proj_fm(sk, blk, b)
                    cs = slice(blk * 512, (blk + 1) * 512)
                    P.op("dve", lambda b=b, cs=cs: nc.vector.tensor_copy(Ka[0][0:64, cs], ps[b][0:64, :]), reads=[PK(b), ("Ka", 0)], writes=[("Ka", 0)])
                    P.op("dve", lambda b=b, cs=cs: nc.vector.tensor_copy(Ka[1][0:64, cs], ps[b][64:128, :]), reads=[PK(b), ("Ka", 1)], writes=[("Ka", 1)])
                    P.op("act", lambda b=b, blk=blk: nc.scalar.activation(ksq[:, blk, :], ps[b][:, :], AF.Square), reads=[PK(b)], writes=[("ksq", blk)])
                    if moba:
                        P.op("dve", lambda b=b, blk=blk: nc.vector.reduce_sum(out=kmf[:, 2 * blk:2 * blk + 2],
                                                                             in_=ps[b][:, :].rearrange("p (n k) -> p n k", n=2), axis=AX.X),
                             reads=[PK(b), "kmf"], writes=["kmf"])
                qbanks = []
                for blk in range(NBLK):
                    b = bank("pj", [0, 1, 2, 3])
                    qbanks.append(b)
                    proj_fm(sq_, blk, b)
                    cs = slice(blk * 512, (blk + 1) * 512)
                    P.op("act", lambda b=b, cs=cs: nc.scalar.activation(Qa[0][0:64, cs], ps[b][0:64, :], AF.Copy, scale=0.125),
                         reads=[PK(b), ("Qa", 0, blk)], writes=[("Qa", 0, blk)])
                    P.op("act", lambda b=b, cs=cs: nc.scalar.activation(Qa[1][0:64, cs], ps[b][64:128, :], AF.Copy, scale=0.125),
                         reads=[PK(b), ("Qa", 1, blk)], writes=[("Qa", 1, blk)])
                for blk in range(NBLK):
                    for st in range(2):
                        bn = bank("fin4", [4, 5, 6, 7])
                        rows = slice(64 * st, 64 * st + 64)
                        P.op("pe", lambda rows=rows, blk=blk, bn=bn: nc.tensor.matmul(ps[bn][0:1, :], cb[rows, CB_ONES:CB_ONES + 1], ksq[rows, blk, :],
                                                                                   start=True, stop=True),
                             reads=[("ksq", blk), "cb"], writes=[PK(bn)])
                        P.op("dve", lambda bn=bn, st=st, blk=blk: nc.vector.reduce_max(out=kst[0:1, st * 4 + blk: st * 4 + blk + 1], in_=ps[bn][0:1, :], axis=AX.X),
                             reads=[PK(bn), "kst"], writes=["kst"])
                for st in range(2):
                    P.op("dve", lambda st=st: nc.vector.reduce_max(out=kst[0:1, 8 + st:9 + st], in_=kst[0:1, st * 4:st * 4 + 4], axis=AX.X),
                         reads=["kst"], writes=["kst"])
                    P.op("dve", lambda st=st: nc.vector.tensor_scalar(kst[0:1, 10 + st:11 + st], kst[0:1, 8 + st:9 + st], -1.0 / 16, None, op0=ALU.mult),
                         reads=["kst"], writes=["kst"])
                if moba:
                    P.op("act", lambda: nc.scalar.activation(kmb[0][0:64, :], kmf[0:64, :], AF.Copy, scale=1.0 / 256), reads=["kmf"], writes=["kmb0"])
                    P.op("act", lambda: nc.scalar.activation(kmb[1][0:64, :], kmf[64:128, :], AF.Copy, scale=1.0 / 256), reads=["kmf"], writes=["kmb1"])
                for blk in range(NBLK):
                    b = qbanks[blk]
                    cs = slice(blk * 512, (blk + 1) * 512)
                    P.op("act", lambda b=b, blk=blk: nc.scalar.activation(ksq[:, blk, :], ps[b][:, :], AF.Square), reads=[PK(b)], writes=[("ksq", blk)])
                    for st in range(2):
                        bn = bank("fin4", [4, 5, 6, 7])
                        rows = slice(64 * st, 64 * st + 64)
                        P.op("pe", lambda rows=rows, blk=blk, bn=bn: nc.tensor.matmul(ps[bn][0:1, :], cb[rows, CB_ONES:CB_ONES + 1], ksq[rows, blk, :],
                                                                                   start=True, stop=True),
                             reads=[("ksq", blk), "cb"], writes=[PK(bn)])
                        P.op("act", lambda bn=bn, st=st, cs=cs: nc.scalar.activation(Qa[st][96:97, cs], ps[bn][0:1, :], AF.Identity,
                                                                                  bias=kst[0:1, 10 + st:11 + st], scale=-1.0 / 16),
                             reads=[PK(bn), "kst", ("Qa", st, blk)], writes=[("Qa", st, blk)])
                if moba:
                    for st in range(2):
                        bg = bank("fin", [6, 7])
                        for k in range(8):
                            qt = 8 + k
                            P.op("pe", lambda st=st, qt=qt, k=k, bg=bg: nc.tensor.matmul(ps[bg][:, k * 8:k * 8 + 8], Qa[st][0:64, qt * 128:(qt + 1) * 128],
                                                                                      kmb[st][0:64, :], start=True, stop=True),
                                 reads=[("Qa", st, qt // 4), "kmb%d" % st], writes=[PK(bg)])
                        P.op("dve", lambda: nc.vector.memset(gw[:], -1e30), reads=["gw"], writes=["gw"])
                        P.op("dve", lambda: nc.vector.memset(selb[:], 0.0), reads=["selb"], writes=["selb"])
                        for k in range(8):
                            j = (8 + k) // 2
                            P.op("dve", lambda bg=bg, j=j, k=k: nc.vector.tensor_copy(gw[:, k * 8:k * 8 + j], ps[bg][:, k * 8:k * 8 + j]),
                                 reads=[PK(bg), "gw"], writes=[("gw", k)])
                        for k in range(8):
                            P.op("dve", lambda k=k: nc.vector.max(out=top8[:, k * 8:k * 8 + 8], in_=gw[:, k * 8:k * 8 + 8]),
                                 reads=[("gw", k), "gw", "top8"], writes=[("top8", k)])
                        for k in range(8):
                            j = (8 + k) // 2
                            P.op("dve", lambda j=j, k=k: nc.vector.tensor_scalar(selb[:, k * 8:k * 8 + j], gw[:, k * 8:k * 8 + j], top8[:, k * 8 + 2:k * 8 + 3], 1.0,
                                                                                 op0=ALU.is_ge, op1=ALU.subtract),
                                 reads=[("gw", k), ("top8", k), "selb"], writes=[("selb", k)])
                        for hb in range(2):
                            bt = bank("pj", [0, 1, 2, 3])
                            for kk in range(4):
                                k = hb * 4 + kk
                                P.op("pe", lambda bt=bt, k=k, kk=kk: nc.tensor.transpose(ps[bt][0:8, kk * 128:(kk + 1) * 128], selb[:, k * 8:k * 8 + 8], identf[:]),
                                     reads=[("selb", k), "selb", "identf"], writes=[PK(bt)])
                            P.op("act", lambda st=st, hb=hb, bt=bt: nc.scalar.activation(Qa[st][64:72, 1024 + hb * 512:1024 + (hb + 1) * 512], ps[bt][0:8, :],
                                                                                     AF.Copy, scale=-NEG),
                                 reads=[PK(bt), ("Qa", st, 2 + hb)], writes=[("Qa", st, 2 + hb)])
                        P.op("dve", lambda: nc.vector.memset(top8[:, 0:1], 0.0), reads=[("gw", k) for k in range(8)] + [("top8", k) for k in range(8)] + [("selb", k) for k in range(8)],
                             writes=["gw", "top8", "selb"])

                def score(st, i, J, c0):
                    b = bank("sc", [0, 1, 2, 3])
                    P.op("pe", lambda: nc.tensor.matmul(ps[b][:, c0:512], Ka[st][0:97, i * 128:(i + 1) * 128], Qa[st][0:97, J * 512 + c0:(J + 1) * 512],
                                                        start=True, stop=(i < 4 * J)),
                         reads=[("Ka", st), ("Qa", st, J)], writes=[PK(b)])
                    diag_mask(b, c0, i, J)
                    pti = bank("pt", [0, 1, 2])
                    P.op("act", lambda: nc.scalar.activation(pt[:, pti, c0:512], ps[b][:, c0:512], AF.Exp), reads=[PK(b)], writes=[("pt", pti)])
                    return pti

                def pv(st, i, J, c0, pti, first, last):
                    for a in range(nacc):
                        ba = 4 + st * nacc + a
                        lhs = vcol_fn(st, i) if a == 0 else ones_bf
                        P.op("pe", lambda ba=ba, lhs=lhs: nc.tensor.matmul(ps[ba][:, c0:512], lhs, pt[:, pti, c0:512], start=first, stop=last),
                             reads=[("V", i), ("pt", pti), "cb"], writes=[PK(ba)])

                attn_pair([0, 1], pv, pv_m, score)

            def vdst_a(tt, b):
                P.op("act", lambda tt=tt, b=b: nc.scalar.copy(
                    Vb[:, tt, :].rearrange("p (h e) -> p h e", h=4)[:, :, 0:64], ps[b][:, 0:256].rearrange("p (h e) -> p h e", h=4)),
                    reads=[PK(b)], writes=[("V", tt)])
            proj_v(256, 256, vdst_a, es)
            for pr in range(2):
                def fin_moba(J, pr=pr):
                    for st in range(2):
                        o_ps = ps[4 + st]
                        P.op("act", lambda o_ps=o_ps: nc.scalar.activation(ft[0:64, 0, :], o_ps[64:128, :], AF.Ln), reads=[PK(4 + st), "sft0"], writes=["sft0"])
                        P.op("act", lambda: nc.scalar.activation(ft[0:64, 0, :], ft[0:64, 0, :], AF.Exp, scale=-1.0), reads=["sft0"], writes=["sft0"])
                        P.op("dve", lambda o_ps=o_ps, st=st: nc.vector.tensor_tensor(mixT[64 * st:64 * st + 64, 2 + pr, J * 512:(J + 1) * 512],
                                                                                     o_ps[0:64, :], ft[0:64, 0, :], ALU.mult),
                             reads=[PK(4 + st), "sft0"], writes=[("mixm", st, pr, J)])
                softmax_pair(U_AQ + pr, U_AK + pr, [2 * pr, 2 * pr + 1], True,
                             lambda st, i, pr=pr: Vb[:, i, (2 * pr + st) * 128:(2 * pr + st + 1) * 128], fin_moba, 1)
            if stop == "moba":
                P.flush()
                if dump is not None:
                    dump(7, mixT[:, :, :], [])
                    P.flush()
                return
            def vdst_d(tt, b):
                P.op("act", lambda tt=tt, b=b: nc.scalar.copy(Vb[:, tt, :], ps[b][:, :]), reads=[PK(b)], writes=[("V", tt)])
            proj_v(512, 512, vdst_d, es)
            neglam = lamt[:, 8 * l:8 * l + 1]
            gsub = lamt[:, 8 * l + 1:8 * l + 2]
            for h in range(4):
                def fin_diff(J, h=h):
                    P.op("act", lambda: nc.scalar.activation(ft[:, 0, :], ps[5][:, :], AF.Ln), reads=[PK(5), "sft0"], writes=["sft0"])
                    P.op("act", lambda: nc.scalar.activation(ft[:, 1, :], ps[7][:, :], AF.Ln), reads=[PK(7), "sft1"], writes=["sft1"])
                    P.op("act", lambda: nc.scalar.activation(ft[:, 0, :], ft[:, 0, :], AF.Exp, scale=-1.0), reads=["sft0"], writes=["sft0"])
                    P.op("act", lambda: nc.scalar.activation(ft[:, 1, :], ft[:, 1, :], AF.Exp, scale=-1.0), reads=["sft1"], writes=["sft1"])
                    P.op("dve", lambda: nc.vector.tensor_tensor(ft[:, 2, :], ps[4][:, :], ft[:, 0, :], ALU.mult), reads=[PK(4), "sft0", "sft2"], writes=["sft2"])
                    P.op("dve", lambda: nc.vector.tensor_tensor(ft[:, 3, :], ps[6][:, :], ft[:, 1, :], ALU.mult), reads=[PK(6), "sft1", "sft3"], writes=["sft3"])
                    P.op("dve", lambda: nc.vector.scalar_tensor_tensor(out=ft[:, 4, :], in0=ft[:, 3, :], scalar=neglam, in1=ft[:, 2, :], op0=ALU.mult, op1=ALU.add),
                         reads=["sft2", "sft3", "lamt", "sft4"], writes=["sft4"])
                    qi = bank("ksq", [0, 1])
                    P.op("act", lambda qi=qi: nc.scalar.activation(ksq[:, qi, :], ft[:, 4, :], AF.Square), reads=["sft4"], writes=[("ksq", qi)])
                    bn = bank("sc", [0, 1, 2, 3])
                    P.op("pe", lambda qi=qi, bn=bn: nc.tensor.matmul(ps[bn][:, :], ones_bf, ksq[:, qi, :], start=True, stop=True),
                         reads=[("ksq", qi), "cb"], writes=[PK(bn)])
                    P.op("act", lambda bn=bn: nc.scalar.activation(ft[:, 5, :], ps[bn][:, :], AF.Ln, bias=EPS, scale=1.0 / 128), reads=[PK(bn), "sft5"], writes=["sft5"])
                    P.op("act", lambda: nc.scalar.activation(ft[:, 5, :], ft[:, 5, :], AF.Exp, scale=-0.5), reads=["sft5"], writes=["sft5"])
                    P.op("dve", lambda: nc.vector.scalar_tensor_tensor(out=mixT[:, 4 + h, J * 512:(J + 1) * 512], in0=ft[:, 4, :], scalar=gsub, in1=ft[:, 5, :],
                                                                       op0=ALU.mult, op1=ALU.mult),
                         reads=["sft4", "sft5", "lamt"], writes=[("mixd", h, J)])
                softmax_pair(U_DQ + h, U_DK + h, [4 + h, 4 + h], False,
                             lambda st, i, h=h: Vb[:, i, h * 128:(h + 1) * 128], fin_diff, 2, clear_sel=(h == 0))
            P.flush()
        if dump is not None:
            dump(7, mixT[:, :, :], [])
            P.flush()
        if stop == "diff":
            return

        with contextlib.ExitStack() as es:
            wo = sbt(es, "wo", [128, 8, 1024], BF16)
            yb = sbt(es, "o_y", [128, 2, 8, 512], F32)
            tmp = sbt(es, "o_tmp", [128, 2, 512], F32)
            for dm in range(8):
                P.op("pool", lambda s, dm=dm: nc.gpsimd.dma_start(out=wo[:, dm, :], in_=wo_d[l, dm]).then_inc(s, 16), writes=[("wo", dm)], dma=1)

            def o_proj(blk):
                cs = slice(blk * 512, (blk + 1) * 512)
                ys = blk % 2
                for dm in range(8):
                    b = bank("yp", [4, 5, 6, 7])
                    for c in range(8):
                        P.op("pe", lambda c=c, dm=dm, b=b: nc.tensor.matmul(ps[b][:, :], wo[:, dm, c * 128:(c + 1) * 128], mixT[:, c, cs],
                                                                         start=(c == 0), stop=(c == 7)),
                             reads=[("wo", dm)], writes=[PK(b)])
                    if dm % 2:
                        P.op("act", lambda dm=dm, b=b: nc.scalar.copy(yb[:, ys, dm, :], ps[b][:, :]), reads=[PK(b)], writes=[("oy", ys, dm)])
                    else:
                        P.op("dve", lambda dm=dm, b=b: nc.vector.tensor_copy(yb[:, ys, dm, :], ps[b][:, :]), reads=[PK(b)], writes=[("oy", ys, dm)])

            def o_norm(blk):
                cs = slice(blk * 512, (blk + 1) * 512)
                ys = blk % 2
                rms_stats(es, "m", lambda c: yb[:, ys, c, :], lambda c: ("oy", ys, c), 8, 512, 1.0 / D, sqt, rst, [0, 1, 2, 3])
                for c in range(8):
                    ti = c % 2
                    P.op("dve", lambda c=c, ti=ti: nc.vector.scalar_tensor_tensor(out=tmp[:, ti, :], in0=yb[:, ys, c, :], scalar=gpost(c), in1=rst[:, :],
                                                                                  op0=ALU.mult, op1=ALU.mult),
                         reads=[("oy", ys, c), "mstat", "small"], writes=[("otmp", ti)])
                    if c % 2:
                        P.op("pool", lambda c=c, ti=ti: nc.gpsimd.tensor_tensor(xT[:, c, cs], xT[:, c, cs], tmp[:, ti, :], ALU.add),
                             reads=[("otmp", ti), X(c, blk)], writes=[X(c, blk)])
                    else:
                        P.op("dve", lambda c=c, ti=ti: nc.vector.tensor_tensor(xT[:, c, cs], xT[:, c, cs], tmp[:, ti, :], ALU.add),
                             reads=[("otmp", ti), X(c, blk)], writes=[X(c, blk)])

            o_proj(0)
            for blk in range(NBLK):
                if blk + 1 < NBLK:
                    o_proj(blk + 1)
                o_norm(blk)
            P.flush()


_CACHE = {}


def _prep_inputs(inputs):
    small, cbt, qrows, krows, blockind, identf = _host_tables(inputs)
    w = _host_weights(inputs)
    shared = {"small": small, "cb": cbt, "qrows": qrows, "krows": krows, "blockind": blockind, "identf": identf}
    shared.update(w)
    return shared


def kernel(**inputs):
    x = np.ascontiguousarray(np.asarray(inputs["x"], np.float32))
    shared = _prep_inputs(inputs)
    if "nc" not in _CACHE:
        _CACHE["nc"] = build_program()
    nc = _CACHE["nc"]
    in_maps = []
    for c in range(NCORES):
        m = {"x": x[c * SEQ_PER_CORE:(c + 1) * SEQ_PER_CORE]}
        m.update(shared)
        in_maps.append(m)
    res = run_bass_kernel_spmd(nc, in_maps, core_ids=list(range(NCORES)))
    out = np.concatenate([np.asarray(r["out"], np.float32) for r in res.results], axis=0)
    return out
```
